# Optimizing a Trainium2 kernel written in Bass

```python
import math
import jax, jax.numpy as jnp
from jax import lax
import numpy as np

D_MODEL = 1024
BATCH = 4
SEQ = 4096
DEPTH = 1

CTX_LEN = 256
GRID_W = 64
ROPE_BASE = 10000.0
NORM_EPS = 1e-6
Q_BLOCK = 128

MLA_HEADS = 8
MLA_NOPE = 64
MLA_ROPE = 32
MLA_V = 64
MLA_Q_RANK = 256
MLA_KV_RANK = 128
DIFF_HEADS = 4
DIFF_QK = 64
DIFF_V = 2 * DIFF_QK

MIX_WIDTH = MLA_HEADS * MLA_V + DIFF_HEADS * DIFF_V
IN_SPLITS = (
    MLA_Q_RANK,
    MLA_Q_RANK + MLA_KV_RANK,
    MLA_Q_RANK + MLA_KV_RANK + MLA_ROPE,
    MLA_Q_RANK + MLA_KV_RANK + MLA_ROPE + DIFF_HEADS * 2 * DIFF_QK,
    MLA_Q_RANK + MLA_KV_RANK + MLA_ROPE + 2 * DIFF_HEADS * 2 * DIFF_QK,
)
IN_COLS = IN_SPLITS[-1] + DIFF_HEADS * DIFF_V

N_EXPERTS = 256
TOP_K = 8
N_GROUPS = 8
TOPK_GROUPS = 4
EXPERT_FF = 256
SHARED_FF = 256
ROUTED_SCALE = 2.5
MOE_BLOCK = 128

kernel_name = 'hybrid_mla_diffattn_moe_dit'


def _rms(x, g):
    xf = x.astype(jnp.float32)
    y = xf * lax.rsqrt(jnp.mean(xf * xf, axis=-1, keepdims=True) + NORM_EPS)
    return (y * g.astype(jnp.float32)).astype(x.dtype)


def _modulate(h, shift, scale):
    return h * (1 + scale) + shift


def _axial_tables(n_ctx, rows, rot_dim):
    n_freq = rot_dim // 4
    inv = ROPE_BASE ** (-(jnp.arange(n_freq, dtype=jnp.float32) / n_freq))
    row = jnp.repeat(jnp.arange(rows, dtype=jnp.float32), GRID_W)
    col = jnp.tile(jnp.arange(GRID_W, dtype=jnp.float32), rows)
    theta = jnp.concatenate([row[:, None] * inv, col[:, None] * inv], axis=-1)
    theta = jnp.concatenate([jnp.zeros((n_ctx, 2 * n_freq), jnp.float32), theta], axis=0)
    return jnp.cos(theta), jnp.sin(theta)


def _rope(x, cos, sin):
    half = x.shape[-1] // 2
    x1, x2 = x[..., :half], x[..., half:]
    cos = cos.astype(x.dtype)
    sin = sin.astype(x.dtype)
    return jnp.concatenate([x1 * cos - x2 * sin, x1 * sin + x2 * cos], axis=-1)


def _sweep(fn, qs):
    def to_blocks(q):
        b, h, s, d = q.shape
        return q.reshape(b, h, s // Q_BLOCK, Q_BLOCK, d).transpose(2, 0, 1, 3, 4)
    out = lax.map(lambda args: fn(*args), tuple(to_blocks(q) for q in qs))
    nb, b, h, qb, dv = out.shape
    return out.transpose(1, 2, 0, 3, 4).reshape(b, h, nb * qb, dv)


def _attend(q, k, v, scale):
    s = jnp.einsum('bhqd,bhkd->bhqk', q, k, preferred_element_type=jnp.float32) * scale
    p = jax.nn.softmax(s, axis=-1)
    return jnp.einsum('bhqk,bhkd->bhqd', p.astype(v.dtype), v)


def _diff_attend(q1, q2, k1, k2, v, lam, scale):
    s1 = jnp.einsum('bhqd,bhkd->bhqk', q1, k1, preferred_element_type=jnp.float32) * scale
    s2 = jnp.einsum('bhqd,bhkd->bhqk', q2, k2, preferred_element_type=jnp.float32) * scale
    p = jax.nn.softmax(s1, axis=-1) - lam * jax.nn.softmax(s2, axis=-1)
    return jnp.einsum('bhqk,bhkd->bhqd', p.astype(v.dtype), v)


def _merge_heads(o):
    b, h, s, d = o.shape
    return o.transpose(0, 2, 1, 3).reshape(b, s, h * d)


def _mixer(h, n_ctx, lam_init, w_in, g_q, w_uq, g_kv, w_ukv, lq1, lk1, lq2, lk2, g_sub, w_out,
           cos_m, sin_m, cos_d, sin_d, with_ctx):
    b, n, _ = h.shape
    p = h @ w_in
    c_q, c_kv, k_r, dq, dk, dv = jnp.split(p, IN_SPLITS, axis=-1)

    q_m = (_rms(c_q, g_q) @ w_uq).reshape(b, n, MLA_HEADS, MLA_NOPE + MLA_ROPE).transpose(0, 2, 1, 3)
    q_m = jnp.concatenate([q_m[..., :MLA_NOPE], _rope(q_m[..., MLA_NOPE:], cos_m, sin_m)], axis=-1)
    kv = (_rms(c_kv, g_kv) @ w_ukv).reshape(b, n, MLA_HEADS, MLA_NOPE + MLA_V).transpose(0, 2, 1, 3)
    k_nope, v_m = kv[..., :MLA_NOPE], kv[..., MLA_NOPE:]
    k_r = _rope(k_r, cos_m, sin_m)
    k_m = jnp.concatenate([k_nope, jnp.broadcast_to(k_r[:, None], (b, MLA_HEADS, n, MLA_ROPE))], axis=-1)
    scale_m = 1.0 / math.sqrt(MLA_NOPE + MLA_ROPE)

    dq = _rope(dq.reshape(b, n, DIFF_HEADS, 2, DIFF_QK).transpose(0, 2, 3, 1, 4), cos_d, sin_d)
    dk = _rope(dk.reshape(b, n, DIFF_HEADS, 2, DIFF_QK).transpose(0, 2, 3, 1, 4), cos_d, sin_d)
    q1, q2 = dq[:, :, 0], dq[:, :, 1]
    k1, k2 = dk[:, :, 0], dk[:, :, 1]
    v_d = dv.reshape(b, n, DIFF_HEADS, DIFF_V).transpose(0, 2, 1, 3)
    lam = (jnp.exp(jnp.sum(lq1.astype(jnp.float32) * lk1.astype(jnp.float32)))
           - jnp.exp(jnp.sum(lq2.astype(jnp.float32) * lk2.astype(jnp.float32))) + lam_init)
    scale_d = 1.0 / math.sqrt(DIFF_QK)

    def combine(o_m, o_d):
        o_d = _rms(o_d, g_sub) * (1.0 - lam_init)
        return jnp.concatenate([_merge_heads(o_m), _merge_heads(o_d)], axis=-1) @ w_out

    o_m_lat = _sweep(lambda qb: _attend(qb, k_m, v_m, scale_m), (q_m[:, :, n_ctx:],))
    o_d_lat = _sweep(lambda a, c: _diff_attend(a, c, k1, k2, v_d, lam, scale_d),
                     (q1[:, :, n_ctx:], q2[:, :, n_ctx:]))
    y_lat = combine(o_m_lat, o_d_lat)
    y_ctx = None
    if with_ctx:
        o_m_ctx = _attend(q_m[:, :, :n_ctx], k_m[:, :, :n_ctx], v_m[:, :, :n_ctx], scale_m)
        o_d_ctx = _diff_attend(q1[:, :, :n_ctx], q2[:, :, :n_ctx], k1[:, :, :n_ctx], k2[:, :, :n_ctx],
                               v_d[:, :, :n_ctx], lam, scale_d)
        y_ctx = combine(o_m_ctx, o_d_ctx)
    return y_lat, y_ctx


def _swiglu(x, w_gate, w_up, w_down):
    return (jax.nn.silu(x @ w_gate) * (x @ w_up)) @ w_down


def _grouped_experts(hf, e_idx, w, w1, w3, w2):
    t, k = e_idx.shape
    n_assign = t * k
    n_exp = w1.shape[0]
    n_blocks = -(-(n_assign + n_exp * (MOE_BLOCK - 1)) // MOE_BLOCK)
    n_slots = n_blocks * MOE_BLOCK
    e_flat = e_idx.reshape(n_assign)
    tok_flat = jnp.repeat(jnp.arange(t, dtype=jnp.int32), k)
    w_flat = w.reshape(n_assign)
    order = jnp.argsort(e_flat)
    e_sorted = e_flat[order]
    counts = jnp.bincount(e_flat, length=n_exp)
    start = jnp.cumsum(counts) - counts
    padded = (counts + MOE_BLOCK - 1) // MOE_BLOCK * MOE_BLOCK
    pend = jnp.cumsum(padded)
    pstart = pend - padded
    dest = pstart[e_sorted] + (jnp.arange(n_assign, dtype=jnp.int32) - start[e_sorted])
    slot_tok = jnp.zeros((n_slots,), jnp.int32).at[dest].set(tok_flat[order])
    slot_w = jnp.zeros((n_slots,), w.dtype).at[dest].set(w_flat[order])
    blk_e = jnp.minimum(jnp.searchsorted(pend, jnp.arange(n_blocks, dtype=pend.dtype) * MOE_BLOCK, side='right'),
                        n_exp - 1)

    def block(args):
        tok, wt, e = args
        return _swiglu(hf[tok], w1[e], w3[e], w2[e]) * wt[:, None]

    y = lax.map(block, (slot_tok.reshape(n_blocks, MOE_BLOCK), slot_w.reshape(n_blocks, MOE_BLOCK), blk_e))
    return jax.ops.segment_sum(y.reshape(n_slots, -1), slot_tok, num_segments=t)


def _moe(h, w_r, b_r, w1, w3, w2, ws1, ws3, ws2):
    b, n, d = h.shape
    t = b * n
    hf = h.reshape(t, d)
    s = jax.nn.sigmoid(jnp.einsum('td,de->te', hf, w_r, preferred_element_type=jnp.float32))
    s_sel = s + b_r.astype(jnp.float32)
    per_group = N_EXPERTS // N_GROUPS
    grp_score = lax.top_k(s_sel.reshape(t, N_GROUPS, per_group), 2)[0].sum(-1)
    _, g_idx = lax.top_k(grp_score, TOPK_GROUPS)
    g_mask = jax.nn.one_hot(g_idx, N_GROUPS, dtype=jnp.float32).sum(1) > 0
    e_mask = jnp.repeat(g_mask, per_group, axis=1)
    _, e_idx = lax.top_k(jnp.where(e_mask, s_sel, -jnp.inf), TOP_K)
    wts = jnp.take_along_axis(s, e_idx, axis=1)
    wts = wts / jnp.sum(wts, axis=-1, keepdims=True) * ROUTED_SCALE
    routed = _grouped_experts(hf, e_idx, wts.astype(h.dtype), w1, w3, w2)
    shared = _swiglu(hf, ws1, ws3, ws2)
    return (routed + shared).reshape(b, n, d)


def setup_inputs(seed: int = 0) -> dict:
    key = jax.random.key(seed)
    ks = jax.random.split(key, 32)
    L, D = DEPTH, D_MODEL

    def nrm(k, shape, scale):
        return jax.random.normal(k, shape, jnp.float32) * scale

    return {
        'x': nrm(ks[0], (BATCH, SEQ, D), 1.0),
        'c': nrm(ks[1], (BATCH, D), 1.0),
        'ctx': nrm(ks[2], (BATCH, CTX_LEN, D), 1.0),
        'c_ctx': nrm(ks[3], (D,), 1.0),
        'w_mod': nrm(ks[4], (L, D, 6 * D), 0.5 * D ** -0.5),
        'b_mod': nrm(ks[5], (L, 6 * D), 0.02),
        'g_attn': 1.0 + nrm(ks[6], (L, D), 0.02),
        'g_ffn': 1.0 + nrm(ks[7], (L, D), 0.02),
        'w_in': nrm(ks[8], (L, D, IN_COLS), D ** -0.5),
        'g_q_lat': 1.0 + nrm(ks[9], (L, MLA_Q_RANK), 0.02),
        'w_uq': nrm(ks[10], (L, MLA_Q_RANK, MLA_HEADS * (MLA_NOPE + MLA_ROPE)), MLA_Q_RANK ** -0.5),
        'g_kv_lat': 1.0 + nrm(ks[11], (L, MLA_KV_RANK), 0.02),
        'w_ukv': nrm(ks[12], (L, MLA_KV_RANK, MLA_HEADS * (MLA_NOPE + MLA_V)), MLA_KV_RANK ** -0.5),
        'lam_q1': nrm(ks[13], (L, DIFF_QK), 0.1),
        'lam_k1': nrm(ks[14], (L, DIFF_QK), 0.1),
        'lam_q2': nrm(ks[15], (L, DIFF_QK), 0.1),
        'lam_k2': nrm(ks[16], (L, DIFF_QK), 0.1),
        'g_subln': 1.0 + nrm(ks[17], (L, DIFF_V), 0.02),
        'w_out': nrm(ks[18], (L, MIX_WIDTH, D), MIX_WIDTH ** -0.5),
        'w_router': nrm(ks[19], (L, D, N_EXPERTS), D ** -0.5),
        'router_bias': nrm(ks[20], (L, N_EXPERTS), 0.01),
        'w1': nrm(ks[21], (L, N_EXPERTS, D, EXPERT_FF), D ** -0.5),
        'w3': nrm(ks[22], (L, N_EXPERTS, D, EXPERT_FF), D ** -0.5),
        'w2': nrm(ks[23], (L, N_EXPERTS, EXPERT_FF, D), EXPERT_FF ** -0.5),
        'ws1': nrm(ks[24], (L, D, SHARED_FF), D ** -0.5),
        'ws3': nrm(ks[25], (L, D, SHARED_FF), D ** -0.5),
        'ws2': nrm(ks[26], (L, SHARED_FF, D), SHARED_FF ** -0.5),
        'g_final': 1.0 + nrm(ks[27], (D,), 0.02),
    }


def reference(x, c, ctx, c_ctx, w_mod, b_mod, g_attn, g_ffn, w_in, g_q_lat, w_uq, g_kv_lat, w_ukv,
              lam_q1, lam_k1, lam_q2, lam_k2, g_subln, w_out, w_router, router_bias, w1, w3, w2,
              ws1, ws3, ws2, g_final):
    n_lat = x.shape[1]
    n_ctx = ctx.shape[1]
    rows = n_lat // GRID_W
    cos_m, sin_m = _axial_tables(n_ctx, rows, MLA_ROPE)
    cos_d, sin_d = _axial_tables(n_ctx, rows, DIFF_QK)
    xc = ctx
    for l in range(DEPTH):
        with_ctx = l < DEPTH - 1
        lam_init = 0.8 - 0.6 * math.exp(-0.3 * l)
        mod_x = jnp.split(jax.nn.silu(c) @ w_mod[l] + b_mod[l], 6, axis=-1)
        mod_c = jnp.split(jax.nn.silu(c_ctx) @ w_mod[l] + b_mod[l], 6, axis=-1)
        sh1, sc1, gt1, sh2, sc2, gt2 = [m[:, None, :] for m in mod_x]
        csh1, csc1, cgt1, csh2, csc2, cgt2 = mod_c

        h = jnp.concatenate([_modulate(_rms(xc, g_attn[l]), csh1, csc1),
                             _modulate(_rms(x, g_attn[l]), sh1, sc1)], axis=1)
        y_lat, y_ctx = _mixer(h, n_ctx, lam_init, w_in[l], g_q_lat[l], w_uq[l], g_kv_lat[l], w_ukv[l],
                              lam_q1[l], lam_k1[l], lam_q2[l], lam_k2[l], g_subln[l], w_out[l],
                              cos_m, sin_m, cos_d, sin_d, with_ctx)
        x = x + gt1 * y_lat
        x = x + gt2 * _moe(_modulate(_rms(x, g_ffn[l]), sh2, sc2), w_router[l], router_bias[l],
                           w1[l], w3[l], w2[l], ws1[l], ws3[l], ws2[l])
        if with_ctx:
            xc = xc + cgt1 * y_ctx
            xc = xc + cgt2 * _moe(_modulate(_rms(xc, g_ffn[l]), csh2, csc2), w_router[l], router_bias[l],
                                  w1[l], w3[l], w2[l], ws1[l], ws3[l], ws2[l])
    return _rms(x, g_final)
```

```python
import math
from contextlib import ExitStack
import numpy as np
import concourse.bass as bass
import concourse.mybir as mybir
from concourse.bass_utils import run_bass_kernel_spmd

F32 = mybir.dt.float32
BF16 = mybir.dt.bfloat16
I32 = mybir.dt.int32
AF = mybir.ActivationFunctionType
ALU = mybir.AluOpType

NCORES = 8
D = 1024
NKT = 34
NQT = 16
QT0 = 18
EPS = 1e-6
NEXP = 256
CAP = 128


class Res:
    __slots__ = ("name", "w", "r", "dsem", "dcount")

    def __init__(self, name=""):
        self.name = name
        self.w = None
        self.r = {}
        self.dsem = None
        self.dcount = 0


class _Rec:
    def __init__(self):
        self.call = None

    def __getattr__(self, name):
        def f(*a, **k):
            self.call = (name, a, k)
            return self
        return f


def _eager(fn):
    rec = _Rec()
    fn(rec)
    name, a, k = rec.call
    return lambda e: getattr(e, name)(*a, **k)


class EngQ:
    def __init__(self, fw, name):
        self.name = name
        self.sem = fw.new_sem("q_" + name)
        self.count = 0
        self.waited = {}
        self.thunks = []


class FW:
    ENGS = ["sync", "scalar", "vector", "gpsimd", "tensor"]

    def __init__(self, nc, es):
        self.nc = nc
        self.es = es
        self.nsem = 0
        self.sem_pool = []
        self.q = {n: EngQ(self, n) for n in self.ENGS}
        self.same_engine_wait = {"scalar": True, "vector": True, "gpsimd": True,
                                 "tensor": False, "sync": False}

    def new_sem(self, name):
        self.nsem += 1
        return self.es.enter_context(self.nc.semaphore(f"{name}_{self.nsem}"))

    def _deps(self, reads, writes, nowaw):
        deps = []
        for r in reads:
            if r.w is not None:
                deps.append(r.w)
        for w in writes:
            if w.w is not None and not nowaw:
                deps.append(w.w)
            deps.extend(w.r.values())
        return deps

    def _waits(self, q, deps):
        waits = []
        for (sem, val, dq) in deps:
            if dq is q and not self.same_engine_wait[q.name]:
                continue
            k = id(sem)
            if q.waited.get(k, 0) >= val:
                continue
            q.waited[k] = val
            waits.append((sem, val))
        return waits

    def _commit(self, tok, reads, writes):
        for w in writes:
            w.w = tok
            w.r = {}
        for r in reads:
            k = id(tok[0])
            old = r.r.get(k)
            if old is None or old[1] < tok[1]:
                r.r[k] = tok

    def op(self, eng, fn, reads=(), writes=()):
        fn = _eager(fn)
        q = self.q[eng]
        waits = self._waits(q, self._deps(reads, writes, False))
        q.count += 1
        sem = q.sem

        def thunk(e):
            for (s, v) in waits:
                e.wait_ge(s, v)
            fn(e).then_inc(sem, 1)
        q.thunks.append(thunk)
        self._commit((sem, q.count, q), reads, writes)

    def dma(self, eng, fn, reads=(), writes=(), nowaw=False, eager=True):
        if eager:
            fn = _eager(fn)
        q = self.q[eng]
        waits = self._waits(q, self._deps(reads, writes, nowaw))
        dst = writes[0]
        if dst.dsem is None:
            if self.sem_pool:
                dst.dsem, dst.dcount = self.sem_pool.pop()
            else:
                dst.dsem = self.new_sem("d")
        dst.dcount += 16
        sem, val = dst.dsem, dst.dcount

        def thunk(e):
            for (s, v) in waits:
                e.wait_ge(s, v)
            fn(e).then_inc(sem, 16)
        q.thunks.append(thunk)
        self._commit((sem, val, None), reads, writes)

    def final_wait(self, eng, ress):
        q = self.q[eng]
        deps = []
        for r in ress:
            if r.w is not None:
                deps.append(r.w)
            deps.extend(r.r.values())
        waits = self._waits(q, deps)

        def thunk(e):
            for (s, v) in waits:
                e.wait_ge(s, v)
        q.thunks.append(thunk)

    def finish(self):
        with self.nc.Block() as block:
            for name in self.ENGS:
                q = self.q[name]
                if not q.thunks:
                    continue

                def body(e, q=q):
                    for t in q.thunks:
                        t(e)
                getattr(block, name)(body)


class Buf:
    def __init__(self, t, name):
        self.t = t
        self.R = Res(name)

    def __getitem__(self, k):
        return self.t[k]


def build_program(stage="full"):
    nc = bass.Bass("TRN2", target_bir_lowering=False)

    def din(name, shape, dt=F32):
        return nc.dram_tensor(name, list(shape), dt, kind="ExternalInput").ap()

    xk = din("xk", [NKT * 128, D])
    cv = din("cv", [2, D])
    tabm = din("tabm", [2, 128, NKT * 128])
    tabd = din("tabd", [2, 128, NKT * 128])
    w_mod = din("w_mod", [D, 6 * D])
    b_mod = din("b_mod", [1, 6 * D])
    g_attn = din("g_attn", [8, 128])
    g_ffn = din("g_ffn", [1, D])
    g_final = din("g_final", [1, D])
    w_cq = din("w_cq", [D, 256])
    w_ckv = din("w_ckv", [D, 128])
    w_krp = din("w_krp", [D, 2, 96])
    w_dq = din("w_dq", [D, 2, 512])
    w_dk = din("w_dk", [D, 2, 512])
    w_dv = din("w_dv", [D, 512])
    g_q = din("g_q", [2, 128])
    w_uq = din("w_uq", [256, 2, 8, 96])
    g_kv = din("g_kv", [128, 1])
    w_kn = din("w_kn", [128, 512])
    w_v = din("w_v", [128, 512])
    lam4 = din("lam4", [4, 64])
    g_sub = din("g_sub", [1, 128])
    w_out = din("w_out", [D, D])
    w_router = din("w_router", [D, 256])
    r_bias = din("r_bias", [1, 256])
    if stage in ("full", "dbg"):
        w1 = din("w1", [NEXP, D, 256])
        w3 = din("w3", [NEXP, D, 256])
        w2 = din("w2", [NEXP, 256, D])
    ws1 = din("ws1", [D, 256])
    ws3 = din("ws3", [D, 256])
    ws2 = din("ws2", [256, D])
    out = nc.dram_tensor("out", [NQT * 128, D], F32, kind="ExternalOutput").ap()
    xg = nc.dram_tensor("xg", [384 * 128, D], BF16, kind="Internal").ap()
    yg = nc.dram_tensor("yg", [384 * 128, D], BF16, kind="Internal").ap()
    h2_d = nc.dram_tensor("h2_d", [NQT * 128, D], F32, kind="Internal").ap()
    ao_d = nc.dram_tensor("ao_d", [NQT * 128, D], BF16, kind="Internal" if stage == "full" else "ExternalOutput").ap()

    with ExitStack() as es0:
        fw = FW(nc, es0)

        uniq = [0]

        def sb(es, name, shape, dt=F32):
            uniq[0] += 1
            name = f"{name}_{uniq[0]}"
            return Buf(es.enter_context(nc.sbuf_tensor(name, list(shape), dt)), name)

        def ring(es, name, n, shape, dt=F32):
            return [sb(es, f"{name}{i}", shape, dt) for i in range(n)]

        PB = [Buf(es0.enter_context(nc.psum_tensor(f"pb{i}", [128, 512], F32)), f"pb{i}") for i in range(8)]

        def pbf(i):
            return PB[i].t[:].bitcast(BF16)

        V = lambda fn, R=(), W=(): fw.op("vector", fn, [b.R for b in R], [b.R for b in W])
        A = lambda fn, R=(), W=(): fw.op("scalar", fn, [b.R for b in R], [b.R for b in W])
        G = lambda fn, R=(), W=(): fw.op("gpsimd", fn, [b.R for b in R], [b.R for b in W])
        T = lambda fn, R=(), W=(): fw.op("tensor", fn, [b.R for b in R], [b.R for b in W])

        def DM(eng, fn, R=(), W=(), nowaw=False, eager=True):
            fw.dma(eng, fn, [b.R if isinstance(b, Buf) else b for b in R],
                   [b.R if isinstance(b, Buf) else b for b in W], nowaw=nowaw, eager=eager)

        def scope_end(bufs):
            rs = [b.R if isinstance(b, Buf) else b for b in bufs]
            for en in FW.ENGS:
                fw.final_wait(en, rs)
            for r in rs:
                if r.dsem is not None:
                    fw.sem_pool.append((r.dsem, r.dcount))
                    r.dsem = None

        dumps = {}
        R_dump = Res("dumps")

        def dump(name, ap, shape, dt, R):
            if stage != "dbg" or name in dumps:
                return
            d = nc.dram_tensor("dmp_" + name, list(shape), dt, kind="ExternalOutput").ap()
            dumps[name] = R_dump
            DM("sync", lambda e: e.dma_start(out=d, in_=ap), R, [R_dump], nowaw=True)

        ident_f = sb(es0, "ident_f", [128, 128], F32)
        ident_b = sb(es0, "ident_b", [128, 128], BF16)
        ones_f = sb(es0, "ones_f", [128, 128], F32)
        ones_b = sb(es0, "ones_b", [128, 128], BF16)
        tri_b = sb(es0, "tri_b", [128, 128], BF16)
        mhalf = sb(es0, "mhalf", [128, 8], F32)
        sel2 = sb(es0, "sel2", [2, 2, 128], F32)
        iota_e = sb(es0, "iota_e", [128, 256], F32)
        with ExitStack() as es:
            it = sb(es, "it_i", [128, 128], I32)
            itf = sb(es, "it_f", [128, 128], F32)
            it2 = sb(es, "it2_i", [128, 256], I32)
            G(lambda e: e.iota(it[:], pattern=[[1, 128]], base=0, channel_multiplier=-1), [], [it])
            V(lambda e: e.tensor_copy(out=itf[:], in_=it[:]), [it], [itf])
            V(lambda e: e.tensor_single_scalar(out=ident_f[:], in_=itf[:], scalar=0.0, op=ALU.is_equal), [itf], [ident_f])
            V(lambda e: e.tensor_single_scalar(out=ident_b[:], in_=itf[:], scalar=0.0, op=ALU.is_equal), [itf], [ident_b])
            V(lambda e: e.tensor_single_scalar(out=tri_b[:], in_=itf[:], scalar=0.0, op=ALU.is_gt), [itf], [tri_b])
            G(lambda e: e.iota(it2[:], pattern=[[128, 256]], base=1, channel_multiplier=0), [], [it2])
            V(lambda e: e.tensor_copy(out=iota_e[:], in_=it2[:]), [it2], [iota_e])
            G(lambda e: e.memset(ones_f[:], 1.0), [], [ones_f])
            G(lambda e: e.memset(ones_b[:], 1.0), [], [ones_b])
            G(lambda e: e.memset(mhalf[:], -0.5), [], [mhalf])
            G(lambda e: e.memset(sel2[:, 0, :], 0.0), [], [sel2])
            G(lambda e: e.memset(sel2[0:1, 0, :], 1.0), [], [sel2])
            G(lambda e: e.memset(sel2[:, 1, :], 1.0), [], [sel2])
            G(lambda e: e.memset(sel2[0:1, 1, :], 0.0), [], [sel2])
            scope_end([it, itf, it2])

        modrow_d = nc.dram_tensor("modrow_d", [2, 6 * D], F32, kind="Internal").ap()
        R_modd = Res("modrow_d")
        G1T = sb(es0, "G1T", [128, 2, 8], F32)
        S1T = sb(es0, "S1T", [128, 2, 8], F32)
        with ExitStack() as es:
            svT = sb(es, "svT", [128, 2, 8], F32)
            modrow = sb(es, "modrow", [2, 6 * D], F32)
            bm = sb(es, "bm", [2, 6 * D], F32)
            gaT0 = sb(es, "gaT0", [8, 128], F32)
            gaT = sb(es, "gaT", [128, 8], F32)
            wblk = ring(es, "wblk", 2, [128, 8, 1024], F32)
            DM("sync", lambda e: e.dma_start(out=svT[:], in_=cv.rearrange("r (p k) -> p r k", k=8)), [], [svT])
            DM("sync", lambda e: e.dma_start(out=bm[0:1, :], in_=b_mod), [], [bm])
            DM("sync", lambda e: e.dma_start(out=bm[1:2, :], in_=b_mod), [], [bm])
            DM("sync", lambda e: e.dma_start(out=gaT0[:], in_=g_attn), [], [gaT0])
            A(lambda e: e.activation(out=svT[:], in_=svT[:], func=AF.Silu), [svT], [svT])
            wm = w_mod.rearrange("(p k) n -> p k n", k=8)
            for nb in range(6):
                wb_ = wblk[nb % 2]
                DM("sync", lambda e, wb_=wb_, nb=nb: e.dma_start(out=wb_[:], in_=wm[:, :, nb * 1024:(nb + 1) * 1024]), [], [wb_])
                for hf in range(2):
                    pb = PB[hf]
                    for k in range(8):
                        T(lambda e, k=k, pb=pb, wb_=wb_, hf=hf: e.matmul(pb[0:2, :], lhsT=svT[:, :, k], rhs=wb_[:, k, hf * 512:(hf + 1) * 512],
                                                                         start=(k == 0), stop=(k == 7)), [svT, wb_], [pb])
                    c0 = nb * 1024 + hf * 512
                    V(lambda e, pb=pb, c0=c0: e.tensor_tensor(out=modrow[:, c0:c0 + 512], in0=pb[0:2, :], in1=bm[:, c0:c0 + 512], op=ALU.add),
                      [pb, bm], [modrow])
            pt = PB[2]
            T(lambda e: e.transpose(out=pt[:, 0:8], in_=gaT0[:], identity=ident_f[0:8, 0:8]), [gaT0, ident_f], [pt])
            V(lambda e: e.tensor_copy(out=gaT[:], in_=pt[:, 0:8]), [pt], [gaT])
            pt3 = PB[3]
            for j in range(16):
                T(lambda e, j=j: e.transpose(out=pt3[:, j * 2:j * 2 + 2], in_=modrow[:, j * 128:(j + 1) * 128], identity=ident_f[0:2, 0:2]),
                  [modrow, ident_f], [pt3])
            ptv = pt3.t[:, 0:32].rearrange("p (j r) -> p r j", r=2)
            V(lambda e: e.tensor_copy(out=S1T[:], in_=ptv[:, :, 0:8]), [pt3], [S1T])
            for r in range(2):
                V(lambda e, r=r: e.scalar_tensor_tensor(out=G1T[:, r, :], in0=ptv[:, r, 8:16], scalar=1.0, in1=gaT[:],
                                                        op0=ALU.add, op1=ALU.mult), [pt3, gaT], [G1T])
            DM("sync", lambda e: e.dma_start(out=modrow_d, in_=modrow[:]), [modrow], [R_modd])
            dump("modrow", modrow[:], [2, 6 * D], F32, [modrow])
            dump("G1T", G1T[:], [128, 2, 8], F32, [G1T])
            dump("S1T", S1T[:], [128, 2, 8], F32, [S1T])
            scope_end([svT, bm, gaT0, gaT, modrow] + wblk)

        neglam = sb(es0, "neglam", [128, 1], F32)
        gsub08 = sb(es0, "gsub08", [128, 128], F32)
        with ExitStack() as es:
            lt = sb(es, "lt", [128, 4, 64], F32)
            lj = sb(es, "lj", [128, 64], F32)
            ld = sb(es, "ld", [128, 2], F32)
            DM("sync", lambda e: e.dma_start(out=lt[:], in_=lam4.rearrange("a b -> (a b)").partition_broadcast(128).rearrange("p (a b) -> p a b", a=4)), [], [lt])
            DM("sync", lambda e: e.dma_start(out=gsub08[:], in_=g_sub.rearrange("a b -> (a b)").partition_broadcast(128)), [], [gsub08])
            for i in range(2):
                V(lambda e: e.tensor_tensor(out=lj[:], in0=lt[:, 2 * i, :], in1=lt[:, 2 * i + 1, :], op=ALU.mult), [lt], [lj])
                V(lambda e: e.tensor_reduce(out=ld[:, i:i + 1], in_=lj[:], axis=mybir.AxisListType.X, op=ALU.add), [lj], [ld])
            A(lambda e: e.activation(out=ld[:], in_=ld[:], func=AF.Exp), [ld], [ld])
            V(lambda e: e.scalar_tensor_tensor(out=neglam[:], in0=ld[:, 1:2], scalar=-0.2, in1=ld[:, 0:1], op0=ALU.add, op1=ALU.subtract),
              [ld], [neglam])
            V(lambda e: e.tensor_scalar(out=gsub08[:], in0=gsub08[:], scalar1=0.8, scalar2=None, op0=ALU.mult), [gsub08], [gsub08])
            dump("neglam", neglam[:], [128, 1], F32, [neglam])
            dump("gsub08", gsub08[:], [128, 128], F32, [gsub08])
            scope_end([lt, lj, ld])

        R_ao = Res("ao_d")

        def attention_pass(kind):
            mla = kind == "mla"
            with ExitStack() as es:
                NH = 8 if mla else 4
                DV = 65 if mla else 129
                kT = sb(es, "kT", [128, NH, NKT * 128], BF16)
                Vaug = sb(es, "Vaug", [128, NKT, NH, DV], BF16)
                qT = sb(es, "qT", [128, NH, 512], BF16)
                xts = ring(es, "xt", 4, [128, D], F32)
                xsr = ring(es, "xs", 2, [128, D], BF16)
                junk = sb(es, "junk", [128, D], BF16)
                st = ring(es, "st", 4, [128, 4], F32)
                hTr = ring(es, "hT", 2, [128, 8, 512], BF16)
                tab = sb(es, "tab", [128, 2, 512], F32)
                tmpA = sb(es, "tmpA", [128, 512], F32)
                tmpB = sb(es, "tmpB", [128, 512], F32)
                PT = ring(es, "PT", 4, [128, 512], BF16)
                aos = sb(es, "aos", [128, 4, 512], BF16)
                rec = sb(es, "rec", [128, 4], F32)
                tab_d = tabm if mla else tabd
                G(lambda e: e.memset(Vaug[:, :, :, DV - 1:DV], 1.0), [], [Vaug])
                kp = lambda ap: ap.rearrange("(k p) n -> p k n", p=128)
                if mla:
                    wckv = sb(es, "wckv", [128, 8, 128], BF16)
                    wkrp = sb(es, "wkrp", [128, 8, 2, 96], BF16)
                    wcq = sb(es, "wcq", [128, 8, 256], BF16)
                    wuq = sb(es, "wuq", [128, 2, 2, 8, 96], BF16)
                    gq = sb(es, "gq", [128, 2], F32)
                    gq0 = sb(es, "gq0", [2, 128], F32)
                    wkn = sb(es, "wkn", [128, 512], BF16)
                    wv = sb(es, "wv", [128, 512], BF16)
                    gkv = sb(es, "gkv", [128, 1], F32)
                    sq = sb(es, "sq", [128, 2, 512], F32)
                    rb = sb(es, "rb", [128, 512], F32)
                    cn = sb(es, "cn", [128, 2, 512], BF16)
                    DM("gpsimd", lambda e: e.dma_start(out=wckv[:], in_=kp(w_ckv)), [], [wckv])
                    DM("gpsimd", lambda e: e.dma_start(out=wkrp[:], in_=w_krp.rearrange("(k p) a n -> p k a n", p=128)), [], [wkrp])
                    DM("gpsimd", lambda e: e.dma_start(out=wcq[:], in_=kp(w_cq)), [], [wcq])
                    DM("gpsimd", lambda e: e.dma_start(out=wuq[:].rearrange("p k a h n -> p k (a h n)"), in_=w_uq.rearrange("(k p) a h n -> p k (a h n)", p=128)), [], [wuq])
                    DM("gpsimd", lambda e: e.dma_start(out=wkn[:], in_=w_kn), [], [wkn])
                    DM("gpsimd", lambda e: e.dma_start(out=wv[:], in_=w_v), [], [wv])
                    DM("sync", lambda e: e.dma_start(out=gq0[:], in_=g_q), [], [gq0])
                    DM("sync", lambda e: e.dma_start(out=gkv[:], in_=g_kv), [], [gkv])
                    T(lambda e: e.transpose(out=PB[2][:, 0:2], in_=gq0[:], identity=ident_f[0:2, 0:2]), [gq0, ident_f], [PB[2]])
                    V(lambda e: e.tensor_copy(out=gq[:], in_=PB[2][:, 0:2]), [PB[2]], [gq])
                else:
                    wdk = sb(es, "wdk", [128, 8, 2, 512], BF16)
                    wdq = sb(es, "wdq", [128, 8, 2, 512], BF16)
                    wdv = sb(es, "wdv", [128, 8, 512], BF16)
                    t0 = sb(es, "t0", [128, 4, 128], F32)
                    t1 = sb(es, "t1", [128, 4, 128], F32)
                    ssq = sb(es, "ssq", [128, 4], F32)
                    DM("gpsimd", lambda e: e.dma_start(out=wdk[:], in_=w_dk.rearrange("(k p) a n -> p k a n", p=128)), [], [wdk])
                    DM("gpsimd", lambda e: e.dma_start(out=wdq[:], in_=w_dq.rearrange("(k p) a n -> p k a n", p=128)), [], [wdq])
                    DM("gpsimd", lambda e: e.dma_start(out=wdv[:], in_=kp(w_dv)), [], [wdv])

                cnt = {"x": 0, "s": 0, "g": 0}

                def make_hT(tiles):
                    hT = hTr[cnt["g"] % 2]
                    cnt["g"] += 1
                    cur = []
                    for ti, t in enumerate(tiles):
                        xt = xts[cnt["x"] % 4]
                        s_ = st[cnt["x"] % 4]
                        cnt["x"] += 1
                        cur.append((xt, s_))
                        DM("sync", lambda e: e.dma_start(out=xt[:], in_=xk[t * 128:(t + 1) * 128, :]), [], [xt])
                        A(lambda e: e.activation(out=junk[:], in_=xt[:], func=AF.Square, accum_out=s_[:, 0:1]), [xt], [junk, s_])
                        V(lambda e: e.tensor_scalar(out=s_[:, 1:2], in0=s_[:, 0:1], scalar1=1.0 / D, scalar2=EPS, op0=ALU.mult, op1=ALU.add), [s_], [s_])
                        G(lambda e: e.tensor_tensor(out=s_[:, 2:3], in0=s_[:, 1:2], in1=mhalf[:, 0:1], op=ALU.pow), [s_, mhalf], [s_])
                    for ti, t in enumerate(tiles):
                        xt, s_ = cur[ti]
                        r = 1 if t < 2 else 0
                        x_ = xsr[cnt["s"] % 2]
                        pbi = 2 + (cnt["s"] % 2)
                        cnt["s"] += 1
                        V(lambda e: e.tensor_scalar(out=x_[:], in0=xt[:], scalar1=s_[:, 2:3], scalar2=None, op0=ALU.mult), [xt, s_], [x_])
                        dump(kind + "_st0", s_[:], [128, 4], F32, [s_])
                        for k in range(8):
                            T(lambda e: e.transpose(out=pbf(pbi)[:, k * 128:(k + 1) * 128], in_=x_[:, k * 128:(k + 1) * 128], identity=ident_b[:]),
                              [x_, ident_b], [PB[pbi]])
                        for k in range(8):
                            A(lambda e: e.activation(out=hT[:, k, ti * 128:(ti + 1) * 128], in_=pbf(pbi)[:, k * 128:(k + 1) * 128],
                                                     func=AF.Identity, scale=G1T[:, r, k:k + 1], bias=S1T[:, r, k:k + 1]),
                              [PB[pbi], G1T, S1T], [hT])
                    return hT

                def load_tab(c0, n):
                    DM("sync", lambda e: e.dma_start(out=tab[:, :, 0:n], in_=tab_d[:, :, c0:c0 + n].rearrange("a p n -> p a n")), [], [tab])

                def rope_evac(pa, pb_, rows, n, dst_fn):
                    V(lambda e: e.tensor_tensor(out=tmpA[0:rows, 0:n], in0=pa[0:rows, 0:n], in1=tab[0:rows, 0, 0:n], op=ALU.mult), [pa, tab], [tmpA])
                    V(lambda e: e.tensor_tensor(out=tmpB[0:rows, 0:n], in0=pb_[0:rows, 0:n], in1=tab[0:rows, 1, 0:n], op=ALU.mult), [pb_, tab], [tmpB])
                    dst_fn()

                def rms_T(psrc_list, nchunk, n, width, dst, gain):
                    for c in range(nchunk):
                        A(lambda e, c=c: e.activation(out=sq[:, c, 0:n], in_=psrc_list[c][:, 0:n], func=AF.Square), [psrc_list[c]], [sq])
                    for c in range(nchunk):
                        T(lambda e, c=c: e.matmul(PB[3][:, 0:n], lhsT=ones_f[:], rhs=sq[:, c, 0:n], start=(c == 0), stop=(c == nchunk - 1)),
                          [ones_f, sq], [PB[3]])
                    A(lambda e: e.activation(out=rb[:, 0:n], in_=PB[3][:, 0:n], func=AF.Ln, scale=1.0 / width, bias=EPS), [PB[3]], [rb])
                    A(lambda e: e.activation(out=rb[:, 0:n], in_=rb[:, 0:n], func=AF.Exp, scale=-0.5), [rb], [rb])
                    for c in range(nchunk):
                        V(lambda e, c=c: e.scalar_tensor_tensor(out=dst[:, c, 0:n], in0=psrc_list[c][:, 0:n], scalar=gain[:, c:c + 1], in1=rb[:, 0:n],
                                                                op0=ALU.mult, op1=ALU.mult), [psrc_list[c], rb, gain], [dst])

                groups = [[0, 1]] + [list(range(2 + 4 * g, 6 + 4 * g)) for g in range(8)]
                for tiles in groups:
                    n = len(tiles) * 128
                    c0 = tiles[0] * 128
                    hT = make_hT(tiles)
                    load_tab(c0, n)
                    if mla:
                        for k in range(8):
                            T(lambda e, k=k: e.matmul(PB[0][:, 0:n], lhsT=wckv[:, k, :], rhs=hT[:, k, 0:n], start=(k == 0), stop=(k == 7)), [wckv, hT], [PB[0]])
                        rms_T([PB[0]], 1, n, 128.0, cn, gkv)
                        for a in range(2):
                            for k in range(8):
                                T(lambda e, k=k, a=a: e.matmul(PB[a][0:96, 0:n], lhsT=wkrp[:, k, a, :], rhs=hT[:, k, 0:n], start=(k == 0), stop=(k == 7)),
                                  [wkrp, hT], [PB[a]])

                        def fin():
                            for h in range(8):
                                V(lambda e, h=h: e.tensor_tensor(out=kT[64:96, h, c0:c0 + n], in0=tmpA[64:96, 0:n], in1=tmpB[64:96, 0:n], op=ALU.add),
                                  [tmpA, tmpB], [kT])
                        V(lambda e: e.tensor_tensor(out=tmpA[64:96, 0:n], in0=PB[0][64:96, 0:n], in1=tab[64:96, 0, 0:n], op=ALU.mult), [PB[0], tab], [tmpA])
                        V(lambda e: e.tensor_tensor(out=tmpB[64:96, 0:n], in0=PB[1][64:96, 0:n], in1=tab[64:96, 1, 0:n], op=ALU.mult), [PB[1], tab], [tmpB])
                        fin()
                        for h in range(8):
                            pb = PB[h % 2]
                            T(lambda e, h=h, pb=pb: e.matmul(pb[0:64, 0:n], lhsT=wkn[:, h * 64:(h + 1) * 64], rhs=cn[:, 0, 0:n], start=True, stop=True), [wkn, cn], [pb])
                            A(lambda e, h=h, pb=pb: e.activation(out=kT[0:64, h, c0:c0 + n], in_=pb[0:64, 0:n], func=AF.Copy), [pb], [kT])
                        for ti, t in enumerate(tiles):
                            T(lambda e, ti=ti: e.matmul(PB[3][:, :], lhsT=cn[:, 0, ti * 128:(ti + 1) * 128], rhs=wv[:], start=True, stop=True), [cn, wv], [PB[3]])
                            V(lambda e, t=t: e.tensor_copy(out=Vaug[:, t, :, 0:64], in_=PB[3].t[:, :].rearrange("p (h d) -> p h d", h=8)), [PB[3]], [Vaug])
                    else:
                        for h in range(4):
                            for a in range(2):
                                for k in range(8):
                                    T(lambda e, k=k, a=a, h=h: e.matmul(PB[a][:, 0:n], lhsT=wdk[:, k, a, h * 128:(h + 1) * 128], rhs=hT[:, k, 0:n],
                                                                        start=(k == 0), stop=(k == 7)), [wdk, hT], [PB[a]])
                            rope_evac(PB[0], PB[1], 128, n, lambda h=h: V(
                                lambda e: e.tensor_tensor(out=kT[:, h, c0:c0 + n], in0=tmpA[:, 0:n], in1=tmpB[:, 0:n], op=ALU.add), [tmpA, tmpB], [kT]))
                        for ti, t in enumerate(tiles):
                            for k in range(8):
                                T(lambda e, k=k, ti=ti: e.matmul(PB[3][:, :], lhsT=hT[:, k, ti * 128:(ti + 1) * 128], rhs=wdv[:, k, :], start=(k == 0), stop=(k == 7)),
                                  [hT, wdv], [PB[3]])
                            V(lambda e, t=t: e.tensor_copy(out=Vaug[:, t, :, 0:128], in_=PB[3].t[:, :].rearrange("p (h d) -> p h d", h=4)), [PB[3]], [Vaug])

                    dump(kind + "_hT0", hT[:, :, 0:256], [128, 8, 256], BF16, [hT])
                    dump(kind + "_tab0", tab[:], [128, 2, 512], F32, [tab])
                    dump(kind + "_kT0", kT[:, 0, 0:256], [128, 256], BF16, [kT])
                    dump(kind + "_V0", Vaug[:, 0, :, :], [128, NH, DV], BF16, [Vaug])
                    if mla:
                        dump("cn0", cn[:, 0, 0:256], [128, 256], BF16, [cn])
                        dump("rb0", rb[:, 0:256], [128, 256], F32, [rb])

                scale = 1.0 / math.sqrt(96.0) if mla else 1.0 / 8.0
                pt_i = 0
                s_i = 0
                o_i = 0
                for qc in range(4):
                    tiles = list(range(QT0 + 4 * qc, QT0 + 4 * qc + 4))
                    c0 = tiles[0] * 128
                    hT = make_hT(tiles)
                    load_tab(c0, 512)
                    if mla:
                        for c in range(2):
                            for k in range(8):
                                T(lambda e, k=k, c=c: e.matmul(PB[c][:, :], lhsT=wcq[:, k, c * 128:(c + 1) * 128], rhs=hT[:, k, :], start=(k == 0), stop=(k == 7)),
                                  [wcq, hT], [PB[c]])
                        rms_T([PB[0], PB[1]], 2, 512, 256.0, cn, gq)
                        for h in range(8):
                            for a in range(2):
                                for k in range(2):
                                    T(lambda e, k=k, a=a, h=h: e.matmul(PB[a][0:96, :], lhsT=wuq[:, k, a, h, :], rhs=cn[:, k, :], start=(k == 0), stop=(k == 1)),
                                      [wuq, cn], [PB[a]])
                            rope_evac(PB[0], PB[1], 96, 512, lambda h=h: V(
                                lambda e: e.tensor_tensor(out=qT[0:96, h, :], in0=tmpA[0:96, :], in1=tmpB[0:96, :], op=ALU.add), [tmpA, tmpB], [qT]))
                    else:
                        for h in range(4):
                            for a in range(2):
                                for k in range(8):
                                    T(lambda e, k=k, a=a, h=h: e.matmul(PB[a][:, :], lhsT=wdq[:, k, a, h * 128:(h + 1) * 128], rhs=hT[:, k, :],
                                                                        start=(k == 0), stop=(k == 7)), [wdq, hT], [PB[a]])
                            rope_evac(PB[0], PB[1], 128, 512, lambda h=h: V(
                                lambda e: e.tensor_tensor(out=qT[:, h, :], in0=tmpA[:, :], in1=tmpB[:, :], op=ALU.add), [tmpA, tmpB], [qT]))

                    dump(kind + "_qT0", qT[:, 0, :], [128, 512], BF16, [qT])
                    if mla:
                        dump("cnq", cn[:], [128, 2, 512], BF16, [cn])
                    LOOK = 2
                    SB = [PB[3], PB[4], PB[5]]
                    steps = [(h, j, kt) for h in range(NH) for j in range(1 if mla else 2) for kt in range(NKT)]
                    inflight = {}

                    def emit_S(si):
                        h, j, kt = steps[si]
                        KR = slice(0, 96) if mla else slice(j * 64, (j + 1) * 64)
                        ps = SB[si % 3]
                        p_ = PT[si % 4]
                        T(lambda e: e.matmul(ps[:, :], lhsT=kT[KR, h, kt * 128:(kt + 1) * 128], rhs=qT[KR, h, :], start=True, stop=True), [kT, qT], [ps])
                        A(lambda e: e.activation(out=p_[:], in_=ps[:, :], func=AF.Exp, scale=scale), [ps], [p_])
                        dump(kind + "_PT0", p_[:], [128, 512], BF16, [p_])
                        inflight[si] = p_

                    for si in range(min(LOOK, len(steps))):
                        emit_S(si)
                    for si, (h, j, kt) in enumerate(steps):
                        if si + LOOK < len(steps):
                            emit_S(si + LOOK)
                        p_ = inflight.pop(si)
                        if kt == 0:
                            if mla:
                                ob = [PB[6 + (o_i % 2)]]
                            else:
                                ob = [PB[6], PB[7]] if (o_i % 2 == 0) else [PB[0], PB[1]]
                            o_i += 1
                        for qs in range(4):
                            if mla:
                                o_, col = ob[0], qs * 65
                            else:
                                o_, col = ob[qs // 2], (qs % 2) * 129
                            first = (kt == 0) and (col == 0)
                            T(lambda e: e.matmul(o_[:, col:col + DV], lhsT=p_[:, qs * 128:(qs + 1) * 128], rhs=Vaug[:, kt, h, :],
                                                 start=first, stop=(kt == NKT - 1), skip_group_check=True), [p_, Vaug], [o_])
                        if kt != NKT - 1:
                            continue
                        for qs in range(4):
                            if mla:
                                o_, col = ob[0], qs * 65
                            else:
                                o_, col = ob[qs // 2], (qs % 2) * 129
                            V(lambda e: e.reciprocal(out=rec[:, qs:qs + 1], in_=o_[:, col + DV - 1:col + DV]), [o_], [rec])
                            if mla:
                                V(lambda e: e.tensor_scalar(out=aos[:, qs, h * 64:(h + 1) * 64], in0=o_[:, col:col + 64],
                                                            scalar1=rec[:, qs:qs + 1], scalar2=None, op0=ALU.mult), [o_, rec], [aos])
                            else:
                                tj = t0 if j == 0 else t1
                                V(lambda e: e.tensor_scalar(out=tj[:, qs, :], in0=o_[:, col:col + 128],
                                                            scalar1=rec[:, qs:qs + 1], scalar2=None, op0=ALU.mult), [o_, rec], [tj])
                        if (not mla) and j == 1:
                            V(lambda e: e.scalar_tensor_tensor(out=t0[:].rearrange("p a b -> p (a b)"), in0=t1[:].rearrange("p a b -> p (a b)"), scalar=neglam[:, 0:1],
                                                               in1=t0[:].rearrange("p a b -> p (a b)"), op0=ALU.mult, op1=ALU.add), [t0, t1, neglam], [t0])
                            V(lambda e: e.tensor_tensor(out=t1[:], in0=t0[:], in1=t0[:], op=ALU.mult), [t0], [t1])
                            V(lambda e: e.tensor_reduce(out=ssq[:], in_=t1[:], axis=mybir.AxisListType.X, op=ALU.add), [t1], [ssq])
                            V(lambda e: e.tensor_scalar(out=ssq[:], in0=ssq[:], scalar1=1.0 / 128, scalar2=EPS, op0=ALU.mult, op1=ALU.add), [ssq], [ssq])
                            G(lambda e: e.tensor_tensor(out=ssq[:], in0=ssq[:], in1=mhalf[:, 0:4], op=ALU.pow), [ssq, mhalf], [ssq])
                            for qs in range(4):
                                V(lambda e: e.scalar_tensor_tensor(out=aos[:, qs, h * 128:(h + 1) * 128], in0=t0[:, qs, :], scalar=ssq[:, qs:qs + 1],
                                                                   in1=gsub08[:], op0=ALU.mult, op1=ALU.mult), [t0, ssq, gsub08], [aos])
                    dump(kind + "_aos0", aos[:], [128, 4, 512], BF16, [aos])
                    cb = 0 if mla else 512
                    for qs in range(4):
                        r0 = (qc * 4 + qs) * 128
                        DM("sync", lambda e, qs=qs, r0=r0: e.dma_start(out=ao_d[r0:r0 + 128, cb:cb + 512], in_=aos[:, qs, :]), [aos], [R_ao], nowaw=True)
                scope_end([kT, Vaug, qT, junk, tab, tmpA, tmpB, aos, rec] + hTr + xts + xsr + st + PT)

        attention_pass("mla")
        attention_pass("diff")

        x1 = sb(es0, "x1", [128, NQT, D], F32)
        rows = sb(es0, "rows", [128, 5, D], F32)
        R_GT1, R_G2, R_SH2, R_GT2, R_GF = range(5)
        with ExitStack() as es:
            gff = sb(es, "gff", [128, D], F32)
            DM("sync", lambda e: e.dma_start(out=gff[:], in_=g_ffn.rearrange("a b -> (a b)").partition_broadcast(128)), [], [gff])
            DM("sync", lambda e: e.dma_start(out=rows[:, R_GF, :], in_=g_final.rearrange("a b -> (a b)").partition_broadcast(128)), [], [rows])
            for (dst, ch) in [(R_GT1, 2), (R_SH2, 3), (R_G2, 4), (R_GT2, 5)]:
                DM("sync", lambda e, dst=dst, ch=ch: e.dma_start(out=rows[:, dst, :], in_=modrow_d[0, ch * D:(ch + 1) * D].partition_broadcast(128)), [R_modd], [rows])
            V(lambda e: e.scalar_tensor_tensor(out=rows[:, R_G2, :], in0=rows[:, R_G2, :], scalar=1.0, in1=gff[:], op0=ALU.add, op1=ALU.mult), [rows, gff], [rows])
            wo = sb(es, "wo", [128, 8, D], BF16)
            DM("gpsimd", lambda e: e.dma_start(out=wo[:], in_=w_out.rearrange("(k p) n -> p k n", p=128)), [], [wo])
            aot = ring(es, "aot", 2, [128, D], BF16)
            aoT = ring(es, "aoT", 2, [128, 8, 128], BF16)
            xq = ring(es, "xq", 2, [128, D], F32)
            ty = ring(es, "ty", 2, [128, D], F32)
            for i in range(NQT):
                a_, aT_, x_, ty_ = aot[i % 2], aoT[i % 2], xq[i % 2], ty[i % 2]
                DM("sync", lambda e, a_=a_, i=i: e.dma_start(out=a_[:], in_=ao_d[i * 128:(i + 1) * 128, :]), [R_ao], [a_])
                DM("sync", lambda e, x_=x_, i=i: e.dma_start(out=x_[:], in_=xk[(QT0 + i) * 128:(QT0 + i + 1) * 128, :]), [], [x_])
                for k in range(8):
                    T(lambda e, k=k, a_=a_: e.transpose(out=pbf(2)[:, k * 128:(k + 1) * 128], in_=a_[:, k * 128:(k + 1) * 128], identity=ident_b[:]), [a_, ident_b], [PB[2]])
                A(lambda e, aT_=aT_: e.activation(out=aT_[:].rearrange("p k t -> p (k t)"), in_=pbf(2)[:, :], func=AF.Copy), [PB[2]], [aT_])
                for hf in range(2):
                    pb = PB[hf]
                    for k in range(8):
                        T(lambda e, k=k, pb=pb, aT_=aT_, hf=hf: e.matmul(pb[:, :], lhsT=aT_[:, k, :], rhs=wo[:, k, hf * 512:(hf + 1) * 512], start=(k == 0), stop=(k == 7)),
                          [aT_, wo], [pb])
                    V(lambda e, pb=pb, hf=hf, ty_=ty_: e.tensor_tensor(out=ty_[:, hf * 512:(hf + 1) * 512], in0=pb[:, :], in1=rows[:, R_GT1, hf * 512:(hf + 1) * 512], op=ALU.mult),
                      [pb, rows], [ty_])
                G(lambda e, i=i, ty_=ty_, x_=x_: e.tensor_tensor(out=x1[:, i, :], in0=ty_[:], in1=x_[:], op=ALU.add), [ty_, x_], [x1])
            scope_end([gff, wo] + aot + aoT + xq + ty)

        if stage == "dbg":
            dump("x1a", x1[:], [128, NQT, D], F32, [x1])
            for i in range(NQT):
                DM("sync", lambda e, i=i: e.dma_start(out=x1[:, i, :], in_=xk[(QT0 + i) * 128:(QT0 + i + 1) * 128, :]), [], [x1])
        if stage == "attn":
            R_out = Res("out")
            for i in range(NQT):
                DM("sync", lambda e, i=i: e.dma_start(out=out[i * 128:(i + 1) * 128, :], in_=x1[:, i, :]), [x1], [R_out], nowaw=True)
            scope_end([R_out, R_ao])
            fw.finish()
            return nc
        NBLK = 384
        idx_all = sb(es0, "idx_all", [128, NQT, 8], I32)
        wk_all = sb(es0, "wk_all", [128, NQT, 8], F32)
        idxw = sb(es0, "idxw", [128, NBLK], I32)
        R_xg = Res("xg")
        R_yg = Res("yg")
        R_h2d = Res("h2d")
        pk = lambda ap: ap.rearrange("(p k) n -> p k n", k=8)
        with ExitStack() as es:
            wr = sb(es, "wr", [128, 8, 256], F32)
            wsg = sb(es, "wsg", [128, 8, 512], F32)
            wsd = sb(es, "wsd", [128, 2, D], F32)
            rbias = sb(es, "rbias", [128, 256], F32)
            base = sb(es, "base", [128, 256], F32)
            cnt = sb(es, "cnt", [128, 256], F32)
            selb_all = sb(es, "selb_all", [128, NQT, 256], BF16)
            wn_all = sb(es, "wn_all", [128, NQT, 256], F32)
            DM("sync", lambda e: e.dma_start(out=wr[:], in_=pk(w_router)), [], [wr])
            DM("sync", lambda e: e.dma_start(out=wsg[:, :, 0:256], in_=pk(ws1)), [], [wsg])
            DM("sync", lambda e: e.dma_start(out=wsg[:, :, 256:512], in_=pk(ws3)), [], [wsg])
            DM("sync", lambda e: e.dma_start(out=wsd[:], in_=ws2.rearrange("(j p) n -> p j n", p=128)), [], [wsd])
            DM("sync", lambda e: e.dma_start(out=rbias[:], in_=r_bias.rearrange("a b -> (a b)").partition_broadcast(128)), [], [rbias])
            G(lambda e: e.memset(base[:], 0.0), [], [base])
            G(lambda e: e.memset(cnt[:], 0.0), [], [cnt])
            h2 = ring(es, "h2", 2, [128, D], F32)
            h2T = sb(es, "h2T", [128, 8, 128], F32)
            junk = sb(es, "junkf", [128, D], F32)
            st = ring(es, "st2", 2, [128, 4], F32)
            s_ = sb(es, "s_", [128, 256], F32)
            ssel = sb(es, "ssel", [128, 256], F32)
            m8 = sb(es, "m8", [128, 8, 8], F32)
            gs = sb(es, "gs", [128, 8], F32)
            gm = sb(es, "gm", [128, 8], F32)
            msk = sb(es, "msk", [128, 256], F32)
            self_ = sb(es, "self", [128, 256], F32)
            Dm = sb(es, "Dm", [128, 256], F32)
            d8 = sb(es, "d8", [128, 8], F32)
            wsum = sb(es, "wsum", [128, 2], F32)
            sg = sb(es, "sg", [128, 256], F32)
            hs = sb(es, "hs", [128, 256], F32)
            hsT = sb(es, "hsT", [128, 2, 128], F32)
            ysh = sb(es, "ysh", [128, D], F32)
            for i in range(NQT):
                h2_ = h2[i % 2]
                s2 = st[i % 2]
                A(lambda e: e.activation(out=junk[:], in_=x1[:, i, :], func=AF.Square, accum_out=s2[:, 0:1]), [x1], [junk, s2])
                V(lambda e: e.tensor_scalar(out=s2[:, 1:2], in0=s2[:, 0:1], scalar1=1.0 / D, scalar2=EPS, op0=ALU.mult, op1=ALU.add), [s2], [s2])
                G(lambda e: e.tensor_tensor(out=s2[:, 2:3], in0=s2[:, 1:2], in1=mhalf[:, 0:1], op=ALU.pow), [s2, mhalf], [s2])
                V(lambda e: e.scalar_tensor_tensor(out=junk[:], in0=x1[:, i, :], scalar=s2[:, 2:3], in1=rows[:, R_G2, :], op0=ALU.mult, op1=ALU.mult),
                  [x1, s2, rows], [junk])
                V(lambda e: e.tensor_tensor(out=h2_[:].rearrange("t (k p) -> t p k", k=8), in0=junk[:].rearrange("t (p k) -> t p k", k=8),
                                            in1=rows[:, R_SH2, :].rearrange("t (p k) -> t p k", k=8), op=ALU.add), [junk, rows], [h2_])
                DM("sync", lambda e: e.dma_start(out=h2_d[i * 128:(i + 1) * 128, :], in_=h2_[:]), [h2_], [R_h2d], nowaw=True)
                for hf in range(2):
                    for k in range(4):
                        kk = hf * 4 + k
                        T(lambda e: e.transpose(out=PB[hf][:, k * 128:(k + 1) * 128], in_=h2_[:, kk * 128:(kk + 1) * 128], identity=ident_f[:]),
                          [h2_, ident_f], [PB[hf]])
                    A(lambda e: e.activation(out=h2T[:, hf * 4:(hf + 1) * 4, :].rearrange("p k t -> p (k t)"), in_=PB[hf][:, :], func=AF.Copy), [PB[hf]], [h2T])
                for k in range(8):
                    T(lambda e: e.matmul(PB[2][:, 0:256], lhsT=h2T[:, k, :], rhs=wr[:, k, :], start=(k == 0), stop=(k == 7)), [h2T, wr], [PB[2]])
                A(lambda e: e.activation(out=s_[:], in_=PB[2][:, 0:256], func=AF.Sigmoid), [PB[2]], [s_])
                V(lambda e: e.tensor_tensor(out=ssel[:], in0=s_[:], in1=rbias[:], op=ALU.add), [s_, rbias], [ssel])
                for g in range(8):
                    V(lambda e: e.max(out=m8[:, g, :], in_=ssel[:, g * 32:(g + 1) * 32]), [ssel], [m8])
                V(lambda e: e.tensor_tensor(out=gs[:], in0=m8[:, :, 0], in1=m8[:, :, 1], op=ALU.add), [m8], [gs])
                V(lambda e: e.max(out=d8[:], in_=gs[:]), [gs], [d8])
                V(lambda e: e.tensor_scalar(out=gm[:], in0=gs[:], scalar1=d8[:, 3:4], scalar2=None, op0=ALU.is_ge), [gs, d8], [gm])
                for g in range(8):
                    V(lambda e: e.tensor_scalar(out=msk[:, g * 32:(g + 1) * 32], in0=ssel[:, g * 32:(g + 1) * 32], scalar1=4.0, scalar2=gm[:, g:g + 1],
                                                op0=ALU.add, op1=ALU.mult), [ssel, gm], [msk])
                V(lambda e: e.max(out=d8[:], in_=msk[:]), [msk], [d8])
                V(lambda e: e.tensor_scalar(out=self_[:], in0=msk[:], scalar1=d8[:, 7:8], scalar2=None, op0=ALU.is_ge), [msk, d8], [self_])
                V(lambda e: e.tensor_copy(out=selb_all[:, i, :], in_=self_[:]), [self_], [selb_all])
                V(lambda e: e.tensor_tensor(out=wn_all[:, i, :], in0=s_[:], in1=self_[:], op=ALU.mult), [s_, self_], [wn_all])
                V(lambda e: e.tensor_reduce(out=wsum[:, 0:1], in_=wn_all[:, i, :], axis=mybir.AxisListType.X, op=ALU.add), [wn_all], [wsum])
                V(lambda e: e.reciprocal(out=wsum[:, 1:2], in_=wsum[:, 0:1]), [wsum], [wsum])
                V(lambda e: e.tensor_scalar(out=wn_all[:, i, :], in0=wn_all[:, i, :], scalar1=wsum[:, 1:2], scalar2=2.5, op0=ALU.mult, op1=ALU.mult), [wn_all, wsum], [wn_all])
                T(lambda e: e.matmul(PB[3][:, 0:256], lhsT=ones_b[:], rhs=selb_all[:, i, :], start=True, stop=True), [ones_b, selb_all], [PB[3]])
                V(lambda e: e.tensor_tensor(out=cnt[:], in0=PB[3][:, 0:256], in1=cnt[:], op=ALU.add), [PB[3], cnt], [cnt])
                dump("h2_0", h2_[:], [128, D], F32, [h2_])
                dump("s_0", s_[:], [128, 256], F32, [s_])
                dump("self0", self_[:], [128, 256], F32, [self_])
                dump("gs0", gs[:], [128, 8], F32, [gs])
                for k in range(8):
                    T(lambda e: e.matmul(PB[4][:, :], lhsT=h2T[:, k, :], rhs=wsg[:, k, :], start=(k == 0), stop=(k == 7)), [h2T, wsg], [PB[4]])
                A(lambda e: e.activation(out=sg[:], in_=PB[4][:, 0:256], func=AF.Silu), [PB[4]], [sg])
                V(lambda e: e.tensor_tensor(out=hs[:], in0=PB[4][:, 256:512], in1=sg[:], op=ALU.mult), [PB[4], sg], [hs])
                for j in range(2):
                    T(lambda e: e.transpose(out=PB[5][:, j * 128:(j + 1) * 128], in_=hs[:, j * 128:(j + 1) * 128], identity=ident_f[:]), [hs, ident_f], [PB[5]])
                A(lambda e: e.activation(out=hsT[:].rearrange("p j t -> p (j t)"), in_=PB[5][:, 0:256], func=AF.Copy), [PB[5]], [hsT])
                for hf in range(2):
                    pb = PB[6 + hf]
                    for j in range(2):
                        T(lambda e: e.matmul(pb[:, :], lhsT=hsT[:, j, :], rhs=wsd[:, j, hf * 512:(hf + 1) * 512], start=(j == 0), stop=(j == 1)),
                          [hsT, wsd], [pb])
                    V(lambda e: e.tensor_tensor(out=ysh[:, hf * 512:(hf + 1) * 512], in0=pb[:, :], in1=rows[:, R_GT2, hf * 512:(hf + 1) * 512], op=ALU.mult),
                      [pb, rows], [ysh])
                G(lambda e: e.tensor_tensor(out=x1[:, i, :], in0=x1[:, i, :], in1=ysh[:], op=ALU.add), [x1, ysh], [x1])
            dump("x1s", x1[:], [128, NQT, D], F32, [x1])
            dump("cnt", cnt[:], [128, 256], F32, [cnt])
            ci = sb(es, "ci", [128, 256], I32)
            padf = sb(es, "padf", [128, 256], F32)
            pend = sb(es, "pend", [128, 256], F32)
            pst1 = sb(es, "pst1", [128, 256], F32)
            thr = sb(es, "thr", [128, 3], F32)
            thr_i = sb(es, "thr_i", [128, 3], I32)
            blk = sb(es, "blk", [128, 3], F32)
            dg = sb(es, "dg", [128, 128], F32)
            iop = sb(es, "iop", [128, 1], F32)
            iop_i = sb(es, "iop_i", [128, 1], I32)
            V(lambda e: e.memset(padf[:], 0.0), [], [padf])
            for m in range(16):
                V(lambda e: e.scalar_tensor_tensor(out=padf[:], in0=cnt[:], scalar=128.0 * m, in1=padf[:], op0=ALU.is_gt, op1=ALU.add), [cnt, padf], [padf])
            V(lambda e: e.tensor_scalar(out=padf[:], in0=padf[:], scalar1=128.0, scalar2=None, op0=ALU.mult), [padf], [padf])
            V(lambda e: e.memset(msk[:], 1.0), [], [msk])
            V(lambda e: e.tensor_tensor_scan(out=pend[:], data0=msk[:], data1=padf[:], initial=0.0, op0=ALU.mult, op1=ALU.add), [padf, msk], [pend])
            V(lambda e: e.tensor_tensor(out=pst1[:], in0=pend[:], in1=padf[:], op=ALU.subtract), [pend, padf], [pst1])
            V(lambda e: e.tensor_scalar(out=pst1[:], in0=pst1[:], scalar1=1.0, scalar2=None, op0=ALU.add), [pst1], [pst1])
            G(lambda e: e.iota(thr_i[:], pattern=[[128 * 128, 3]], base=0, channel_multiplier=128), [], [thr_i])
            V(lambda e: e.tensor_copy(out=thr[:], in_=thr_i[:]), [thr_i], [thr])
            G(lambda e: e.iota(iop_i[:], pattern=[[0, 1]], base=0, channel_multiplier=1), [], [iop_i])
            V(lambda e: e.tensor_copy(out=iop[:], in_=iop_i[:]), [iop_i], [iop])
            for j in range(3):
                V(lambda e: e.tensor_scalar(out=Dm[:], in0=pend[:], scalar1=thr[:, j:j + 1], scalar2=None, op0=ALU.is_le), [pend, thr], [Dm])
                V(lambda e: e.tensor_reduce(out=blk[:, j:j + 1], in_=Dm[:], axis=mybir.AxisListType.X, op=ALU.add), [Dm], [blk])
            for j in range(3):
                V(lambda e: e.tensor_scalar(out=dg[:], in0=ident_f[:], scalar1=blk[:, j:j + 1], scalar2=None, op0=ALU.mult), [ident_f, blk], [dg])
                T(lambda e: e.matmul(PB[2][:, 0:128], lhsT=ones_f[:], rhs=dg[:], start=True, stop=True), [ones_f, dg], [PB[2]])
                V(lambda e: e.tensor_scalar(out=idxw[:, j * 128:(j + 1) * 128], in0=PB[2][:, 0:128], scalar1=128.0, scalar2=iop[:, 0:1], op0=ALU.mult, op1=ALU.add),
                  [PB[2], iop], [idxw])
            dump("pend", pend[:], [128, 256], F32, [pend])
            dump("blk", blk[:], [128, 3], F32, [blk])
            dump("idxw", idxw[:], [128, NBLK], I32, [idxw])
            h2b = ring(es, "h2b", 2, [128, D], BF16)
            for i in range(NQT):
                h2f = h2[i % 2]
                h2_ = h2b[i % 2]
                DM("sync", lambda e: e.dma_start(out=h2f[:], in_=h2_d[i * 128:(i + 1) * 128, :]), [R_h2d], [h2f])
                A(lambda e: e.activation(out=h2_[:], in_=h2f[:], func=AF.Copy), [h2f], [h2_])
                T(lambda e: e.matmul(PB[3][:, 0:256], lhsT=tri_b[:], rhs=selb_all[:, i, :], start=True, stop=True), [tri_b, selb_all], [PB[3]])
                T(lambda e: e.matmul(PB[3][:, 256:512], lhsT=ones_b[:], rhs=selb_all[:, i, :], start=False, stop=True, skip_group_check=True), [ones_b, selb_all], [PB[3]])
                V(lambda e: e.tensor_tensor(out=Dm[:], in0=PB[3][:, 0:256], in1=base[:], op=ALU.add), [PB[3], base], [Dm])
                V(lambda e: e.tensor_tensor(out=Dm[:], in0=Dm[:], in1=pst1[:], op=ALU.add), [Dm, pst1], [Dm])
                V(lambda e: e.tensor_tensor(out=Dm[:], in0=Dm[:], in1=selb_all[:, i, :], op=ALU.mult), [Dm, selb_all], [Dm])
                V(lambda e: e.tensor_tensor(out=base[:], in0=PB[3][:, 256:512], in1=base[:], op=ALU.add), [PB[3], base], [base])
                V(lambda e: e.max(out=d8[:], in_=Dm[:]), [Dm], [d8])
                V(lambda e: e.tensor_scalar(out=idx_all[:, i, :], in0=d8[:], scalar1=-1.0, scalar2=None, op0=ALU.add), [d8], [idx_all])
                for k in range(8):
                    V(lambda e: e.tensor_scalar(out=msk[:], in0=Dm[:], scalar1=d8[:, k:k + 1], scalar2=None, op0=ALU.is_equal), [Dm, d8], [msk])
                    V(lambda e: e.tensor_tensor(out=msk[:], in0=msk[:], in1=wn_all[:, i, :], op=ALU.mult), [msk, wn_all], [msk])
                    V(lambda e: e.tensor_reduce(out=wk_all[:, i, k:k + 1], in_=msk[:], axis=mybir.AxisListType.X, op=ALU.add), [msk], [wk_all])
                dump("Dm0", Dm[:], [128, 256], F32, [Dm])
                for k in range(8):
                    DM("gpsimd", lambda e: e.indirect_dma_start(out=xg, out_offset=bass.IndirectOffsetOnAxis(ap=idx_all[:, i, k:k + 1], axis=0),
                                                                in_=h2_[:], in_offset=None), [h2_, idx_all], [R_xg], nowaw=True)
            dump("idx", idx_all[:], [128, NQT, 8], I32, [idx_all])
            dump("wk", wk_all[:], [128, NQT, 8], F32, [wk_all])
            dump("base", base[:], [128, 256], F32, [base])
            allb = [wr, wsg, wsd, rbias, base, cnt, selb_all, wn_all, h2T, junk, s_, ssel, m8, gs, gm, msk, self_, Dm, d8, wsum, sg, hs, hsT, ysh,
                    ci, padf, pend, pst1, thr, thr_i, blk, dg, iop, iop_i] + h2 + st + h2b
            scope_end(allb)

        w1v = w1.rearrange("e (p k) n -> (e p) (k n)", k=8)
        w3v = w3.rearrange("e (p k) n -> (e p) (k n)", k=8)
        w2v = w2.rearrange("e (p j) n -> (e p) (j n)", j=2)
        with ExitStack() as es:
            wg1f = ring(es, "wg1f", 2, [128, 2048], F32)
            wg3f = ring(es, "wg3f", 2, [128, 2048], F32)
            wdf = ring(es, "wdf", 2, [128, 2048], F32)
            wg1 = ring(es, "wg1", 2, [128, 8, 256], BF16)
            wg3 = ring(es, "wg3", 2, [128, 8, 256], BF16)
            wd = ring(es, "wd", 2, [128, 2, D], BF16)
            Xe = ring(es, "Xe", 2, [128, D], BF16)
            XeT = ring(es, "XeT", 2, [128, 8, 128], BF16)
            sgr = ring(es, "sgr", 2, [128, 256], F32)
            he = ring(es, "he", 2, [128, 256], BF16)
            heT = ring(es, "heT", 2, [128, 2, 128], BF16)
            Ye = ring(es, "Ye", 2, [128, D], BF16)

            breg = {}

            def gat(e, dst, src, off):
                if "r" not in breg:
                    breg["r"] = e.to_reg(NEXP * 128 - 1)
                return e.indirect_dma_start(out=dst, out_offset=None, in_=src, in_offset=off, bounds_check=breg["r"], oob_is_err=False)

            def gathers(bi):
                b = bi % 2
                off = bass.IndirectOffsetOnAxis(ap=idxw[:, bi:bi + 1], axis=0)
                DM("gpsimd", lambda e, d=wg1f[b][:], o=off: gat(e, d, w1v, o), [idxw], [wg1f[b]], eager=False)
                DM("gpsimd", lambda e, d=wg3f[b][:], o=off: gat(e, d, w3v, o), [idxw], [wg3f[b]], eager=False)
                DM("gpsimd", lambda e, d=wdf[b][:], o=off: gat(e, d, w2v, o), [idxw], [wdf[b]], eager=False)

            def casts(bi):
                b = bi % 2
                A(lambda e: e.activation(out=wg1[b][:].rearrange("p k n -> p (k n)"), in_=wg1f[b][:], func=AF.Copy), [wg1f[b]], [wg1[b]])
                V(lambda e: e.tensor_copy(out=wg3[b][:].rearrange("p k n -> p (k n)"), in_=wg3f[b][:]), [wg3f[b]], [wg3[b]])
                A(lambda e: e.activation(out=wd[b][:].rearrange("p j n -> p (j n)"), in_=wdf[b][:], func=AF.Copy), [wdf[b]], [wd[b]])
                DM("sync", lambda e: e.dma_start(out=Xe[b][:], in_=xg[bi * 128:(bi + 1) * 128, :]), [R_xg], [Xe[b]])

            def stage1(bi):
                b = bi % 2
                for k in range(8):
                    T(lambda e: e.transpose(out=pbf(0)[:, k * 128:(k + 1) * 128], in_=Xe[b][:, k * 128:(k + 1) * 128], identity=ident_b[:]), [Xe[b], ident_b], [PB[0]])
                A(lambda e: e.activation(out=XeT[b][:].rearrange("p k t -> p (k t)"), in_=pbf(0)[:, :], func=AF.Copy), [PB[0]], [XeT[b]])
                pg = PB[2 + b]
                for k in range(8):
                    T(lambda e: e.matmul(pg[:, 0:256], lhsT=XeT[b][:, k, :], rhs=wg1[b][:, k, :], start=(k == 0), stop=(k == 7)), [XeT[b], wg1[b]], [pg])
                for k in range(8):
                    T(lambda e: e.matmul(pg[:, 256:512], lhsT=XeT[b][:, k, :], rhs=wg3[b][:, k, :], start=(k == 0), stop=(k == 7), skip_group_check=True), [XeT[b], wg3[b]], [pg])

            def stage2(bi):
                b = bi % 2
                pg = PB[2 + b]
                A(lambda e: e.activation(out=sgr[b][:], in_=pg[:, 0:256], func=AF.Silu), [pg], [sgr[b]])
                V(lambda e: e.tensor_tensor(out=he[b][:].rearrange("t (j p) -> t p j", j=2), in0=pg.t[:, 256:512].rearrange("t (p j) -> t p j", j=2),
                                            in1=sgr[b][:].rearrange("t (p j) -> t p j", j=2), op=ALU.mult), [pg, sgr[b]], [he[b]])
                for j in range(2):
                    T(lambda e: e.transpose(out=pbf(1)[:, j * 128:(j + 1) * 128], in_=he[b][:, j * 128:(j + 1) * 128], identity=ident_b[:]), [he[b], ident_b], [PB[1]])
                V(lambda e: e.tensor_copy(out=heT[b][:].rearrange("p j t -> p (j t)"), in_=pbf(1)[:, 0:256]), [PB[1]], [heT[b]])
                for hf in range(2):
                    pb = PB[4 + 2 * b + hf]
                    for j in range(2):
                        T(lambda e: e.matmul(pb[:, :], lhsT=heT[b][:, j, :], rhs=wd[b][:, j, hf * 512:(hf + 1) * 512], start=(j == 0), stop=(j == 1)),
                          [heT[b], wd[b]], [pb])
                V(lambda e: e.tensor_copy(out=Ye[b][:, 0:512], in_=PB[4 + 2 * b][:, :]), [PB[4 + 2 * b]], [Ye[b]])
                V(lambda e: e.tensor_copy(out=Ye[b][:, 512:1024], in_=PB[5 + 2 * b][:, :]), [PB[5 + 2 * b]], [Ye[b]])
                dump("he0", he[b][:], [128, 256], BF16, [he[b]])
                dump("Ye0", Ye[b][:], [128, D], BF16, [Ye[b]])
                DM("sync", lambda e: e.dma_start(out=yg[bi * 128:(bi + 1) * 128, :], in_=Ye[b][:]), [Ye[b]], [R_yg], nowaw=True)

            gathers(0)
            gathers(1)
            casts(0)
            stage1(0)
            for bi in range(NBLK):
                if bi + 2 < NBLK:
                    gathers(bi + 2)
                if bi + 1 < NBLK:
                    casts(bi + 1)
                    stage1(bi + 1)
                stage2(bi)
            allb = wg1f + wg3f + wdf + wg1 + wg3 + wd + Xe + XeT + sgr + he + heT + Ye
            scope_end(allb)

        R_out = Res("out")
        with ExitStack() as es:
            Yg = ring(es, "Yg", 4, [128, D], BF16)
            acc = ring(es, "acc", 2, [128, D], F32)
            st = ring(es, "st3", 2, [128, 4], F32)
            junk = sb(es, "junk3", [128, D], F32)
            gi = 0
            for i in range(NQT):
                ac = acc[i % 2]
                s2 = st[i % 2]
                for k in range(8):
                    y_ = Yg[gi % 4]
                    gi += 1
                    DM("gpsimd", lambda e, y_=y_, i=i, k=k: e.indirect_dma_start(out=y_[:], out_offset=None, in_=yg,
                                                                                 in_offset=bass.IndirectOffsetOnAxis(ap=idx_all[:, i, k:k + 1], axis=0)),
                       [R_yg, idx_all], [y_])
                    if k == 0:
                        V(lambda e, y_=y_, ac=ac, i=i: e.tensor_scalar(out=ac[:], in0=y_[:], scalar1=wk_all[:, i, 0:1], scalar2=None, op0=ALU.mult), [y_, wk_all], [ac])
                    else:
                        V(lambda e, y_=y_, ac=ac, i=i, k=k: e.scalar_tensor_tensor(out=ac[:], in0=y_[:], scalar=wk_all[:, i, k:k + 1], in1=ac[:], op0=ALU.mult, op1=ALU.add),
                          [y_, wk_all, ac], [ac])
                V(lambda e, ac=ac: e.tensor_tensor(out=ac[:], in0=ac[:], in1=rows[:, R_GT2, :], op=ALU.mult), [ac, rows], [ac])
                V(lambda e, ac=ac, i=i: e.tensor_tensor(out=ac[:], in0=ac[:], in1=x1[:, i, :], op=ALU.add), [ac, x1], [ac])
                A(lambda e, ac=ac, s2=s2: e.activation(out=junk[:], in_=ac[:], func=AF.Square, accum_out=s2[:, 0:1]), [ac], [junk, s2])
                V(lambda e, s2=s2: e.tensor_scalar(out=s2[:, 1:2], in0=s2[:, 0:1], scalar1=1.0 / D, scalar2=EPS, op0=ALU.mult, op1=ALU.add), [s2], [s2])
                G(lambda e, s2=s2: e.tensor_tensor(out=s2[:, 2:3], in0=s2[:, 1:2], in1=mhalf[:, 0:1], op=ALU.pow), [s2, mhalf], [s2])
                V(lambda e, ac=ac, s2=s2: e.scalar_tensor_tensor(out=ac[:], in0=ac[:], scalar=s2[:, 2:3], in1=rows[:, R_GF, :], op0=ALU.mult, op1=ALU.mult),
                  [ac, s2, rows], [ac])
                DM("sync", lambda e, ac=ac, i=i: e.dma_start(out=out[i * 128:(i + 1) * 128, :], in_=ac[:]), [ac], [R_out], nowaw=True)
            allb = Yg + acc + st + [junk]
            scope_end(allb + [R_out, R_dump])
        fw.finish()
    return nc


def _rope_tables(order_pos):
    n = order_pos.shape[0]
    pos = np.maximum(order_pos, 0)
    row = (pos // 64).astype(np.float32)
    col = (pos % 64).astype(np.float32)
    isctx = (order_pos < 0)

    def theta(rot_dim):
        nf = rot_dim // 4
        inv = (np.float32(10000.0) ** (-(np.arange(nf, dtype=np.float32) / np.float32(nf)))).astype(np.float32)
        th = np.concatenate([row[:, None] * inv, col[:, None] * inv], axis=-1).astype(np.float32)
        th[isctx] = 0.0
        return th

    thm = theta(32)
    thd = theta(64)
    tabm = np.zeros((2, 128, n), np.float32)
    tabm[0, 0:64, :] = 1.0
    cm, sm = np.cos(thm).T, np.sin(thm).T
    tabm[0, 64:80], tabm[0, 80:96] = cm, cm
    tabm[1, 64:80], tabm[1, 80:96] = -sm, sm
    tabd = np.zeros((2, 128, n), np.float32)
    cd, sd = np.cos(thd).T, np.sin(thd).T
    for g in range(2):
        tabd[0, g * 64:g * 64 + 32], tabd[0, g * 64 + 32:g * 64 + 64] = cd, cd
        tabd[1, g * 64:g * 64 + 32], tabd[1, g * 64 + 32:g * 64 + 64] = -sd, sd
    return tabm, tabd


def _swap_halves(w, group):
    shp = w.shape
    v = w.reshape(shp[0], -1, 2, group // 2)
    return np.ascontiguousarray(v[:, :, ::-1, :]).reshape(shp)


_PROGRAM = None


def kernel(x, c, ctx, c_ctx, w_mod, b_mod, g_attn, g_ffn, w_in, g_q_lat, w_uq, g_kv_lat, w_ukv,
           lam_q1, lam_k1, lam_q2, lam_k2, g_subln, w_out, w_router, router_bias, w1, w3, w2,
           ws1, ws3, ws2, g_final):
    global _PROGRAM
    f = lambda a: np.ascontiguousarray(np.asarray(a, dtype=np.float32))
    x, c, ctx, c_ctx = f(x), f(c), f(ctx), f(c_ctx)
    w_in0 = f(w_in)[0]
    cq, ckv, kr = w_in0[:, 0:256], w_in0[:, 256:384], w_in0[:, 384:416]
    dq, dk, dv = w_in0[:, 416:928], w_in0[:, 928:1440], w_in0[:, 1440:1952]
    krp = np.zeros((D, 2, 96), np.float32)
    krp[:, 0, 64:96] = kr
    krp[:, 1, 64:96] = _swap_halves(kr, 32)
    dqw = np.stack([dq, _swap_halves(dq, 64)], axis=1)
    dkw = np.stack([dk, _swap_halves(dk, 64)], axis=1)
    uq = f(w_uq)[0].reshape(256, 8, 96)
    uqp = np.zeros((256, 8, 96), np.float32)
    uqp[:, :, 64:96] = _swap_halves(np.ascontiguousarray(uq[:, :, 64:96]).reshape(256, 256), 32).reshape(256, 8, 32)
    uqw = np.ascontiguousarray(np.stack([uq, uqp], axis=1))
    ukv = f(w_ukv)[0].reshape(128, 8, 128)
    wkn = np.ascontiguousarray(ukv[:, :, 0:64]).reshape(128, 512)
    wv = np.ascontiguousarray(ukv[:, :, 64:128]).reshape(128, 512)
    shared = {
        "w_mod": f(w_mod)[0], "b_mod": f(b_mod), "g_attn": f(g_attn).reshape(8, 128), "g_ffn": f(g_ffn), "g_final": f(g_final).reshape(1, D),
        "w_cq": f(cq), "w_ckv": f(ckv), "w_krp": krp, "w_dq": f(dqw), "w_dk": f(dkw), "w_dv": f(dv),
        "g_q": f(g_q_lat).reshape(2, 128), "w_uq": uqw, "g_kv": f(g_kv_lat).reshape(128, 1), "w_kn": wkn, "w_v": wv,
        "lam4": np.concatenate([f(lam_q1), f(lam_k1), f(lam_q2), f(lam_k2)], axis=0), "g_sub": f(g_subln),
        "w_out": f(w_out)[0], "w_router": f(w_router)[0], "r_bias": f(router_bias),
        "w1": f(w1)[0], "w3": f(w3)[0], "w2": f(w2)[0], "ws1": f(ws1)[0], "ws3": f(ws3)[0], "ws2": f(ws2)[0],
    }
    in_maps = []
    for core in range(NCORES):
        b, qh = core // 2, core % 2
        own = slice(qh * 2048, (qh + 1) * 2048)
        oth = slice((1 - qh) * 2048, (2 - qh) * 2048)
        xkc = np.concatenate([ctx[b], x[b, oth], x[b, own]], axis=0)
        pos = np.concatenate([-np.ones(256, np.int64), np.arange(oth.start, oth.stop), np.arange(own.start, own.stop)])
        tabm, tabd = _rope_tables(pos)
        m = dict(shared)
        m.update({"xk": np.ascontiguousarray(xkc), "cv": np.ascontiguousarray(np.stack([c[b], c_ctx], axis=0)), "tabm": tabm, "tabd": tabd})
        in_maps.append(m)
    if _PROGRAM is None:
        _PROGRAM = build_program()
    res = run_bass_kernel_spmd(_PROGRAM, in_maps, core_ids=list(range(NCORES)))
    outp = np.zeros((4, 4096, D), np.float32)
    for core in range(NCORES):
        b, qh = core // 2, core % 2
        outp[b, qh * 2048:(qh + 1) * 2048] = np.asarray(res.results[core]["out"], dtype=np.float32)
    return outp
```

```python
import math
from contextlib import ExitStack
import numpy as np
import concourse.bass as bass
import concourse.mybir as mybir
from concourse.bass_utils import run_bass_kernel_spmd

F32 = mybir.dt.float32
BF16 = mybir.dt.bfloat16
I32 = mybir.dt.int32
AF = mybir.ActivationFunctionType
ALU = mybir.AluOpType

NCORES = 8
D = 1024
NKT = 34
NQT = 16
QT0 = 18
EPS = 1e-6
NEXP = 256
CAP = 128


class Res:
    __slots__ = ("name", "w", "r", "dsem", "dcount")

    def __init__(self, name=""):
        self.name = name
        self.w = None
        self.r = {}
        self.dsem = None
        self.dcount = 0


class _Rec:
    def __init__(self):
        self.call = None

    def __getattr__(self, name):
        def f(*a, **k):
            self.call = (name, a, k)
            return self
        return f


def _eager(fn):
    rec = _Rec()
    fn(rec)
    name, a, k = rec.call
    return lambda e: getattr(e, name)(*a, **k)


class EngQ:
    def __init__(self, fw, name):
        self.name = name
        self.sem = fw.new_sem("q_" + name)
        self.count = 0
        self.waited = {}
        self.thunks = []


class FW:
    ENGS = ["sync", "scalar", "vector", "gpsimd", "tensor"]

    def __init__(self, nc, es):
        self.nc = nc
        self.es = es
        self.nsem = 0
        self.sem_pool = []
        self.q = {n: EngQ(self, n) for n in self.ENGS}
        self.same_engine_wait = {"scalar": True, "vector": True, "gpsimd": True,
                                 "tensor": False, "sync": False}

    def new_sem(self, name):
        self.nsem += 1
        return self.es.enter_context(self.nc.semaphore(f"{name}_{self.nsem}"))

    def _deps(self, reads, writes, nowaw):
        deps = []
        for r in reads:
            if r.w is not None:
                deps.append(r.w)
        for w in writes:
            if w.w is not None and not nowaw:
                deps.append(w.w)
            deps.extend(w.r.values())
        return deps

    def _waits(self, q, deps):
        waits = []
        for (sem, val, dq) in deps:
            if dq is q and not self.same_engine_wait[q.name]:
                continue
            k = id(sem)
            if q.waited.get(k, 0) >= val:
                continue
            q.waited[k] = val
            waits.append((sem, val))
        return waits

    def _commit(self, tok, reads, writes):
        for w in writes:
            w.w = tok
            w.r = {}
        for r in reads:
            k = id(tok[0])
            old = r.r.get(k)
            if old is None or old[1] < tok[1]:
                r.r[k] = tok

    def op(self, eng, fn, reads=(), writes=()):
        fn = _eager(fn)
        q = self.q[eng]
        waits = self._waits(q, self._deps(reads, writes, False))
        q.count += 1
        sem = q.sem

        def thunk(e):
            for (s, v) in waits:
                e.wait_ge(s, v)
            fn(e).then_inc(sem, 1)
        q.thunks.append(thunk)
        self._commit((sem, q.count, q), reads, writes)

    def dma(self, eng, fn, reads=(), writes=(), nowaw=False, eager=True):
        if eager:
            fn = _eager(fn)
        q = self.q[eng]
        waits = self._waits(q, self._deps(reads, writes, nowaw))
        dst = writes[0]
        if dst.dsem is None:
            if self.sem_pool:
                dst.dsem, dst.dcount = self.sem_pool.pop()
            else:
                dst.dsem = self.new_sem("d")
        dst.dcount += 16
        sem, val = dst.dsem, dst.dcount

        def thunk(e):
            for (s, v) in waits:
                e.wait_ge(s, v)
            fn(e).then_inc(sem, 16)
        q.thunks.append(thunk)
        self._commit((sem, val, None), reads, writes)

    def final_wait(self, eng, ress):
        q = self.q[eng]
        deps = []
        for r in ress:
            if r.w is not None:
                deps.append(r.w)
            deps.extend(r.r.values())
        waits = self._waits(q, deps)

        def thunk(e):
            for (s, v) in waits:
                e.wait_ge(s, v)
        q.thunks.append(thunk)

    def finish(self):
        with self.nc.Block() as block:
            for name in self.ENGS:
                q = self.q[name]
                if not q.thunks:
                    continue

                def body(e, q=q):
                    for t in q.thunks:
                        t(e)
                getattr(block, name)(body)


class Buf:
    def __init__(self, t, name):
        self.t = t
        self.R = Res(name)

    def __getitem__(self, k):
        return self.t[k]


def build_program(stage="full"):
    nc = bass.Bass("TRN2", target_bir_lowering=False)

    def din(name, shape, dt=F32):
        return nc.dram_tensor(name, list(shape), dt, kind="ExternalInput").ap()

    xk = din("xk", [NKT * 128, D])
    cv = din("cv", [2, D])
    tabm = din("tabm", [2, 128, NKT * 128])
    tabd = din("tabd", [2, 128, NKT * 128])
    w_mod = din("w_mod", [D, 6 * D])
    b_mod = din("b_mod", [1, 6 * D])
    g_attn = din("g_attn", [8, 128])
    g_ffn = din("g_ffn", [1, D])
    g_final = din("g_final", [1, D])
    w_cq = din("w_cq", [D, 256])
    w_ckv = din("w_ckv", [D, 128])
    w_krp = din("w_krp", [D, 2, 96])
    w_dq = din("w_dq", [D, 2, 512])
    w_dk = din("w_dk", [D, 2, 512])
    w_dv = din("w_dv", [D, 512])
    g_q = din("g_q", [2, 128])
    w_uq = din("w_uq", [256, 2, 8, 96])
    g_kv = din("g_kv", [128, 1])
    w_kn = din("w_kn", [128, 512])
    w_v = din("w_v", [128, 512])
    lam4 = din("lam4", [4, 64])
    g_sub = din("g_sub", [1, 128])
    w_out = din("w_out", [D, D])
    w_router = din("w_router", [D, 256])
    r_bias = din("r_bias", [1, 256])
    if stage in ("full", "dbg"):
        w1 = din("w1", [NEXP, D, 256])
        w3 = din("w3", [NEXP, D, 256])
        w2 = din("w2", [NEXP, 256, D])
    ws1 = din("ws1", [D, 256])
    ws3 = din("ws3", [D, 256])
    ws2 = din("ws2", [256, D])
    out = nc.dram_tensor("out", [NQT * 128, D], F32, kind="ExternalOutput").ap()
    xg = nc.dram_tensor("xg", [384 * 128, D], BF16, kind="Internal").ap()
    yg = nc.dram_tensor("yg", [384 * 128, D], BF16, kind="Internal").ap()
    h2_d = nc.dram_tensor("h2_d", [NQT * 128, D], F32, kind="Internal").ap()
    ao_d = nc.dram_tensor("ao_d", [NQT * 128, D], BF16, kind="Internal" if stage == "full" else "ExternalOutput").ap()

    with ExitStack() as es0:
        fw = FW(nc, es0)

        uniq = [0]

        def sb(es, name, shape, dt=F32):
            uniq[0] += 1
            name = f"{name}_{uniq[0]}"
            return Buf(es.enter_context(nc.sbuf_tensor(name, list(shape), dt)), name)

        def ring(es, name, n, shape, dt=F32):
            return [sb(es, f"{name}{i}", shape, dt) for i in range(n)]

        PB = [Buf(es0.enter_context(nc.psum_tensor(f"pb{i}", [128, 512], F32)), f"pb{i}") for i in range(8)]

        def pbf(i):
            return PB[i].t[:].bitcast(BF16)

        V = lambda fn, R=(), W=(): fw.op("vector", fn, [b.R for b in R], [b.R for b in W])
        A = lambda fn, R=(), W=(): fw.op("scalar", fn, [b.R for b in R], [b.R for b in W])
        G = lambda fn, R=(), W=(): fw.op("gpsimd", fn, [b.R for b in R], [b.R for b in W])
        T = lambda fn, R=(), W=(): fw.op("tensor", fn, [b.R for b in R], [b.R for b in W])

        def DM(eng, fn, R=(), W=(), nowaw=False, eager=True):
            fw.dma(eng, fn, [b.R if isinstance(b, Buf) else b for b in R],
                   [b.R if isinstance(b, Buf) else b for b in W], nowaw=nowaw, eager=eager)

        def scope_end(bufs):
            rs = [b.R if isinstance(b, Buf) else b for b in bufs]
            for en in FW.ENGS:
                fw.final_wait(en, rs)
            for r in rs:
                if r.dsem is not None:
                    fw.sem_pool.append((r.dsem, r.dcount))
                    r.dsem = None

        dumps = {}
        R_dump = Res("dumps")

        def dump(name, ap, shape, dt, R):
            if stage != "dbg" or name in dumps:
                return
            d = nc.dram_tensor("dmp_" + name, list(shape), dt, kind="ExternalOutput").ap()
            dumps[name] = R_dump
            DM("sync", lambda e: e.dma_start(out=d, in_=ap), R, [R_dump], nowaw=True)

        ident_f = sb(es0, "ident_f", [128, 128], F32)
        ident_b = sb(es0, "ident_b", [128, 128], BF16)
        ones_f = sb(es0, "ones_f", [128, 128], F32)
        ones_b = sb(es0, "ones_b", [128, 128], BF16)
        tri_b = sb(es0, "tri_b", [128, 128], BF16)
        mhalf = sb(es0, "mhalf", [128, 8], F32)
        sel2 = sb(es0, "sel2", [2, 2, 128], F32)
        iota_e = sb(es0, "iota_e", [128, 256], F32)
        with ExitStack() as es:
            it = sb(es, "it_i", [128, 128], I32)
            itf = sb(es, "it_f", [128, 128], F32)
            it2 = sb(es, "it2_i", [128, 256], I32)
            G(lambda e: e.iota(it[:], pattern=[[1, 128]], base=0, channel_multiplier=-1), [], [it])
            V(lambda e: e.tensor_copy(out=itf[:], in_=it[:]), [it], [itf])
            V(lambda e: e.tensor_single_scalar(out=ident_f[:], in_=itf[:], scalar=0.0, op=ALU.is_equal), [itf], [ident_f])
            V(lambda e: e.tensor_single_scalar(out=ident_b[:], in_=itf[:], scalar=0.0, op=ALU.is_equal), [itf], [ident_b])
            V(lambda e: e.tensor_single_scalar(out=tri_b[:], in_=itf[:], scalar=0.0, op=ALU.is_gt), [itf], [tri_b])
            G(lambda e: e.iota(it2[:], pattern=[[128, 256]], base=1, channel_multiplier=0), [], [it2])
            V(lambda e: e.tensor_copy(out=iota_e[:], in_=it2[:]), [it2], [iota_e])
            G(lambda e: e.memset(ones_f[:], 1.0), [], [ones_f])
            G(lambda e: e.memset(ones_b[:], 1.0), [], [ones_b])
            G(lambda e: e.memset(mhalf[:], -0.5), [], [mhalf])
            G(lambda e: e.memset(sel2[:, 0, :], 0.0), [], [sel2])
            G(lambda e: e.memset(sel2[0:1, 0, :], 1.0), [], [sel2])
            G(lambda e: e.memset(sel2[:, 1, :], 1.0), [], [sel2])
            G(lambda e: e.memset(sel2[0:1, 1, :], 0.0), [], [sel2])
            scope_end([it, itf, it2])

        modrow_d = nc.dram_tensor("modrow_d", [2, 6 * D], F32, kind="Internal").ap()
        R_modd = Res("modrow_d")
        G1T = sb(es0, "G1T", [128, 2, 8], F32)
        S1T = sb(es0, "S1T", [128, 2, 8], F32)
        with ExitStack() as es:
            svT = sb(es, "svT", [128, 2, 8], F32)
            modrow = sb(es, "modrow", [2, 6 * D], F32)
            bm = sb(es, "bm", [2, 6 * D], F32)
            gaT0 = sb(es, "gaT0", [8, 128], F32)
            gaT = sb(es, "gaT", [128, 8], F32)
            wblk = ring(es, "wblk", 2, [128, 8, 1024], F32)
            DM("sync", lambda e: e.dma_start(out=svT[:], in_=cv.rearrange("r (p k) -> p r k", k=8)), [], [svT])
            DM("sync", lambda e: e.dma_start(out=bm[0:1, :], in_=b_mod), [], [bm])
            DM("sync", lambda e: e.dma_start(out=bm[1:2, :], in_=b_mod), [], [bm])
            DM("sync", lambda e: e.dma_start(out=gaT0[:], in_=g_attn), [], [gaT0])
            A(lambda e: e.activation(out=svT[:], in_=svT[:], func=AF.Silu), [svT], [svT])
            wm = w_mod.rearrange("(p k) n -> p k n", k=8)
            for nb in range(6):
                wb_ = wblk[nb % 2]
                DM("sync", lambda e, wb_=wb_, nb=nb: e.dma_start(out=wb_[:], in_=wm[:, :, nb * 1024:(nb + 1) * 1024]), [], [wb_])
                for hf in range(2):
                    pb = PB[hf]
                    for k in range(8):
                        T(lambda e, k=k, pb=pb, wb_=wb_, hf=hf: e.matmul(pb[0:2, :], lhsT=svT[:, :, k], rhs=wb_[:, k, hf * 512:(hf + 1) * 512],
                                                                         start=(k == 0), stop=(k == 7)), [svT, wb_], [pb])
                    c0 = nb * 1024 + hf * 512
                    V(lambda e, pb=pb, c0=c0: e.tensor_tensor(out=modrow[:, c0:c0 + 512], in0=pb[0:2, :], in1=bm[:, c0:c0 + 512], op=ALU.add),
                      [pb, bm], [modrow])
            pt = PB[2]
            T(lambda e: e.transpose(out=pt[:, 0:8], in_=gaT0[:], identity=ident_f[0:8, 0:8]), [gaT0, ident_f], [pt])
            V(lambda e: e.tensor_copy(out=gaT[:], in_=pt[:, 0:8]), [pt], [gaT])
            pt3 = PB[3]
            for j in range(16):
                T(lambda e, j=j: e.transpose(out=pt3[:, j * 2:j * 2 + 2], in_=modrow[:, j * 128:(j + 1) * 128], identity=ident_f[0:2, 0:2]),
                  [modrow, ident_f], [pt3])
            ptv = pt3.t[:, 0:32].rearrange("p (j r) -> p r j", r=2)
            V(lambda e: e.tensor_copy(out=S1T[:], in_=ptv[:, :, 0:8]), [pt3], [S1T])
            for r in range(2):
                V(lambda e, r=r: e.scalar_tensor_tensor(out=G1T[:, r, :], in0=ptv[:, r, 8:16], scalar=1.0, in1=gaT[:],
                                                        op0=ALU.add, op1=ALU.mult), [pt3, gaT], [G1T])
            DM("sync", lambda e: e.dma_start(out=modrow_d, in_=modrow[:]), [modrow], [R_modd])
            dump("modrow", modrow[:], [2, 6 * D], F32, [modrow])
            dump("G1T", G1T[:], [128, 2, 8], F32, [G1T])
            dump("S1T", S1T[:], [128, 2, 8], F32, [S1T])
            scope_end([svT, bm, gaT0, gaT, modrow] + wblk)

        neglam = sb(es0, "neglam", [128, 1], F32)
        gsub08 = sb(es0, "gsub08", [128, 128], F32)
        with ExitStack() as es:
            lt = sb(es, "lt", [128, 4, 64], F32)
            lj = sb(es, "lj", [128, 64], F32)
            ld = sb(es, "ld", [128, 2], F32)
            DM("sync", lambda e: e.dma_start(out=lt[:], in_=lam4.rearrange("a b -> (a b)").partition_broadcast(128).rearrange("p (a b) -> p a b", a=4)), [], [lt])
            DM("sync", lambda e: e.dma_start(out=gsub08[:], in_=g_sub.rearrange("a b -> (a b)").partition_broadcast(128)), [], [gsub08])
            for i in range(2):
                V(lambda e: e.tensor_tensor(out=lj[:], in0=lt[:, 2 * i, :], in1=lt[:, 2 * i + 1, :], op=ALU.mult), [lt], [lj])
                V(lambda e: e.tensor_reduce(out=ld[:, i:i + 1], in_=lj[:], axis=mybir.AxisListType.X, op=ALU.add), [lj], [ld])
            A(lambda e: e.activation(out=ld[:], in_=ld[:], func=AF.Exp), [ld], [ld])
            V(lambda e: e.scalar_tensor_tensor(out=neglam[:], in0=ld[:, 1:2], scalar=-0.2, in1=ld[:, 0:1], op0=ALU.add, op1=ALU.subtract),
              [ld], [neglam])
            V(lambda e: e.tensor_scalar(out=gsub08[:], in0=gsub08[:], scalar1=0.8, scalar2=None, op0=ALU.mult), [gsub08], [gsub08])
            dump("neglam", neglam[:], [128, 1], F32, [neglam])
            dump("gsub08", gsub08[:], [128, 128], F32, [gsub08])
            scope_end([lt, lj, ld])

        R_ao = Res("ao_d")

        def attention_pass(kind):
            mla = kind == "mla"
            with ExitStack() as es:
                NH = 8 if mla else 4
                DV = 65 if mla else 129
                kT = sb(es, "kT", [128, NH, NKT * 128], BF16)
                Vaug = sb(es, "Vaug", [128, NKT, NH, DV], BF16)
                qT = sb(es, "qT", [128, NH, 512], BF16)
                xts = ring(es, "xt", 4, [128, D], F32)
                xsr = ring(es, "xs", 2, [128, D], BF16)
                junk = sb(es, "junk", [128, D], BF16)
                st = ring(es, "st", 4, [128, 4], F32)
                hTr = ring(es, "hT", 2, [128, 8, 512], BF16)
                tab = sb(es, "tab", [128, 2, 512], F32)
                tmpA = sb(es, "tmpA", [128, 512], F32)
                tmpB = sb(es, "tmpB", [128, 512], F32)
                PT = ring(es, "PT", 4, [128, 512], BF16)
                aos = sb(es, "aos", [128, 4, 512], BF16)
                rec = sb(es, "rec", [128, 4], F32)
                tab_d = tabm if mla else tabd
                G(lambda e: e.memset(Vaug[:, :, :, DV - 1:DV], 1.0), [], [Vaug])
                kp = lambda ap: ap.rearrange("(k p) n -> p k n", p=128)
                if mla:
                    wckv = sb(es, "wckv", [128, 8, 128], BF16)
                    wkrp = sb(es, "wkrp", [128, 8, 2, 96], BF16)
                    wcq = sb(es, "wcq", [128, 8, 256], BF16)
                    wuq = sb(es, "wuq", [128, 2, 2, 8, 96], BF16)
                    gq = sb(es, "gq", [128, 2], F32)
                    gq0 = sb(es, "gq0", [2, 128], F32)
                    wkn = sb(es, "wkn", [128, 512], BF16)
                    wv = sb(es, "wv", [128, 512], BF16)
                    gkv = sb(es, "gkv", [128, 1], F32)
                    sq = sb(es, "sq", [128, 2, 512], F32)
                    rb = sb(es, "rb", [128, 512], F32)
                    cn = sb(es, "cn", [128, 2, 512], BF16)
                    DM("gpsimd", lambda e: e.dma_start(out=wckv[:], in_=kp(w_ckv)), [], [wckv])
                    DM("gpsimd", lambda e: e.dma_start(out=wkrp[:], in_=w_krp.rearrange("(k p) a n -> p k a n", p=128)), [], [wkrp])
                    DM("gpsimd", lambda e: e.dma_start(out=wcq[:], in_=kp(w_cq)), [], [wcq])
                    DM("gpsimd", lambda e: e.dma_start(out=wuq[:].rearrange("p k a h n -> p k (a h n)"), in_=w_uq.rearrange("(k p) a h n -> p k (a h n)", p=128)), [], [wuq])
                    DM("gpsimd", lambda e: e.dma_start(out=wkn[:], in_=w_kn), [], [wkn])
                    DM("gpsimd", lambda e: e.dma_start(out=wv[:], in_=w_v), [], [wv])
                    DM("sync", lambda e: e.dma_start(out=gq0[:], in_=g_q), [], [gq0])
                    DM("sync", lambda e: e.dma_start(out=gkv[:], in_=g_kv), [], [gkv])
                    T(lambda e: e.transpose(out=PB[2][:, 0:2], in_=gq0[:], identity=ident_f[0:2, 0:2]), [gq0, ident_f], [PB[2]])
                    V(lambda e: e.tensor_copy(out=gq[:], in_=PB[2][:, 0:2]), [PB[2]], [gq])
                else:
                    wdk = sb(es, "wdk", [128, 8, 2, 512], BF16)
                    wdq = sb(es, "wdq", [128, 8, 2, 512], BF16)
                    wdv = sb(es, "wdv", [128, 8, 512], BF16)
                    t0 = sb(es, "t0", [128, 4, 128], F32)
                    t1 = sb(es, "t1", [128, 4, 128], F32)
                    ssq = sb(es, "ssq", [128, 4], F32)
                    DM("gpsimd", lambda e: e.dma_start(out=wdk[:], in_=w_dk.rearrange("(k p) a n -> p k a n", p=128)), [], [wdk])
                    DM("gpsimd", lambda e: e.dma_start(out=wdq[:], in_=w_dq.rearrange("(k p) a n -> p k a n", p=128)), [], [wdq])
                    DM("gpsimd", lambda e: e.dma_start(out=wdv[:], in_=kp(w_dv)), [], [wdv])

                cnt = {"x": 0, "s": 0, "g": 0}

                def make_hT(tiles):
                    hT = hTr[cnt["g"] % 2]
                    cnt["g"] += 1
                    cur = []
                    for ti, t in enumerate(tiles):
                        xt = xts[cnt["x"] % 4]
                        s_ = st[cnt["x"] % 4]
                        cnt["x"] += 1
                        cur.append((xt, s_))
                        DM("sync", lambda e: e.dma_start(out=xt[:], in_=xk[t * 128:(t + 1) * 128, :]), [], [xt])
                        A(lambda e: e.activation(out=junk[:], in_=xt[:], func=AF.Square, accum_out=s_[:, 0:1]), [xt], [junk, s_])
                        V(lambda e: e.tensor_scalar(out=s_[:, 1:2], in0=s_[:, 0:1], scalar1=1.0 / D, scalar2=EPS, op0=ALU.mult, op1=ALU.add), [s_], [s_])
                        G(lambda e: e.tensor_tensor(out=s_[:, 2:3], in0=s_[:, 1:2], in1=mhalf[:, 0:1], op=ALU.pow), [s_, mhalf], [s_])
                    for ti, t in enumerate(tiles):
                        xt, s_ = cur[ti]
                        r = 1 if t < 2 else 0
                        x_ = xsr[cnt["s"] % 2]
                        pbi = 2 + (cnt["s"] % 2)
                        cnt["s"] += 1
                        V(lambda e: e.tensor_scalar(out=x_[:], in0=xt[:], scalar1=s_[:, 2:3], scalar2=None, op0=ALU.mult), [xt, s_], [x_])
                        dump(kind + "_st0", s_[:], [128, 4], F32, [s_])
                        for k in range(8):
                            T(lambda e: e.transpose(out=pbf(pbi)[:, k * 128:(k + 1) * 128], in_=x_[:, k * 128:(k + 1) * 128], identity=ident_b[:]),
                              [x_, ident_b], [PB[pbi]])
                        for k in range(8):
                            A(lambda e: e.activation(out=hT[:, k, ti * 128:(ti + 1) * 128], in_=pbf(pbi)[:, k * 128:(k + 1) * 128],
                                                     func=AF.Identity, scale=G1T[:, r, k:k + 1], bias=S1T[:, r, k:k + 1]),
                              [PB[pbi], G1T, S1T], [hT])
                    return hT

                def load_tab(c0, n):
                    DM("sync", lambda e: e.dma_start(out=tab[:, :, 0:n], in_=tab_d[:, :, c0:c0 + n].rearrange("a p n -> p a n")), [], [tab])

                def rope_evac(pa, pb_, rows, n, dst_fn):
                    V(lambda e: e.tensor_tensor(out=tmpA[0:rows, 0:n], in0=pa[0:rows, 0:n], in1=tab[0:rows, 0, 0:n], op=ALU.mult), [pa, tab], [tmpA])
                    V(lambda e: e.tensor_tensor(out=tmpB[0:rows, 0:n], in0=pb_[0:rows, 0:n], in1=tab[0:rows, 1, 0:n], op=ALU.mult), [pb_, tab], [tmpB])
                    dst_fn()

                def rms_T(psrc_list, nchunk, n, width, dst, gain):
                    for c in range(nchunk):
                        A(lambda e, c=c: e.activation(out=sq[:, c, 0:n], in_=psrc_list[c][:, 0:n], func=AF.Square), [psrc_list[c]], [sq])
                    for c in range(nchunk):
                        T(lambda e, c=c: e.matmul(PB[3][:, 0:n], lhsT=ones_f[:], rhs=sq[:, c, 0:n], start=(c == 0), stop=(c == nchunk - 1)),
                          [ones_f, sq], [PB[3]])
                    A(lambda e: e.activation(out=rb[:, 0:n], in_=PB[3][:, 0:n], func=AF.Ln, scale=1.0 / width, bias=EPS), [PB[3]], [rb])
                    A(lambda e: e.activation(out=rb[:, 0:n], in_=rb[:, 0:n], func=AF.Exp, scale=-0.5), [rb], [rb])
                    for c in range(nchunk):
                        V(lambda e, c=c: e.scalar_tensor_tensor(out=dst[:, c, 0:n], in0=psrc_list[c][:, 0:n], scalar=gain[:, c:c + 1], in1=rb[:, 0:n],
                                                                op0=ALU.mult, op1=ALU.mult), [psrc_list[c], rb, gain], [dst])

                groups = [[0, 1]] + [list(range(2 + 4 * g, 6 + 4 * g)) for g in range(8)]
                for tiles in groups:
                    n = len(tiles) * 128
                    c0 = tiles[0] * 128
                    hT = make_hT(tiles)
                    load_tab(c0, n)
                    if mla:
                        for k in range(8):
                            T(lambda e, k=k: e.matmul(PB[0][:, 0:n], lhsT=wckv[:, k, :], rhs=hT[:, k, 0:n], start=(k == 0), stop=(k == 7)), [wckv, hT], [PB[0]])
                        rms_T([PB[0]], 1, n, 128.0, cn, gkv)
                        for a in range(2):
                            for k in range(8):
                                T(lambda e, k=k, a=a: e.matmul(PB[a][0:96, 0:n], lhsT=wkrp[:, k, a, :], rhs=hT[:, k, 0:n], start=(k == 0), stop=(k == 7)),
                                  [wkrp, hT], [PB[a]])

                        def fin():
                            for h in range(8):
                                V(lambda e, h=h: e.tensor_tensor(out=kT[64:96, h, c0:c0 + n], in0=tmpA[64:96, 0:n], in1=tmpB[64:96, 0:n], op=ALU.add),
                                  [tmpA, tmpB], [kT])
                        V(lambda e: e.tensor_tensor(out=tmpA[64:96, 0:n], in0=PB[0][64:96, 0:n], in1=tab[64:96, 0, 0:n], op=ALU.mult), [PB[0], tab], [tmpA])
                        V(lambda e: e.tensor_tensor(out=tmpB[64:96, 0:n], in0=PB[1][64:96, 0:n], in1=tab[64:96, 1, 0:n], op=ALU.mult), [PB[1], tab], [tmpB])
                        fin()
                        for h in range(8):
                            pb = PB[h % 2]
                            T(lambda e, h=h, pb=pb: e.matmul(pb[0:64, 0:n], lhsT=wkn[:, h * 64:(h + 1) * 64], rhs=cn[:, 0, 0:n], start=True, stop=True), [wkn, cn], [pb])
                            A(lambda e, h=h, pb=pb: e.activation(out=kT[0:64, h, c0:c0 + n], in_=pb[0:64, 0:n], func=AF.Copy), [pb], [kT])
                        for ti, t in enumerate(tiles):
                            T(lambda e, ti=ti: e.matmul(PB[3][:, :], lhsT=cn[:, 0, ti * 128:(ti + 1) * 128], rhs=wv[:], start=True, stop=True), [cn, wv], [PB[3]])
                            V(lambda e, t=t: e.tensor_copy(out=Vaug[:, t, :, 0:64], in_=PB[3].t[:, :].rearrange("p (h d) -> p h d", h=8)), [PB[3]], [Vaug])
                    else:
                        for h in range(4):
                            for a in range(2):
                                for k in range(8):
                                    T(lambda e, k=k, a=a, h=h: e.matmul(PB[a][:, 0:n], lhsT=wdk[:, k, a, h * 128:(h + 1) * 128], rhs=hT[:, k, 0:n],
                                                                        start=(k == 0), stop=(k == 7)), [wdk, hT], [PB[a]])
                            rope_evac(PB[0], PB[1], 128, n, lambda h=h: V(
                                lambda e: e.tensor_tensor(out=kT[:, h, c0:c0 + n], in0=tmpA[:, 0:n], in1=tmpB[:, 0:n], op=ALU.add), [tmpA, tmpB], [kT]))
                        for ti, t in enumerate(tiles):
                            for k in range(8):
                                T(lambda e, k=k, ti=ti: e.matmul(PB[3][:, :], lhsT=hT[:, k, ti * 128:(ti + 1) * 128], rhs=wdv[:, k, :], start=(k == 0), stop=(k == 7)),
                                  [hT, wdv], [PB[3]])
                            V(lambda e, t=t: e.tensor_copy(out=Vaug[:, t, :, 0:128], in_=PB[3].t[:, :].rearrange("p (h d) -> p h d", h=4)), [PB[3]], [Vaug])

                    dump(kind + "_hT0", hT[:, :, 0:256], [128, 8, 256], BF16, [hT])
                    dump(kind + "_tab0", tab[:], [128, 2, 512], F32, [tab])
                    dump(kind + "_kT0", kT[:, 0, 0:256], [128, 256], BF16, [kT])
                    dump(kind + "_V0", Vaug[:, 0, :, :], [128, NH, DV], BF16, [Vaug])
                    if mla:
                        dump("cn0", cn[:, 0, 0:256], [128, 256], BF16, [cn])
                        dump("rb0", rb[:, 0:256], [128, 256], F32, [rb])

                scale = 1.0 / math.sqrt(96.0) if mla else 1.0 / 8.0
                pt_i = 0
                s_i = 0
                o_i = 0
                for qc in range(4):
                    tiles = list(range(QT0 + 4 * qc, QT0 + 4 * qc + 4))
                    c0 = tiles[0] * 128
                    hT = make_hT(tiles)
                    load_tab(c0, 512)
                    if mla:
                        for c in range(2):
                            for k in range(8):
                                T(lambda e, k=k, c=c: e.matmul(PB[c][:, :], lhsT=wcq[:, k, c * 128:(c + 1) * 128], rhs=hT[:, k, :], start=(k == 0), stop=(k == 7)),
                                  [wcq, hT], [PB[c]])
                        rms_T([PB[0], PB[1]], 2, 512, 256.0, cn, gq)
                        for h in range(8):
                            for a in range(2):
                                for k in range(2):
                                    T(lambda e, k=k, a=a, h=h: e.matmul(PB[a][0:96, :], lhsT=wuq[:, k, a, h, :], rhs=cn[:, k, :], start=(k == 0), stop=(k == 1)),
                                      [wuq, cn], [PB[a]])
                            rope_evac(PB[0], PB[1], 96, 512, lambda h=h: V(
                                lambda e: e.tensor_tensor(out=qT[0:96, h, :], in0=tmpA[0:96, :], in1=tmpB[0:96, :], op=ALU.add), [tmpA, tmpB], [qT]))
                    else:
                        for h in range(4):
                            for a in range(2):
                                for k in range(8):
                                    T(lambda e, k=k, a=a, h=h: e.matmul(PB[a][:, :], lhsT=wdq[:, k, a, h * 128:(h + 1) * 128], rhs=hT[:, k, :],
                                                                        start=(k == 0), stop=(k == 7)), [wdq, hT], [PB[a]])
                            rope_evac(PB[0], PB[1], 128, 512, lambda h=h: V(
                                lambda e: e.tensor_tensor(out=qT[:, h, :], in0=tmpA[:, :], in1=tmpB[:, :], op=ALU.add), [tmpA, tmpB], [qT]))

                    dump(kind + "_qT0", qT[:, 0, :], [128, 512], BF16, [qT])
                    if mla:
                        dump("cnq", cn[:], [128, 2, 512], BF16, [cn])
                    LOOK = 2
                    SB = [PB[3], PB[4], PB[5]]
                    steps = [(h, j, kt) for h in range(NH) for j in range(1 if mla else 2) for kt in range(NKT)]
                    inflight = {}

                    def emit_S(si):
                        h, j, kt = steps[si]
                        KR = slice(0, 96) if mla else slice(j * 64, (j + 1) * 64)
                        ps = SB[si % 3]
                        p_ = PT[si % 4]
                        T(lambda e: e.matmul(ps[:, :], lhsT=kT[KR, h, kt * 128:(kt + 1) * 128], rhs=qT[KR, h, :], start=True, stop=True), [kT, qT], [ps])
                        A(lambda e: e.activation(out=p_[:], in_=ps[:, :], func=AF.Exp, scale=scale), [ps], [p_])
                        dump(kind + "_PT0", p_[:], [128, 512], BF16, [p_])
                        inflight[si] = p_

                    for si in range(min(LOOK, len(steps))):
                        emit_S(si)
                    for si, (h, j, kt) in enumerate(steps):
                        if si + LOOK < len(steps):
                            emit_S(si + LOOK)
                        p_ = inflight.pop(si)
                        if kt == 0:
                            if mla:
                                ob = [PB[6 + (o_i % 2)]]
                            else:
                                ob = [PB[6], PB[7]] if (o_i % 2 == 0) else [PB[0], PB[1]]
                            o_i += 1
                        for qs in range(4):
                            if mla:
                                o_, col = ob[0], qs * 65
                            else:
                                o_, col = ob[qs // 2], (qs % 2) * 129
                            first = (kt == 0) and (col == 0)
                            T(lambda e: e.matmul(o_[:, col:col + DV], lhsT=p_[:, qs * 128:(qs + 1) * 128], rhs=Vaug[:, kt, h, :],
                                                 start=first, stop=(kt == NKT - 1), skip_group_check=True), [p_, Vaug], [o_])
                        if kt != NKT - 1:
                            continue
                        for qs in range(4):
                            if mla:
                                o_, col = ob[0], qs * 65
                            else:
                                o_, col = ob[qs // 2], (qs % 2) * 129
                            V(lambda e: e.reciprocal(out=rec[:, qs:qs + 1], in_=o_[:, col + DV - 1:col + DV]), [o_], [rec])
                            if mla:
                                V(lambda e: e.tensor_scalar(out=aos[:, qs, h * 64:(h + 1) * 64], in0=o_[:, col:col + 64],
                                                            scalar1=rec[:, qs:qs + 1], scalar2=None, op0=ALU.mult), [o_, rec], [aos])
                            else:
                                tj = t0 if j == 0 else t1
                                V(lambda e: e.tensor_scalar(out=tj[:, qs, :], in0=o_[:, col:col + 128],
                                                            scalar1=rec[:, qs:qs + 1], scalar2=None, op0=ALU.mult), [o_, rec], [tj])
                        if (not mla) and j == 1:
                            V(lambda e: e.scalar_tensor_tensor(out=t0[:].rearrange("p a b -> p (a b)"), in0=t1[:].rearrange("p a b -> p (a b)"), scalar=neglam[:, 0:1],
                                                               in1=t0[:].rearrange("p a b -> p (a b)"), op0=ALU.mult, op1=ALU.add), [t0, t1, neglam], [t0])
                            V(lambda e: e.tensor_tensor(out=t1[:], in0=t0[:], in1=t0[:], op=ALU.mult), [t0], [t1])
                            V(lambda e: e.tensor_reduce(out=ssq[:], in_=t1[:], axis=mybir.AxisListType.X, op=ALU.add), [t1], [ssq])
                            V(lambda e: e.tensor_scalar(out=ssq[:], in0=ssq[:], scalar1=1.0 / 128, scalar2=EPS, op0=ALU.mult, op1=ALU.add), [ssq], [ssq])
                            G(lambda e: e.tensor_tensor(out=ssq[:], in0=ssq[:], in1=mhalf[:, 0:4], op=ALU.pow), [ssq, mhalf], [ssq])
                            for qs in range(4):
                                V(lambda e: e.scalar_tensor_tensor(out=aos[:, qs, h * 128:(h + 1) * 128], in0=t0[:, qs, :], scalar=ssq[:, qs:qs + 1],
                                                                   in1=gsub08[:], op0=ALU.mult, op1=ALU.mult), [t0, ssq, gsub08], [aos])
                    dump(kind + "_aos0", aos[:], [128, 4, 512], BF16, [aos])
                    cb = 0 if mla else 512
                    for qs in range(4):
                        r0 = (qc * 4 + qs) * 128
                        DM("sync", lambda e, qs=qs, r0=r0: e.dma_start(out=ao_d[r0:r0 + 128, cb:cb + 512], in_=aos[:, qs, :]), [aos], [R_ao], nowaw=True)
                scope_end([kT, Vaug, qT, junk, tab, tmpA, tmpB, aos, rec] + hTr + xts + xsr + st + PT)

        attention_pass("mla")
        attention_pass("diff")

        x1 = sb(es0, "x1", [128, NQT, D], F32)
        rows = sb(es0, "rows", [128, 5, D], F32)
        R_GT1, R_G2, R_SH2, R_GT2, R_GF = range(5)
        with ExitStack() as es:
            gff = sb(es, "gff", [128, D], F32)
            DM("sync", lambda e: e.dma_start(out=gff[:], in_=g_ffn.rearrange("a b -> (a b)").partition_broadcast(128)), [], [gff])
            DM("sync", lambda e: e.dma_start(out=rows[:, R_GF, :], in_=g_final.rearrange("a b -> (a b)").partition_broadcast(128)), [], [rows])
            for (dst, ch) in [(R_GT1, 2), (R_SH2, 3), (R_G2, 4), (R_GT2, 5)]:
                DM("sync", lambda e, dst=dst, ch=ch: e.dma_start(out=rows[:, dst, :], in_=modrow_d[0, ch * D:(ch + 1) * D].partition_broadcast(128)), [R_modd], [rows])
            V(lambda e: e.scalar_tensor_tensor(out=rows[:, R_G2, :], in0=rows[:, R_G2, :], scalar=1.0, in1=gff[:], op0=ALU.add, op1=ALU.mult), [rows, gff], [rows])
            wo = sb(es, "wo", [128, 8, D], BF16)
            DM("gpsimd", lambda e: e.dma_start(out=wo[:], in_=w_out.rearrange("(k p) n -> p k n", p=128)), [], [wo])
            aot = ring(es, "aot", 2, [128, D], BF16)
            aoT = ring(es, "aoT", 2, [128, 8, 128], BF16)
            xq = ring(es, "xq", 2, [128, D], F32)
            ty = ring(es, "ty", 2, [128, D], F32)
            for i in range(NQT):
                a_, aT_, x_, ty_ = aot[i % 2], aoT[i % 2], xq[i % 2], ty[i % 2]
                DM("sync", lambda e, a_=a_, i=i: e.dma_start(out=a_[:], in_=ao_d[i * 128:(i + 1) * 128, :]), [R_ao], [a_])
                DM("sync", lambda e, x_=x_, i=i: e.dma_start(out=x_[:], in_=xk[(QT0 + i) * 128:(QT0 + i + 1) * 128, :]), [], [x_])
                for k in range(8):
                    T(lambda e, k=k, a_=a_: e.transpose(out=pbf(2)[:, k * 128:(k + 1) * 128], in_=a_[:, k * 128:(k + 1) * 128], identity=ident_b[:]), [a_, ident_b], [PB[2]])
                A(lambda e, aT_=aT_: e.activation(out=aT_[:].rearrange("p k t -> p (k t)"), in_=pbf(2)[:, :], func=AF.Copy), [PB[2]], [aT_])
                for hf in range(2):
                    pb = PB[hf]
                    for k in range(8):
                        T(lambda e, k=k, pb=pb, aT_=aT_, hf=hf: e.matmul(pb[:, :], lhsT=aT_[:, k, :], rhs=wo[:, k, hf * 512:(hf + 1) * 512], start=(k == 0), stop=(k == 7)),
                          [aT_, wo], [pb])
                    V(lambda e, pb=pb, hf=hf, ty_=ty_: e.tensor_tensor(out=ty_[:, hf * 512:(hf + 1) * 512], in0=pb[:, :], in1=rows[:, R_GT1, hf * 512:(hf + 1) * 512], op=ALU.mult),
                      [pb, rows], [ty_])
                G(lambda e, i=i, ty_=ty_, x_=x_: e.tensor_tensor(out=x1[:, i, :], in0=ty_[:], in1=x_[:], op=ALU.add), [ty_, x_], [x1])
            scope_end([gff, wo] + aot + aoT + xq + ty)

        if stage == "dbg":
            dump("x1a", x1[:], [128, NQT, D], F32, [x1])
            for i in range(NQT):
                DM("sync", lambda e, i=i: e.dma_start(out=x1[:, i, :], in_=xk[(QT0 + i) * 128:(QT0 + i + 1) * 128, :]), [], [x1])
        if stage == "attn":
            R_out = Res("out")
            for i in range(NQT):
                DM("sync", lambda e, i=i: e.dma_start(out=out[i * 128:(i + 1) * 128, :], in_=x1[:, i, :]), [x1], [R_out], nowaw=True)
            scope_end([R_out, R_ao])
            fw.finish()
            return nc
        NBLK = 384
        idx_all = sb(es0, "idx_all", [128, NQT, 8], I32)
        wk_all = sb(es0, "wk_all", [128, NQT, 8], F32)
        idxw = sb(es0, "idxw", [128, NBLK], I32)
        R_xg = Res("xg")
        R_yg = Res("yg")
        R_h2d = Res("h2d")
        pk = lambda ap: ap.rearrange("(p k) n -> p k n", k=8)
        with ExitStack() as es:
            wr = sb(es, "wr", [128, 8, 256], F32)
            wsg = sb(es, "wsg", [128, 8, 512], F32)
            wsd = sb(es, "wsd", [128, 2, D], F32)
            rbias = sb(es, "rbias", [128, 256], F32)
            base = sb(es, "base", [128, 256], F32)
            cnt = sb(es, "cnt", [128, 256], F32)
            selb_all = sb(es, "selb_all", [128, NQT, 256], BF16)
            wn_all = sb(es, "wn_all", [128, NQT, 256], F32)
            DM("sync", lambda e: e.dma_start(out=wr[:], in_=pk(w_router)), [], [wr])
            DM("sync", lambda e: e.dma_start(out=wsg[:, :, 0:256], in_=pk(ws1)), [], [wsg])
            DM("sync", lambda e: e.dma_start(out=wsg[:, :, 256:512], in_=pk(ws3)), [], [wsg])
            DM("sync", lambda e: e.dma_start(out=wsd[:], in_=ws2.rearrange("(j p) n -> p j n", p=128)), [], [wsd])
            DM("sync", lambda e: e.dma_start(out=rbias[:], in_=r_bias.rearrange("a b -> (a b)").partition_broadcast(128)), [], [rbias])
            G(lambda e: e.memset(base[:], 0.0), [], [base])
            G(lambda e: e.memset(cnt[:], 0.0), [], [cnt])
            h2 = ring(es, "h2", 2, [128, D], F32)
            h2T = sb(es, "h2T", [128, 8, 128], F32)
            junk = sb(es, "junkf", [128, D], F32)
            st = ring(es, "st2", 2, [128, 4], F32)
            s_ = sb(es, "s_", [128, 256], F32)
            ssel = sb(es, "ssel", [128, 256], F32)
            m8 = sb(es, "m8", [128, 8, 8], F32)
            gs = sb(es, "gs", [128, 8], F32)
            gm = sb(es, "gm", [128, 8], F32)
            msk = sb(es, "msk", [128, 256], F32)
            self_ = sb(es, "self", [128, 256], F32)
            Dm = sb(es, "Dm", [128, 256], F32)
            d8 = sb(es, "d8", [128, 8], F32)
            wsum = sb(es, "wsum", [128, 2], F32)
            sg = sb(es, "sg", [128, 256], F32)
            hs = sb(es, "hs", [128, 256], F32)
            hsT = sb(es, "hsT", [128, 2, 128], F32)
            ysh = sb(es, "ysh", [128, D], F32)
            for i in range(NQT):
                h2_ = h2[i % 2]
                s2 = st[i % 2]
                A(lambda e: e.activation(out=junk[:], in_=x1[:, i, :], func=AF.Square, accum_out=s2[:, 0:1]), [x1], [junk, s2])
                V(lambda e: e.tensor_scalar(out=s2[:, 1:2], in0=s2[:, 0:1], scalar1=1.0 / D, scalar2=EPS, op0=ALU.mult, op1=ALU.add), [s2], [s2])
                G(lambda e: e.tensor_tensor(out=s2[:, 2:3], in0=s2[:, 1:2], in1=mhalf[:, 0:1], op=ALU.pow), [s2, mhalf], [s2])
                V(lambda e: e.scalar_tensor_tensor(out=junk[:], in0=x1[:, i, :], scalar=s2[:, 2:3], in1=rows[:, R_G2, :], op0=ALU.mult, op1=ALU.mult),
                  [x1, s2, rows], [junk])
                V(lambda e: e.tensor_tensor(out=h2_[:].rearrange("t (k p) -> t p k", k=8), in0=junk[:].rearrange("t (p k) -> t p k", k=8),
                                            in1=rows[:, R_SH2, :].rearrange("t (p k) -> t p k", k=8), op=ALU.add), [junk, rows], [h2_])
                DM("sync", lambda e: e.dma_start(out=h2_d[i * 128:(i + 1) * 128, :], in_=h2_[:]), [h2_], [R_h2d], nowaw=True)
                for hf in range(2):
                    for k in range(4):
                        kk = hf * 4 + k
                        T(lambda e: e.transpose(out=PB[hf][:, k * 128:(k + 1) * 128], in_=h2_[:, kk * 128:(kk + 1) * 128], identity=ident_f[:]),
                          [h2_, ident_f], [PB[hf]])
                    A(lambda e: e.activation(out=h2T[:, hf * 4:(hf + 1) * 4, :].rearrange("p k t -> p (k t)"), in_=PB[hf][:, :], func=AF.Copy), [PB[hf]], [h2T])
                for k in range(8):
                    T(lambda e: e.matmul(PB[2][:, 0:256], lhsT=h2T[:, k, :], rhs=wr[:, k, :], start=(k == 0), stop=(k == 7)), [h2T, wr], [PB[2]])
                A(lambda e: e.activation(out=s_[:], in_=PB[2][:, 0:256], func=AF.Sigmoid), [PB[2]], [s_])
                V(lambda e: e.tensor_tensor(out=ssel[:], in0=s_[:], in1=rbias[:], op=ALU.add), [s_, rbias], [ssel])
                for g in range(8):
                    V(lambda e: e.max(out=m8[:, g, :], in_=ssel[:, g * 32:(g + 1) * 32]), [ssel], [m8])
                V(lambda e: e.tensor_tensor(out=gs[:], in0=m8[:, :, 0], in1=m8[:, :, 1], op=ALU.add), [m8], [gs])
                V(lambda e: e.max(out=d8[:], in_=gs[:]), [gs], [d8])
                V(lambda e: e.tensor_scalar(out=gm[:], in0=gs[:], scalar1=d8[:, 3:4], scalar2=None, op0=ALU.is_ge), [gs, d8], [gm])
                for g in range(8):
                    V(lambda e: e.tensor_scalar(out=msk[:, g * 32:(g + 1) * 32], in0=ssel[:, g * 32:(g + 1) * 32], scalar1=4.0, scalar2=gm[:, g:g + 1],
                                                op0=ALU.add, op1=ALU.mult), [ssel, gm], [msk])
                V(lambda e: e.max(out=d8[:], in_=msk[:]), [msk], [d8])
                V(lambda e: e.tensor_scalar(out=self_[:], in0=msk[:], scalar1=d8[:, 7:8], scalar2=None, op0=ALU.is_ge), [msk, d8], [self_])
                V(lambda e: e.tensor_copy(out=selb_all[:, i, :], in_=self_[:]), [self_], [selb_all])
                V(lambda e: e.tensor_tensor(out=wn_all[:, i, :], in0=s_[:], in1=self_[:], op=ALU.mult), [s_, self_], [wn_all])
                V(lambda e: e.tensor_reduce(out=wsum[:, 0:1], in_=wn_all[:, i, :], axis=mybir.AxisListType.X, op=ALU.add), [wn_all], [wsum])
                V(lambda e: e.reciprocal(out=wsum[:, 1:2], in_=wsum[:, 0:1]), [wsum], [wsum])
                V(lambda e: e.tensor_scalar(out=wn_all[:, i, :], in0=wn_all[:, i, :], scalar1=wsum[:, 1:2], scalar2=2.5, op0=ALU.mult, op1=ALU.mult), [wn_all, wsum], [wn_all])
                T(lambda e: e.matmul(PB[3][:, 0:256], lhsT=ones_b[:], rhs=selb_all[:, i, :], start=True, stop=True), [ones_b, selb_all], [PB[3]])
                V(lambda e: e.tensor_tensor(out=cnt[:], in0=PB[3][:, 0:256], in1=cnt[:], op=ALU.add), [PB[3], cnt], [cnt])
                dump("h2_0", h2_[:], [128, D], F32, [h2_])
                dump("s_0", s_[:], [128, 256], F32, [s_])
                dump("self0", self_[:], [128, 256], F32, [self_])
                dump("gs0", gs[:], [128, 8], F32, [gs])
                for k in range(8):
                    T(lambda e: e.matmul(PB[4][:, :], lhsT=h2T[:, k, :], rhs=wsg[:, k, :], start=(k == 0), stop=(k == 7)), [h2T, wsg], [PB[4]])
                A(lambda e: e.activation(out=sg[:], in_=PB[4][:, 0:256], func=AF.Silu), [PB[4]], [sg])
                V(lambda e: e.tensor_tensor(out=hs[:], in0=PB[4][:, 256:512], in1=sg[:], op=ALU.mult), [PB[4], sg], [hs])
                for j in range(2):
                    T(lambda e: e.transpose(out=PB[5][:, j * 128:(j + 1) * 128], in_=hs[:, j * 128:(j + 1) * 128], identity=ident_f[:]), [hs, ident_f], [PB[5]])
                A(lambda e: e.activation(out=hsT[:].rearrange("p j t -> p (j t)"), in_=PB[5][:, 0:256], func=AF.Copy), [PB[5]], [hsT])
                for hf in range(2):
                    pb = PB[6 + hf]
                    for j in range(2):
                        T(lambda e: e.matmul(pb[:, :], lhsT=hsT[:, j, :], rhs=wsd[:, j, hf * 512:(hf + 1) * 512], start=(j == 0), stop=(j == 1)),
                          [hsT, wsd], [pb])
                    V(lambda e: e.tensor_tensor(out=ysh[:, hf * 512:(hf + 1) * 512], in0=pb[:, :], in1=rows[:, R_GT2, hf * 512:(hf + 1) * 512], op=ALU.mult),
                      [pb, rows], [ysh])
                G(lambda e: e.tensor_tensor(out=x1[:, i, :], in0=x1[:, i, :], in1=ysh[:], op=ALU.add), [x1, ysh], [x1])
            dump("x1s", x1[:], [128, NQT, D], F32, [x1])
            dump("cnt", cnt[:], [128, 256], F32, [cnt])
            ci = sb(es, "ci", [128, 256], I32)
            padf = sb(es, "padf", [128, 256], F32)
            pend = sb(es, "pend", [128, 256], F32)
            pst1 = sb(es, "pst1", [128, 256], F32)
            thr = sb(es, "thr", [128, 3], F32)
            thr_i = sb(es, "thr_i", [128, 3], I32)
            blk = sb(es, "blk", [128, 3], F32)
            dg = sb(es, "dg", [128, 128], F32)
            iop = sb(es, "iop", [128, 1], F32)
            iop_i = sb(es, "iop_i", [128, 1], I32)
            V(lambda e: e.memset(padf[:], 0.0), [], [padf])
            for m in range(16):
                V(lambda e: e.scalar_tensor_tensor(out=padf[:], in0=cnt[:], scalar=128.0 * m, in1=padf[:], op0=ALU.is_gt, op1=ALU.add), [cnt, padf], [padf])
            V(lambda e: e.tensor_scalar(out=padf[:], in0=padf[:], scalar1=128.0, scalar2=None, op0=ALU.mult), [padf], [padf])
            V(lambda e: e.memset(msk[:], 1.0), [], [msk])
            V(lambda e: e.tensor_tensor_scan(out=pend[:], data0=msk[:], data1=padf[:], initial=0.0, op0=ALU.mult, op1=ALU.add), [padf, msk], [pend])
            V(lambda e: e.tensor_tensor(out=pst1[:], in0=pend[:], in1=padf[:], op=ALU.subtract), [pend, padf], [pst1])
            V(lambda e: e.tensor_scalar(out=pst1[:], in0=pst1[:], scalar1=1.0, scalar2=None, op0=ALU.add), [pst1], [pst1])
            G(lambda e: e.iota(thr_i[:], pattern=[[128 * 128, 3]], base=0, channel_multiplier=128), [], [thr_i])
            V(lambda e: e.tensor_copy(out=thr[:], in_=thr_i[:]), [thr_i], [thr])
            G(lambda e: e.iota(iop_i[:], pattern=[[0, 1]], base=0, channel_multiplier=1), [], [iop_i])
            V(lambda e: e.tensor_copy(out=iop[:], in_=iop_i[:]), [iop_i], [iop])
            for j in range(3):
                V(lambda e: e.tensor_scalar(out=Dm[:], in0=pend[:], scalar1=thr[:, j:j + 1], scalar2=None, op0=ALU.is_le), [pend, thr], [Dm])
                V(lambda e: e.tensor_reduce(out=blk[:, j:j + 1], in_=Dm[:], axis=mybir.AxisListType.X, op=ALU.add), [Dm], [blk])
            for j in range(3):
                V(lambda e: e.tensor_scalar(out=dg[:], in0=ident_f[:], scalar1=blk[:, j:j + 1], scalar2=None, op0=ALU.mult), [ident_f, blk], [dg])
                T(lambda e: e.matmul(PB[2][:, 0:128], lhsT=ones_f[:], rhs=dg[:], start=True, stop=True), [ones_f, dg], [PB[2]])
                V(lambda e: e.tensor_scalar(out=idxw[:, j * 128:(j + 1) * 128], in0=PB[2][:, 0:128], scalar1=128.0, scalar2=iop[:, 0:1], op0=ALU.mult, op1=ALU.add),
                  [PB[2], iop], [idxw])
            dump("pend", pend[:], [128, 256], F32, [pend])
            dump("blk", blk[:], [128, 3], F32, [blk])
            dump("idxw", idxw[:], [128, NBLK], I32, [idxw])
            h2b = ring(es, "h2b", 2, [128, D], BF16)
            for i in range(NQT):
                h2f = h2[i % 2]
                h2_ = h2b[i % 2]
                DM("sync", lambda e: e.dma_start(out=h2f[:], in_=h2_d[i * 128:(i + 1) * 128, :]), [R_h2d], [h2f])
                A(lambda e: e.activation(out=h2_[:], in_=h2f[:], func=AF.Copy), [h2f], [h2_])
                T(lambda e: e.matmul(PB[3][:, 0:256], lhsT=tri_b[:], rhs=selb_all[:, i, :], start=True, stop=True), [tri_b, selb_all], [PB[3]])
                T(lambda e: e.matmul(PB[3][:, 256:512], lhsT=ones_b[:], rhs=selb_all[:, i, :], start=False, stop=True, skip_group_check=True), [ones_b, selb_all], [PB[3]])
                V(lambda e: e.tensor_tensor(out=Dm[:], in0=PB[3][:, 0:256], in1=base[:], op=ALU.add), [PB[3], base], [Dm])
                V(lambda e: e.tensor_tensor(out=Dm[:], in0=Dm[:], in1=pst1[:], op=ALU.add), [Dm, pst1], [Dm])
                V(lambda e: e.tensor_tensor(out=Dm[:], in0=Dm[:], in1=selb_all[:, i, :], op=ALU.mult), [Dm, selb_all], [Dm])
                V(lambda e: e.tensor_tensor(out=base[:], in0=PB[3][:, 256:512], in1=base[:], op=ALU.add), [PB[3], base], [base])
                V(lambda e: e.max(out=d8[:], in_=Dm[:]), [Dm], [d8])
                V(lambda e: e.tensor_scalar(out=idx_all[:, i, :], in0=d8[:], scalar1=-1.0, scalar2=None, op0=ALU.add), [d8], [idx_all])
                for k in range(8):
                    V(lambda e: e.tensor_scalar(out=msk[:], in0=Dm[:], scalar1=d8[:, k:k + 1], scalar2=None, op0=ALU.is_equal), [Dm, d8], [msk])
                    V(lambda e: e.tensor_tensor(out=msk[:], in0=msk[:], in1=wn_all[:, i, :], op=ALU.mult), [msk, wn_all], [msk])
                    V(lambda e: e.tensor_reduce(out=wk_all[:, i, k:k + 1], in_=msk[:], axis=mybir.AxisListType.X, op=ALU.add), [msk], [wk_all])
                dump("Dm0", Dm[:], [128, 256], F32, [Dm])
                for k in range(8):
                    DM("gpsimd", lambda e: e.indirect_dma_start(out=xg, out_offset=bass.IndirectOffsetOnAxis(ap=idx_all[:, i, k:k + 1], axis=0),
                                                                in_=h2_[:], in_offset=None), [h2_, idx_all], [R_xg], nowaw=True)
            dump("idx", idx_all[:], [128, NQT, 8], I32, [idx_all])
            dump("wk", wk_all[:], [128, NQT, 8], F32, [wk_all])
            dump("base", base[:], [128, 256], F32, [base])
            allb = [wr, wsg, wsd, rbias, base, cnt, selb_all, wn_all, h2T, junk, s_, ssel, m8, gs, gm, msk, self_, Dm, d8, wsum, sg, hs, hsT, ysh,
                    ci, padf, pend, pst1, thr, thr_i, blk, dg, iop, iop_i] + h2 + st + h2b
            scope_end(allb)

        w1v = w1.rearrange("e (p k) n -> (e p) (k n)", k=8)
        w3v = w3.rearrange("e (p k) n -> (e p) (k n)", k=8)
        w2v = w2.rearrange("e (p j) n -> (e p) (j n)", j=2)
        with ExitStack() as es:
            NS = 3
            wg1f = ring(es, "wg1f", NS, [128, 2048], F32)
            wg3f = ring(es, "wg3f", NS, [128, 2048], F32)
            wdf = ring(es, "wdf", NS, [128, 2048], F32)
            wg1 = ring(es, "wg1", 2, [128, 8, 256], BF16)
            wg3 = ring(es, "wg3", 2, [128, 8, 256], BF16)
            wd = ring(es, "wd", 2, [128, 2, D], BF16)
            Xe = ring(es, "Xe", 2, [128, D], BF16)
            XeT = ring(es, "XeT", 2, [128, 8, 128], BF16)
            sgr = ring(es, "sgr", 2, [128, 256], F32)
            he = ring(es, "he", 2, [128, 256], BF16)
            heT = ring(es, "heT", 2, [128, 2, 128], BF16)
            Ye = ring(es, "Ye", 2, [128, D], BF16)

            breg = {}

            def gat(e, dst, src, off):
                if "r" not in breg:
                    breg["r"] = e.to_reg(NEXP * 128 - 1)
                return e.indirect_dma_start(out=dst, out_offset=None, in_=src, in_offset=off, bounds_check=breg["r"], oob_is_err=False)

            def gathers(bi):
                b = bi % NS
                off = bass.IndirectOffsetOnAxis(ap=idxw[:, bi:bi + 1], axis=0)
                DM("gpsimd", lambda e, d=wg1f[b][:], o=off: gat(e, d, w1v, o), [idxw], [wg1f[b]], eager=False)
                DM("gpsimd", lambda e, d=wg3f[b][:], o=off: gat(e, d, w3v, o), [idxw], [wg3f[b]], eager=False)
                DM("gpsimd", lambda e, d=wdf[b][:], o=off: gat(e, d, w2v, o), [idxw], [wdf[b]], eager=False)

            def casts(bi):
                b = bi % 2
                f = bi % NS
                A(lambda e: e.activation(out=wg1[b][:].rearrange("p k n -> p (k n)"), in_=wg1f[f][:], func=AF.Copy), [wg1f[f]], [wg1[b]])
                V(lambda e: e.tensor_copy(out=wg3[b][:].rearrange("p k n -> p (k n)"), in_=wg3f[f][:]), [wg3f[f]], [wg3[b]])
                A(lambda e: e.activation(out=wd[b][:].rearrange("p j n -> p (j n)"), in_=wdf[f][:], func=AF.Copy), [wdf[f]], [wd[b]])
                DM("sync", lambda e: e.dma_start(out=Xe[b][:], in_=xg[bi * 128:(bi + 1) * 128, :]), [R_xg], [Xe[b]])

            def stage1(bi):
                b = bi % 2
                for k in range(8):
                    T(lambda e: e.transpose(out=pbf(0)[:, k * 128:(k + 1) * 128], in_=Xe[b][:, k * 128:(k + 1) * 128], identity=ident_b[:]), [Xe[b], ident_b], [PB[0]])
                A(lambda e: e.activation(out=XeT[b][:].rearrange("p k t -> p (k t)"), in_=pbf(0)[:, :], func=AF.Copy), [PB[0]], [XeT[b]])
                pg = PB[2 + b]
                for k in range(8):
                    T(lambda e: e.matmul(pg[:, 0:256], lhsT=XeT[b][:, k, :], rhs=wg1[b][:, k, :], start=(k == 0), stop=(k == 7)), [XeT[b], wg1[b]], [pg])
                for k in range(8):
                    T(lambda e: e.matmul(pg[:, 256:512], lhsT=XeT[b][:, k, :], rhs=wg3[b][:, k, :], start=(k == 0), stop=(k == 7), skip_group_check=True), [XeT[b], wg3[b]], [pg])

            def stage2(bi):
                b = bi % 2
                pg = PB[2 + b]
                A(lambda e: e.activation(out=sgr[b][:], in_=pg[:, 0:256], func=AF.Silu), [pg], [sgr[b]])
                V(lambda e: e.tensor_tensor(out=he[b][:].rearrange("t (j p) -> t p j", j=2), in0=pg.t[:, 256:512].rearrange("t (p j) -> t p j", j=2),
                                            in1=sgr[b][:].rearrange("t (p j) -> t p j", j=2), op=ALU.mult), [pg, sgr[b]], [he[b]])
                for j in range(2):
                    T(lambda e: e.transpose(out=pbf(1)[:, j * 128:(j + 1) * 128], in_=he[b][:, j * 128:(j + 1) * 128], identity=ident_b[:]), [he[b], ident_b], [PB[1]])
                V(lambda e: e.tensor_copy(out=heT[b][:].rearrange("p j t -> p (j t)"), in_=pbf(1)[:, 0:256]), [PB[1]], [heT[b]])
                for hf in range(2):
                    pb = PB[4 + 2 * b + hf]
                    for j in range(2):
                        T(lambda e: e.matmul(pb[:, :], lhsT=heT[b][:, j, :], rhs=wd[b][:, j, hf * 512:(hf + 1) * 512], start=(j == 0), stop=(j == 1)),
                          [heT[b], wd[b]], [pb])
                V(lambda e: e.tensor_copy(out=Ye[b][:, 0:512], in_=PB[4 + 2 * b][:, :]), [PB[4 + 2 * b]], [Ye[b]])
                V(lambda e: e.tensor_copy(out=Ye[b][:, 512:1024], in_=PB[5 + 2 * b][:, :]), [PB[5 + 2 * b]], [Ye[b]])
                dump("he0", he[b][:], [128, 256], BF16, [he[b]])
                dump("Ye0", Ye[b][:], [128, D], BF16, [Ye[b]])
                DM("sync", lambda e: e.dma_start(out=yg[bi * 128:(bi + 1) * 128, :], in_=Ye[b][:]), [Ye[b]], [R_yg], nowaw=True)

            for g0 in range(NS):
                gathers(g0)
            casts(0)
            stage1(0)
            for bi in range(NBLK):
                if bi + NS < NBLK:
                    gathers(bi + NS)
                if bi + 1 < NBLK:
                    casts(bi + 1)
                    stage1(bi + 1)
                stage2(bi)
            allb = wg1f + wg3f + wdf + wg1 + wg3 + wd + Xe + XeT + sgr + he + heT + Ye
            scope_end(allb)

        R_out = Res("out")
        with ExitStack() as es:
            Yg = ring(es, "Yg", 4, [128, D], BF16)
            acc = ring(es, "acc", 2, [128, D], F32)
            st = ring(es, "st3", 2, [128, 4], F32)
            junk = sb(es, "junk3", [128, D], F32)
            gi = 0
            for i in range(NQT):
                ac = acc[i % 2]
                s2 = st[i % 2]
                for k in range(8):
                    y_ = Yg[gi % 4]
                    gi += 1
                    DM("gpsimd", lambda e, y_=y_, i=i, k=k: e.indirect_dma_start(out=y_[:], out_offset=None, in_=yg,
                                                                                 in_offset=bass.IndirectOffsetOnAxis(ap=idx_all[:, i, k:k + 1], axis=0)),
                       [R_yg, idx_all], [y_])
                    if k == 0:
                        V(lambda e, y_=y_, ac=ac, i=i: e.tensor_scalar(out=ac[:], in0=y_[:], scalar1=wk_all[:, i, 0:1], scalar2=None, op0=ALU.mult), [y_, wk_all], [ac])
                    else:
                        V(lambda e, y_=y_, ac=ac, i=i, k=k: e.scalar_tensor_tensor(out=ac[:], in0=y_[:], scalar=wk_all[:, i, k:k + 1], in1=ac[:], op0=ALU.mult, op1=ALU.add),
                          [y_, wk_all, ac], [ac])
                V(lambda e, ac=ac: e.tensor_tensor(out=ac[:], in0=ac[:], in1=rows[:, R_GT2, :], op=ALU.mult), [ac, rows], [ac])
                V(lambda e, ac=ac, i=i: e.tensor_tensor(out=ac[:], in0=ac[:], in1=x1[:, i, :], op=ALU.add), [ac, x1], [ac])
                A(lambda e, ac=ac, s2=s2: e.activation(out=junk[:], in_=ac[:], func=AF.Square, accum_out=s2[:, 0:1]), [ac], [junk, s2])
                V(lambda e, s2=s2: e.tensor_scalar(out=s2[:, 1:2], in0=s2[:, 0:1], scalar1=1.0 / D, scalar2=EPS, op0=ALU.mult, op1=ALU.add), [s2], [s2])
                G(lambda e, s2=s2: e.tensor_tensor(out=s2[:, 2:3], in0=s2[:, 1:2], in1=mhalf[:, 0:1], op=ALU.pow), [s2, mhalf], [s2])
                V(lambda e, ac=ac, s2=s2: e.scalar_tensor_tensor(out=ac[:], in0=ac[:], scalar=s2[:, 2:3], in1=rows[:, R_GF, :], op0=ALU.mult, op1=ALU.mult),
                  [ac, s2, rows], [ac])
                DM("sync", lambda e, ac=ac, i=i: e.dma_start(out=out[i * 128:(i + 1) * 128, :], in_=ac[:]), [ac], [R_out], nowaw=True)
            allb = Yg + acc + st + [junk]
            scope_end(allb + [R_out, R_dump])
        fw.finish()
    return nc


def _rope_tables(order_pos):
    n = order_pos.shape[0]
    pos = np.maximum(order_pos, 0)
    row = (pos // 64).astype(np.float32)
    col = (pos % 64).astype(np.float32)
    isctx = (order_pos < 0)

    def theta(rot_dim):
        nf = rot_dim // 4
        inv = (np.float32(10000.0) ** (-(np.arange(nf, dtype=np.float32) / np.float32(nf)))).astype(np.float32)
        th = np.concatenate([row[:, None] * inv, col[:, None] * inv], axis=-1).astype(np.float32)
        th[isctx] = 0.0
        return th

    thm = theta(32)
    thd = theta(64)
    tabm = np.zeros((2, 128, n), np.float32)
    tabm[0, 0:64, :] = 1.0
    cm, sm = np.cos(thm).T, np.sin(thm).T
    tabm[0, 64:80], tabm[0, 80:96] = cm, cm
    tabm[1, 64:80], tabm[1, 80:96] = -sm, sm
    tabd = np.zeros((2, 128, n), np.float32)
    cd, sd = np.cos(thd).T, np.sin(thd).T
    for g in range(2):
        tabd[0, g * 64:g * 64 + 32], tabd[0, g * 64 + 32:g * 64 + 64] = cd, cd
        tabd[1, g * 64:g * 64 + 32], tabd[1, g * 64 + 32:g * 64 + 64] = -sd, sd
    return tabm, tabd


def _swap_halves(w, group):
    shp = w.shape
    v = w.reshape(shp[0], -1, 2, group // 2)
    return np.ascontiguousarray(v[:, :, ::-1, :]).reshape(shp)


_PROGRAM = None


def kernel(x, c, ctx, c_ctx, w_mod, b_mod, g_attn, g_ffn, w_in, g_q_lat, w_uq, g_kv_lat, w_ukv,
           lam_q1, lam_k1, lam_q2, lam_k2, g_subln, w_out, w_router, router_bias, w1, w3, w2,
           ws1, ws3, ws2, g_final):
    global _PROGRAM
    f = lambda a: np.ascontiguousarray(np.asarray(a, dtype=np.float32))
    x, c, ctx, c_ctx = f(x), f(c), f(ctx), f(c_ctx)
    w_in0 = f(w_in)[0]
    cq, ckv, kr = w_in0[:, 0:256], w_in0[:, 256:384], w_in0[:, 384:416]
    dq, dk, dv = w_in0[:, 416:928], w_in0[:, 928:1440], w_in0[:, 1440:1952]
    krp = np.zeros((D, 2, 96), np.float32)
    krp[:, 0, 64:96] = kr
    krp[:, 1, 64:96] = _swap_halves(kr, 32)
    dqw = np.stack([dq, _swap_halves(dq, 64)], axis=1)
    dkw = np.stack([dk, _swap_halves(dk, 64)], axis=1)
    uq = f(w_uq)[0].reshape(256, 8, 96)
    uqp = np.zeros((256, 8, 96), np.float32)
    uqp[:, :, 64:96] = _swap_halves(np.ascontiguousarray(uq[:, :, 64:96]).reshape(256, 256), 32).reshape(256, 8, 32)
    uqw = np.ascontiguousarray(np.stack([uq, uqp], axis=1))
    ukv = f(w_ukv)[0].reshape(128, 8, 128)
    wkn = np.ascontiguousarray(ukv[:, :, 0:64]).reshape(128, 512)
    wv = np.ascontiguousarray(ukv[:, :, 64:128]).reshape(128, 512)
    shared = {
        "w_mod": f(w_mod)[0], "b_mod": f(b_mod), "g_attn": f(g_attn).reshape(8, 128), "g_ffn": f(g_ffn), "g_final": f(g_final).reshape(1, D),
        "w_cq": f(cq), "w_ckv": f(ckv), "w_krp": krp, "w_dq": f(dqw), "w_dk": f(dkw), "w_dv": f(dv),
        "g_q": f(g_q_lat).reshape(2, 128), "w_uq": uqw, "g_kv": f(g_kv_lat).reshape(128, 1), "w_kn": wkn, "w_v": wv,
        "lam4": np.concatenate([f(lam_q1), f(lam_k1), f(lam_q2), f(lam_k2)], axis=0), "g_sub": f(g_subln),
        "w_out": f(w_out)[0], "w_router": f(w_router)[0], "r_bias": f(router_bias),
        "w1": f(w1)[0], "w3": f(w3)[0], "w2": f(w2)[0], "ws1": f(ws1)[0], "ws3": f(ws3)[0], "ws2": f(ws2)[0],
    }
    in_maps = []
    for core in range(NCORES):
        b, qh = core // 2, core % 2
        own = slice(qh * 2048, (qh + 1) * 2048)
        oth = slice((1 - qh) * 2048, (2 - qh) * 2048)
        xkc = np.concatenate([ctx[b], x[b, oth], x[b, own]], axis=0)
        pos = np.concatenate([-np.ones(256, np.int64), np.arange(oth.start, oth.stop), np.arange(own.start, own.stop)])
        tabm, tabd = _rope_tables(pos)
        m = dict(shared)
        m.update({"xk": np.ascontiguousarray(xkc), "cv": np.ascontiguousarray(np.stack([c[b], c_ctx], axis=0)), "tabm": tabm, "tabd": tabd})
        in_maps.append(m)
    if _PROGRAM is None:
        _PROGRAM = build_program()
    res = run_bass_kernel_spmd(_PROGRAM, in_maps, core_ids=list(range(NCORES)))
    outp = np.zeros((4, 4096, D), np.float32)
    for core in range(NCORES):
        b, qh = core // 2, core % 2
        outp[b, qh * 2048:(qh + 1) * 2048] = np.asarray(res.results[core]["out"], dtype=np.float32)
    return outp
```

```python
import math
from contextlib import ExitStack
import numpy as np
import concourse.bass as bass
import concourse.mybir as mybir
from concourse.bass_utils import run_bass_kernel_spmd

F32 = mybir.dt.float32
BF16 = mybir.dt.bfloat16
I32 = mybir.dt.int32
AF = mybir.ActivationFunctionType
ALU = mybir.AluOpType

NCORES = 8
D = 1024
NKT = 34
NQT = 16
QT0 = 18
EPS = 1e-6
NEXP = 256
CAP = 128


class Res:
    __slots__ = ("name", "w", "r", "dsem", "dcount")

    def __init__(self, name=""):
        self.name = name
        self.w = None
        self.r = {}
        self.dsem = None
        self.dcount = 0


class _Rec:
    def __init__(self):
        self.call = None

    def __getattr__(self, name):
        def f(*a, **k):
            self.call = (name, a, k)
            return self
        return f


def _eager(fn):
    rec = _Rec()
    fn(rec)
    name, a, k = rec.call
    return lambda e: getattr(e, name)(*a, **k)


class EngQ:
    def __init__(self, fw, name):
        self.name = name
        self.sem = fw.new_sem("q_" + name)
        self.count = 0
        self.waited = {}
        self.thunks = []


class FW:
    ENGS = ["sync", "scalar", "vector", "gpsimd", "tensor"]

    def __init__(self, nc, es):
        self.nc = nc
        self.es = es
        self.nsem = 0
        self.sem_pool = []
        self.q = {n: EngQ(self, n) for n in self.ENGS}
        self.same_engine_wait = {"scalar": True, "vector": True, "gpsimd": True,
                                 "tensor": False, "sync": False}

    def new_sem(self, name):
        self.nsem += 1
        return self.es.enter_context(self.nc.semaphore(f"{name}_{self.nsem}"))

    def _deps(self, reads, writes, nowaw):
        deps = []
        for r in reads:
            if r.w is not None:
                deps.append(r.w)
        for w in writes:
            if w.w is not None and not nowaw:
                deps.append(w.w)
            deps.extend(w.r.values())
        return deps

    def _waits(self, q, deps):
        waits = []
        for (sem, val, dq) in deps:
            if dq is q and not self.same_engine_wait[q.name]:
                continue
            k = id(sem)
            if q.waited.get(k, 0) >= val:
                continue
            q.waited[k] = val
            waits.append((sem, val))
        return waits

    def _commit(self, tok, reads, writes):
        for w in writes:
            w.w = tok
            w.r = {}
        for r in reads:
            k = id(tok[0])
            old = r.r.get(k)
            if old is None or old[1] < tok[1]:
                r.r[k] = tok

    def op(self, eng, fn, reads=(), writes=()):
        fn = _eager(fn)
        q = self.q[eng]
        waits = self._waits(q, self._deps(reads, writes, False))
        q.count += 1
        sem = q.sem

        def thunk(e):
            for (s, v) in waits:
                e.wait_ge(s, v)
            fn(e).then_inc(sem, 1)
        q.thunks.append(thunk)
        self._commit((sem, q.count, q), reads, writes)

    def dma(self, eng, fn, reads=(), writes=(), nowaw=False, eager=True):
        if eager:
            fn = _eager(fn)
        q = self.q[eng]
        waits = self._waits(q, self._deps(reads, writes, nowaw))
        dst = writes[0]
        if dst.dsem is None:
            if self.sem_pool:
                dst.dsem, dst.dcount = self.sem_pool.pop()
            else:
                dst.dsem = self.new_sem("d")
        dst.dcount += 16
        sem, val = dst.dsem, dst.dcount

        def thunk(e):
            for (s, v) in waits:
                e.wait_ge(s, v)
            fn(e).then_inc(sem, 16)
        q.thunks.append(thunk)
        self._commit((sem, val, None), reads, writes)

    def final_wait(self, eng, ress):
        q = self.q[eng]
        deps = []
        for r in ress:
            if r.w is not None:
                deps.append(r.w)
            deps.extend(r.r.values())
        waits = self._waits(q, deps)

        def thunk(e):
            for (s, v) in waits:
                e.wait_ge(s, v)
        q.thunks.append(thunk)

    def finish(self):
        with self.nc.Block() as block:
            for name in self.ENGS:
                q = self.q[name]
                if not q.thunks:
                    continue

                def body(e, q=q):
                    for t in q.thunks:
                        t(e)
                getattr(block, name)(body)


class Buf:
    def __init__(self, t, name):
        self.t = t
        self.R = Res(name)

    def __getitem__(self, k):
        return self.t[k]


def build_program(stage="full"):
    nc = bass.Bass("TRN2", target_bir_lowering=False)

    def din(name, shape, dt=F32):
        return nc.dram_tensor(name, list(shape), dt, kind="ExternalInput").ap()

    xk = din("xk", [NKT * 128, D])
    cv = din("cv", [2, D])
    tabm = din("tabm", [2, 128, NKT * 128])
    tabd = din("tabd", [2, 128, NKT * 128])
    w_mod = din("w_mod", [D, 6 * D])
    b_mod = din("b_mod", [1, 6 * D])
    g_attn = din("g_attn", [8, 128])
    g_ffn = din("g_ffn", [1, D])
    g_final = din("g_final", [1, D])
    w_cq = din("w_cq", [D, 256])
    w_ckv = din("w_ckv", [D, 128])
    w_krp = din("w_krp", [D, 2, 96])
    w_dq = din("w_dq", [D, 2, 512])
    w_dk = din("w_dk", [D, 2, 512])
    w_dv = din("w_dv", [D, 512])
    g_q = din("g_q", [2, 128])
    w_uq = din("w_uq", [256, 2, 8, 96])
    g_kv = din("g_kv", [128, 1])
    w_kn = din("w_kn", [128, 512])
    w_v = din("w_v", [128, 512])
    lam4 = din("lam4", [4, 64])
    g_sub = din("g_sub", [1, 128])
    w_out = din("w_out", [D, D])
    w_router = din("w_router", [D, 256])
    r_bias = din("r_bias", [1, 256])
    if stage in ("full", "dbg"):
        w1 = din("w1", [NEXP, D, 256])
        w3 = din("w3", [NEXP, D, 256])
        w2 = din("w2", [NEXP, 256, D])
    ws1 = din("ws1", [D, 256])
    ws3 = din("ws3", [D, 256])
    ws2 = din("ws2", [256, D])
    out = nc.dram_tensor("out", [NQT * 128, D], F32, kind="ExternalOutput").ap()
    xg = nc.dram_tensor("xg", [384 * 128, D], BF16, kind="Internal").ap()
    yg = nc.dram_tensor("yg", [384 * 128, D], BF16, kind="Internal").ap()
    h2_d = nc.dram_tensor("h2_d", [NQT * 128, D], F32, kind="Internal").ap()
    ao_d = nc.dram_tensor("ao_d", [NQT * 128, D], BF16, kind="Internal" if stage == "full" else "ExternalOutput").ap()

    with ExitStack() as es0:
        fw = FW(nc, es0)

        uniq = [0]

        def sb(es, name, shape, dt=F32):
            uniq[0] += 1
            name = f"{name}_{uniq[0]}"
            return Buf(es.enter_context(nc.sbuf_tensor(name, list(shape), dt)), name)

        def ring(es, name, n, shape, dt=F32):
            return [sb(es, f"{name}{i}", shape, dt) for i in range(n)]

        PB = [Buf(es0.enter_context(nc.psum_tensor(f"pb{i}", [128, 512], F32)), f"pb{i}") for i in range(8)]

        def pbf(i):
            return PB[i].t[:].bitcast(BF16)

        V = lambda fn, R=(), W=(): fw.op("vector", fn, [b.R for b in R], [b.R for b in W])
        A = lambda fn, R=(), W=(): fw.op("scalar", fn, [b.R for b in R], [b.R for b in W])
        G = lambda fn, R=(), W=(): fw.op("gpsimd", fn, [b.R for b in R], [b.R for b in W])
        T = lambda fn, R=(), W=(): fw.op("tensor", fn, [b.R for b in R], [b.R for b in W])

        def DM(eng, fn, R=(), W=(), nowaw=False, eager=True):
            fw.dma(eng, fn, [b.R if isinstance(b, Buf) else b for b in R],
                   [b.R if isinstance(b, Buf) else b for b in W], nowaw=nowaw, eager=eager)

        def scope_end(bufs):
            rs = [b.R if isinstance(b, Buf) else b for b in bufs]
            for en in FW.ENGS:
                fw.final_wait(en, rs)
            for r in rs:
                if r.dsem is not None:
                    fw.sem_pool.append((r.dsem, r.dcount))
                    r.dsem = None

        dumps = {}
        R_dump = Res("dumps")

        def dump(name, ap, shape, dt, R):
            if stage != "dbg" or name in dumps:
                return
            d = nc.dram_tensor("dmp_" + name, list(shape), dt, kind="ExternalOutput").ap()
            dumps[name] = R_dump
            DM("sync", lambda e: e.dma_start(out=d, in_=ap), R, [R_dump], nowaw=True)

        ident_f = sb(es0, "ident_f", [128, 128], F32)
        ident_b = sb(es0, "ident_b", [128, 128], BF16)
        ones_f = sb(es0, "ones_f", [128, 128], F32)
        ones_b = sb(es0, "ones_b", [128, 128], BF16)
        tri_b = sb(es0, "tri_b", [128, 128], BF16)
        mhalf = sb(es0, "mhalf", [128, 8], F32)
        sel2 = sb(es0, "sel2", [2, 2, 128], F32)
        iota_e = sb(es0, "iota_e", [128, 256], F32)
        with ExitStack() as es:
            it = sb(es, "it_i", [128, 128], I32)
            itf = sb(es, "it_f", [128, 128], F32)
            it2 = sb(es, "it2_i", [128, 256], I32)
            G(lambda e: e.iota(it[:], pattern=[[1, 128]], base=0, channel_multiplier=-1), [], [it])
            V(lambda e: e.tensor_copy(out=itf[:], in_=it[:]), [it], [itf])
            V(lambda e: e.tensor_single_scalar(out=ident_f[:], in_=itf[:], scalar=0.0, op=ALU.is_equal), [itf], [ident_f])
            V(lambda e: e.tensor_single_scalar(out=ident_b[:], in_=itf[:], scalar=0.0, op=ALU.is_equal), [itf], [ident_b])
            V(lambda e: e.tensor_single_scalar(out=tri_b[:], in_=itf[:], scalar=0.0, op=ALU.is_gt), [itf], [tri_b])
            G(lambda e: e.iota(it2[:], pattern=[[128, 256]], base=1, channel_multiplier=0), [], [it2])
            V(lambda e: e.tensor_copy(out=iota_e[:], in_=it2[:]), [it2], [iota_e])
            G(lambda e: e.memset(ones_f[:], 1.0), [], [ones_f])
            G(lambda e: e.memset(ones_b[:], 1.0), [], [ones_b])
            G(lambda e: e.memset(mhalf[:], -0.5), [], [mhalf])
            G(lambda e: e.memset(sel2[:, 0, :], 0.0), [], [sel2])
            G(lambda e: e.memset(sel2[0:1, 0, :], 1.0), [], [sel2])
            G(lambda e: e.memset(sel2[:, 1, :], 1.0), [], [sel2])
            G(lambda e: e.memset(sel2[0:1, 1, :], 0.0), [], [sel2])
            scope_end([it, itf, it2])

        modrow_d = nc.dram_tensor("modrow_d", [2, 6 * D], F32, kind="Internal").ap()
        R_modd = Res("modrow_d")
        G1T = sb(es0, "G1T", [128, 2, 8], F32)
        S1T = sb(es0, "S1T", [128, 2, 8], F32)
        with ExitStack() as es:
            svT = sb(es, "svT", [128, 2, 8], F32)
            modrow = sb(es, "modrow", [2, 6 * D], F32)
            bm = sb(es, "bm", [2, 6 * D], F32)
            gaT0 = sb(es, "gaT0", [8, 128], F32)
            gaT = sb(es, "gaT", [128, 8], F32)
            wblk = ring(es, "wblk", 2, [128, 8, 1024], F32)
            DM("sync", lambda e: e.dma_start(out=svT[:], in_=cv.rearrange("r (p k) -> p r k", k=8)), [], [svT])
            DM("sync", lambda e: e.dma_start(out=bm[0:1, :], in_=b_mod), [], [bm])
            DM("sync", lambda e: e.dma_start(out=bm[1:2, :], in_=b_mod), [], [bm])
            DM("sync", lambda e: e.dma_start(out=gaT0[:], in_=g_attn), [], [gaT0])
            A(lambda e: e.activation(out=svT[:], in_=svT[:], func=AF.Silu), [svT], [svT])
            wm = w_mod.rearrange("(p k) n -> p k n", k=8)
            for nb in range(6):
                wb_ = wblk[nb % 2]
                DM("sync", lambda e, wb_=wb_, nb=nb: e.dma_start(out=wb_[:], in_=wm[:, :, nb * 1024:(nb + 1) * 1024]), [], [wb_])
                for hf in range(2):
                    pb = PB[hf]
                    for k in range(8):
                        T(lambda e, k=k, pb=pb, wb_=wb_, hf=hf: e.matmul(pb[0:2, :], lhsT=svT[:, :, k], rhs=wb_[:, k, hf * 512:(hf + 1) * 512],
                                                                         start=(k == 0), stop=(k == 7)), [svT, wb_], [pb])
                    c0 = nb * 1024 + hf * 512
                    V(lambda e, pb=pb, c0=c0: e.tensor_tensor(out=modrow[:, c0:c0 + 512], in0=pb[0:2, :], in1=bm[:, c0:c0 + 512], op=ALU.add),
                      [pb, bm], [modrow])
            pt = PB[2]
            T(lambda e: e.transpose(out=pt[:, 0:8], in_=gaT0[:], identity=ident_f[0:8, 0:8]), [gaT0, ident_f], [pt])
            V(lambda e: e.tensor_copy(out=gaT[:], in_=pt[:, 0:8]), [pt], [gaT])
            pt3 = PB[3]
            for j in range(16):
                T(lambda e, j=j: e.transpose(out=pt3[:, j * 2:j * 2 + 2], in_=modrow[:, j * 128:(j + 1) * 128], identity=ident_f[0:2, 0:2]),
                  [modrow, ident_f], [pt3])
            ptv = pt3.t[:, 0:32].rearrange("p (j r) -> p r j", r=2)
            V(lambda e: e.tensor_copy(out=S1T[:], in_=ptv[:, :, 0:8]), [pt3], [S1T])
            for r in range(2):
                V(lambda e, r=r: e.scalar_tensor_tensor(out=G1T[:, r, :], in0=ptv[:, r, 8:16], scalar=1.0, in1=gaT[:],
                                                        op0=ALU.add, op1=ALU.mult), [pt3, gaT], [G1T])
            DM("sync", lambda e: e.dma_start(out=modrow_d, in_=modrow[:]), [modrow], [R_modd])
            dump("modrow", modrow[:], [2, 6 * D], F32, [modrow])
            dump("G1T", G1T[:], [128, 2, 8], F32, [G1T])
            dump("S1T", S1T[:], [128, 2, 8], F32, [S1T])
            scope_end([svT, bm, gaT0, gaT, modrow] + wblk)

        neglam = sb(es0, "neglam", [128, 1], F32)
        gsub08 = sb(es0, "gsub08", [128, 128], F32)
        with ExitStack() as es:
            lt = sb(es, "lt", [128, 4, 64], F32)
            lj = sb(es, "lj", [128, 64], F32)
            ld = sb(es, "ld", [128, 2], F32)
            DM("sync", lambda e: e.dma_start(out=lt[:], in_=lam4.rearrange("a b -> (a b)").partition_broadcast(128).rearrange("p (a b) -> p a b", a=4)), [], [lt])
            DM("sync", lambda e: e.dma_start(out=gsub08[:], in_=g_sub.rearrange("a b -> (a b)").partition_broadcast(128)), [], [gsub08])
            for i in range(2):
                V(lambda e: e.tensor_tensor(out=lj[:], in0=lt[:, 2 * i, :], in1=lt[:, 2 * i + 1, :], op=ALU.mult), [lt], [lj])
                V(lambda e: e.tensor_reduce(out=ld[:, i:i + 1], in_=lj[:], axis=mybir.AxisListType.X, op=ALU.add), [lj], [ld])
            A(lambda e: e.activation(out=ld[:], in_=ld[:], func=AF.Exp), [ld], [ld])
            V(lambda e: e.scalar_tensor_tensor(out=neglam[:], in0=ld[:, 1:2], scalar=-0.2, in1=ld[:, 0:1], op0=ALU.add, op1=ALU.subtract),
              [ld], [neglam])
            V(lambda e: e.tensor_scalar(out=gsub08[:], in0=gsub08[:], scalar1=0.8, scalar2=None, op0=ALU.mult), [gsub08], [gsub08])
            dump("neglam", neglam[:], [128, 1], F32, [neglam])
            dump("gsub08", gsub08[:], [128, 128], F32, [gsub08])
            scope_end([lt, lj, ld])

        R_ao = Res("ao_d")

        def attention_pass(kind):
            mla = kind == "mla"
            with ExitStack() as es:
                NH = 8 if mla else 4
                DV = 65 if mla else 129
                kT = sb(es, "kT", [128, NH, NKT * 128], BF16)
                Vaug = sb(es, "Vaug", [128, NKT, NH, DV], BF16)
                qT = sb(es, "qT", [128, NH, 512], BF16)
                xts = ring(es, "xt", 4, [128, D], F32)
                xsr = ring(es, "xs", 2, [128, D], BF16)
                junk = sb(es, "junk", [128, D], BF16)
                st = ring(es, "st", 4, [128, 4], F32)
                hTr = ring(es, "hT", 2, [128, 8, 512], BF16)
                tab = sb(es, "tab", [128, 2, 512], F32)
                tmpA = sb(es, "tmpA", [128, 512], F32)
                tmpB = sb(es, "tmpB", [128, 512], F32)
                PT = ring(es, "PT", 4, [128, 512], BF16)
                aos = sb(es, "aos", [128, 4, 512], BF16)
                rec = sb(es, "rec", [128, 4], F32)
                tab_d = tabm if mla else tabd
                G(lambda e: e.memset(Vaug[:, :, :, DV - 1:DV], 1.0), [], [Vaug])
                kp = lambda ap: ap.rearrange("(k p) n -> p k n", p=128)
                if mla:
                    wckv = sb(es, "wckv", [128, 8, 128], BF16)
                    wkrp = sb(es, "wkrp", [128, 8, 2, 96], BF16)
                    wcq = sb(es, "wcq", [128, 8, 256], BF16)
                    wuq = sb(es, "wuq", [128, 2, 2, 8, 96], BF16)
                    gq = sb(es, "gq", [128, 2], F32)
                    gq0 = sb(es, "gq0", [2, 128], F32)
                    wkn = sb(es, "wkn", [128, 512], BF16)
                    wv = sb(es, "wv", [128, 512], BF16)
                    gkv = sb(es, "gkv", [128, 1], F32)
                    sq = sb(es, "sq", [128, 2, 512], F32)
                    rb = sb(es, "rb", [128, 512], F32)
                    cn = sb(es, "cn", [128, 2, 512], BF16)
                    DM("gpsimd", lambda e: e.dma_start(out=wckv[:], in_=kp(w_ckv)), [], [wckv])
                    DM("gpsimd", lambda e: e.dma_start(out=wkrp[:], in_=w_krp.rearrange("(k p) a n -> p k a n", p=128)), [], [wkrp])
                    DM("gpsimd", lambda e: e.dma_start(out=wcq[:], in_=kp(w_cq)), [], [wcq])
                    DM("gpsimd", lambda e: e.dma_start(out=wuq[:].rearrange("p k a h n -> p k (a h n)"), in_=w_uq.rearrange("(k p) a h n -> p k (a h n)", p=128)), [], [wuq])
                    DM("gpsimd", lambda e: e.dma_start(out=wkn[:], in_=w_kn), [], [wkn])
                    DM("gpsimd", lambda e: e.dma_start(out=wv[:], in_=w_v), [], [wv])
                    DM("sync", lambda e: e.dma_start(out=gq0[:], in_=g_q), [], [gq0])
                    DM("sync", lambda e: e.dma_start(out=gkv[:], in_=g_kv), [], [gkv])
                    T(lambda e: e.transpose(out=PB[2][:, 0:2], in_=gq0[:], identity=ident_f[0:2, 0:2]), [gq0, ident_f], [PB[2]])
                    V(lambda e: e.tensor_copy(out=gq[:], in_=PB[2][:, 0:2]), [PB[2]], [gq])
                else:
                    wdk = sb(es, "wdk", [128, 8, 2, 512], BF16)
                    wdq = sb(es, "wdq", [128, 8, 2, 512], BF16)
                    wdv = sb(es, "wdv", [128, 8, 512], BF16)
                    t0 = sb(es, "t0", [128, 4, 128], F32)
                    t1 = sb(es, "t1", [128, 4, 128], F32)
                    ssq = sb(es, "ssq", [128, 4], F32)
                    DM("gpsimd", lambda e: e.dma_start(out=wdk[:], in_=w_dk.rearrange("(k p) a n -> p k a n", p=128)), [], [wdk])
                    DM("gpsimd", lambda e: e.dma_start(out=wdq[:], in_=w_dq.rearrange("(k p) a n -> p k a n", p=128)), [], [wdq])
                    DM("gpsimd", lambda e: e.dma_start(out=wdv[:], in_=kp(w_dv)), [], [wdv])

                cnt = {"x": 0, "s": 0, "g": 0}

                def make_hT(tiles):
                    hT = hTr[cnt["g"] % 2]
                    cnt["g"] += 1
                    cur = []
                    for ti, t in enumerate(tiles):
                        xt = xts[cnt["x"] % 4]
                        s_ = st[cnt["x"] % 4]
                        cnt["x"] += 1
                        cur.append((xt, s_))
                        DM("sync", lambda e: e.dma_start(out=xt[:], in_=xk[t * 128:(t + 1) * 128, :]), [], [xt])
                        A(lambda e: e.activation(out=junk[:], in_=xt[:], func=AF.Square, accum_out=s_[:, 0:1]), [xt], [junk, s_])
                        V(lambda e: e.tensor_scalar(out=s_[:, 1:2], in0=s_[:, 0:1], scalar1=1.0 / D, scalar2=EPS, op0=ALU.mult, op1=ALU.add), [s_], [s_])
                        G(lambda e: e.tensor_tensor(out=s_[:, 2:3], in0=s_[:, 1:2], in1=mhalf[:, 0:1], op=ALU.pow), [s_, mhalf], [s_])
                    for ti, t in enumerate(tiles):
                        xt, s_ = cur[ti]
                        r = 1 if t < 2 else 0
                        x_ = xsr[cnt["s"] % 2]
                        pbi = 2 + (cnt["s"] % 2)
                        cnt["s"] += 1
                        V(lambda e: e.tensor_scalar(out=x_[:], in0=xt[:], scalar1=s_[:, 2:3], scalar2=None, op0=ALU.mult), [xt, s_], [x_])
                        dump(kind + "_st0", s_[:], [128, 4], F32, [s_])
                        for k in range(8):
                            T(lambda e: e.transpose(out=pbf(pbi)[:, k * 128:(k + 1) * 128], in_=x_[:, k * 128:(k + 1) * 128], identity=ident_b[:]),
                              [x_, ident_b], [PB[pbi]])
                        for k in range(8):
                            A(lambda e: e.activation(out=hT[:, k, ti * 128:(ti + 1) * 128], in_=pbf(pbi)[:, k * 128:(k + 1) * 128],
                                                     func=AF.Identity, scale=G1T[:, r, k:k + 1], bias=S1T[:, r, k:k + 1]),
                              [PB[pbi], G1T, S1T], [hT])
                    return hT

                def load_tab(c0, n):
                    DM("sync", lambda e: e.dma_start(out=tab[:, :, 0:n], in_=tab_d[:, :, c0:c0 + n].rearrange("a p n -> p a n")), [], [tab])

                def rope_evac(pa, pb_, rows, n, dst_fn):
                    V(lambda e: e.tensor_tensor(out=tmpA[0:rows, 0:n], in0=pa[0:rows, 0:n], in1=tab[0:rows, 0, 0:n], op=ALU.mult), [pa, tab], [tmpA])
                    V(lambda e: e.tensor_tensor(out=tmpB[0:rows, 0:n], in0=pb_[0:rows, 0:n], in1=tab[0:rows, 1, 0:n], op=ALU.mult), [pb_, tab], [tmpB])
                    dst_fn()

                def rms_T(psrc_list, nchunk, n, width, dst, gain):
                    for c in range(nchunk):
                        A(lambda e, c=c: e.activation(out=sq[:, c, 0:n], in_=psrc_list[c][:, 0:n], func=AF.Square), [psrc_list[c]], [sq])
                    for c in range(nchunk):
                        T(lambda e, c=c: e.matmul(PB[3][:, 0:n], lhsT=ones_f[:], rhs=sq[:, c, 0:n], start=(c == 0), stop=(c == nchunk - 1)),
                          [ones_f, sq], [PB[3]])
                    A(lambda e: e.activation(out=rb[:, 0:n], in_=PB[3][:, 0:n], func=AF.Ln, scale=1.0 / width, bias=EPS), [PB[3]], [rb])
                    A(lambda e: e.activation(out=rb[:, 0:n], in_=rb[:, 0:n], func=AF.Exp, scale=-0.5), [rb], [rb])
                    for c in range(nchunk):
                        V(lambda e, c=c: e.scalar_tensor_tensor(out=dst[:, c, 0:n], in0=psrc_list[c][:, 0:n], scalar=gain[:, c:c + 1], in1=rb[:, 0:n],
                                                                op0=ALU.mult, op1=ALU.mult), [psrc_list[c], rb, gain], [dst])

                groups = [[0, 1]] + [list(range(2 + 4 * g, 6 + 4 * g)) for g in range(8)]
                for tiles in groups:
                    n = len(tiles) * 128
                    c0 = tiles[0] * 128
                    hT = make_hT(tiles)
                    load_tab(c0, n)
                    if mla:
                        for k in range(8):
                            T(lambda e, k=k: e.matmul(PB[0][:, 0:n], lhsT=wckv[:, k, :], rhs=hT[:, k, 0:n], start=(k == 0), stop=(k == 7)), [wckv, hT], [PB[0]])
                        rms_T([PB[0]], 1, n, 128.0, cn, gkv)
                        for a in range(2):
                            for k in range(8):
                                T(lambda e, k=k, a=a: e.matmul(PB[a][0:96, 0:n], lhsT=wkrp[:, k, a, :], rhs=hT[:, k, 0:n], start=(k == 0), stop=(k == 7)),
                                  [wkrp, hT], [PB[a]])

                        def fin():
                            for h in range(8):
                                V(lambda e, h=h: e.tensor_tensor(out=kT[64:96, h, c0:c0 + n], in0=tmpA[64:96, 0:n], in1=tmpB[64:96, 0:n], op=ALU.add),
                                  [tmpA, tmpB], [kT])
                        V(lambda e: e.tensor_tensor(out=tmpA[64:96, 0:n], in0=PB[0][64:96, 0:n], in1=tab[64:96, 0, 0:n], op=ALU.mult), [PB[0], tab], [tmpA])
                        V(lambda e: e.tensor_tensor(out=tmpB[64:96, 0:n], in0=PB[1][64:96, 0:n], in1=tab[64:96, 1, 0:n], op=ALU.mult), [PB[1], tab], [tmpB])
                        fin()
                        for h in range(8):
                            pb = PB[h % 2]
                            T(lambda e, h=h, pb=pb: e.matmul(pb[0:64, 0:n], lhsT=wkn[:, h * 64:(h + 1) * 64], rhs=cn[:, 0, 0:n], start=True, stop=True), [wkn, cn], [pb])
                            A(lambda e, h=h, pb=pb: e.activation(out=kT[0:64, h, c0:c0 + n], in_=pb[0:64, 0:n], func=AF.Copy), [pb], [kT])
                        for ti, t in enumerate(tiles):
                            T(lambda e, ti=ti: e.matmul(PB[3][:, :], lhsT=cn[:, 0, ti * 128:(ti + 1) * 128], rhs=wv[:], start=True, stop=True), [cn, wv], [PB[3]])
                            V(lambda e, t=t: e.tensor_copy(out=Vaug[:, t, :, 0:64], in_=PB[3].t[:, :].rearrange("p (h d) -> p h d", h=8)), [PB[3]], [Vaug])
                    else:
                        for h in range(4):
                            for a in range(2):
                                for k in range(8):
                                    T(lambda e, k=k, a=a, h=h: e.matmul(PB[a][:, 0:n], lhsT=wdk[:, k, a, h * 128:(h + 1) * 128], rhs=hT[:, k, 0:n],
                                                                        start=(k == 0), stop=(k == 7)), [wdk, hT], [PB[a]])
                            rope_evac(PB[0], PB[1], 128, n, lambda h=h: V(
                                lambda e: e.tensor_tensor(out=kT[:, h, c0:c0 + n], in0=tmpA[:, 0:n], in1=tmpB[:, 0:n], op=ALU.add), [tmpA, tmpB], [kT]))
                        for ti, t in enumerate(tiles):
                            for k in range(8):
                                T(lambda e, k=k, ti=ti: e.matmul(PB[3][:, :], lhsT=hT[:, k, ti * 128:(ti + 1) * 128], rhs=wdv[:, k, :], start=(k == 0), stop=(k == 7)),
                                  [hT, wdv], [PB[3]])
                            V(lambda e, t=t: e.tensor_copy(out=Vaug[:, t, :, 0:128], in_=PB[3].t[:, :].rearrange("p (h d) -> p h d", h=4)), [PB[3]], [Vaug])

                    dump(kind + "_hT0", hT[:, :, 0:256], [128, 8, 256], BF16, [hT])
                    dump(kind + "_tab0", tab[:], [128, 2, 512], F32, [tab])
                    dump(kind + "_kT0", kT[:, 0, 0:256], [128, 256], BF16, [kT])
                    dump(kind + "_V0", Vaug[:, 0, :, :], [128, NH, DV], BF16, [Vaug])
                    if mla:
                        dump("cn0", cn[:, 0, 0:256], [128, 256], BF16, [cn])
                        dump("rb0", rb[:, 0:256], [128, 256], F32, [rb])

                scale = 1.0 / math.sqrt(96.0) if mla else 1.0 / 8.0
                pt_i = 0
                s_i = 0
                o_i = 0
                for qc in range(4):
                    tiles = list(range(QT0 + 4 * qc, QT0 + 4 * qc + 4))
                    c0 = tiles[0] * 128
                    hT = make_hT(tiles)
                    load_tab(c0, 512)
                    if mla:
                        for c in range(2):
                            for k in range(8):
                                T(lambda e, k=k, c=c: e.matmul(PB[c][:, :], lhsT=wcq[:, k, c * 128:(c + 1) * 128], rhs=hT[:, k, :], start=(k == 0), stop=(k == 7)),
                                  [wcq, hT], [PB[c]])
                        rms_T([PB[0], PB[1]], 2, 512, 256.0, cn, gq)
                        for h in range(8):
                            for a in range(2):
                                for k in range(2):
                                    T(lambda e, k=k, a=a, h=h: e.matmul(PB[a][0:96, :], lhsT=wuq[:, k, a, h, :], rhs=cn[:, k, :], start=(k == 0), stop=(k == 1)),
                                      [wuq, cn], [PB[a]])
                            rope_evac(PB[0], PB[1], 96, 512, lambda h=h: V(
                                lambda e: e.tensor_tensor(out=qT[0:96, h, :], in0=tmpA[0:96, :], in1=tmpB[0:96, :], op=ALU.add), [tmpA, tmpB], [qT]))
                    else:
                        for h in range(4):
                            for a in range(2):
                                for k in range(8):
                                    T(lambda e, k=k, a=a, h=h: e.matmul(PB[a][:, :], lhsT=wdq[:, k, a, h * 128:(h + 1) * 128], rhs=hT[:, k, :],
                                                                        start=(k == 0), stop=(k == 7)), [wdq, hT], [PB[a]])
                            rope_evac(PB[0], PB[1], 128, 512, lambda h=h: V(
                                lambda e: e.tensor_tensor(out=qT[:, h, :], in0=tmpA[:, :], in1=tmpB[:, :], op=ALU.add), [tmpA, tmpB], [qT]))

                    dump(kind + "_qT0", qT[:, 0, :], [128, 512], BF16, [qT])
                    if mla:
                        dump("cnq", cn[:], [128, 2, 512], BF16, [cn])
                    LOOK = 2
                    SB = [PB[3], PB[4], PB[5]]
                    steps = [(h, j, kt) for h in range(NH) for j in range(1 if mla else 2) for kt in range(NKT)]
                    inflight = {}

                    def emit_S(si):
                        h, j, kt = steps[si]
                        KR = slice(0, 96) if mla else slice(j * 64, (j + 1) * 64)
                        ps = SB[si % 3]
                        p_ = PT[si % 4]
                        T(lambda e: e.matmul(ps[:, :], lhsT=kT[KR, h, kt * 128:(kt + 1) * 128], rhs=qT[KR, h, :], start=True, stop=True), [kT, qT], [ps])
                        A(lambda e: e.activation(out=p_[:], in_=ps[:, :], func=AF.Exp, scale=scale), [ps], [p_])
                        dump(kind + "_PT0", p_[:], [128, 512], BF16, [p_])
                        inflight[si] = p_

                    for si in range(min(LOOK, len(steps))):
                        emit_S(si)
                    for si, (h, j, kt) in enumerate(steps):
                        if si + LOOK < len(steps):
                            emit_S(si + LOOK)
                        p_ = inflight.pop(si)
                        if kt == 0:
                            if mla:
                                ob = [PB[6 + (o_i % 2)]]
                            else:
                                ob = [PB[6], PB[7]] if (o_i % 2 == 0) else [PB[0], PB[1]]
                            o_i += 1
                        for qs in range(4):
                            if mla:
                                o_, col = ob[0], qs * 65
                            else:
                                o_, col = ob[qs // 2], (qs % 2) * 129
                            first = (kt == 0) and (col == 0)
                            T(lambda e: e.matmul(o_[:, col:col + DV], lhsT=p_[:, qs * 128:(qs + 1) * 128], rhs=Vaug[:, kt, h, :],
                                                 start=first, stop=(kt == NKT - 1), skip_group_check=True), [p_, Vaug], [o_])
                        if kt != NKT - 1:
                            continue
                        for qs in range(4):
                            if mla:
                                o_, col = ob[0], qs * 65
                            else:
                                o_, col = ob[qs // 2], (qs % 2) * 129
                            V(lambda e: e.reciprocal(out=rec[:, qs:qs + 1], in_=o_[:, col + DV - 1:col + DV]), [o_], [rec])
                            if mla:
                                V(lambda e: e.tensor_scalar(out=aos[:, qs, h * 64:(h + 1) * 64], in0=o_[:, col:col + 64],
                                                            scalar1=rec[:, qs:qs + 1], scalar2=None, op0=ALU.mult), [o_, rec], [aos])
                            else:
                                tj = t0 if j == 0 else t1
                                V(lambda e: e.tensor_scalar(out=tj[:, qs, :], in0=o_[:, col:col + 128],
                                                            scalar1=rec[:, qs:qs + 1], scalar2=None, op0=ALU.mult), [o_, rec], [tj])
                        if (not mla) and j == 1:
                            V(lambda e: e.scalar_tensor_tensor(out=t0[:].rearrange("p a b -> p (a b)"), in0=t1[:].rearrange("p a b -> p (a b)"), scalar=neglam[:, 0:1],
                                                               in1=t0[:].rearrange("p a b -> p (a b)"), op0=ALU.mult, op1=ALU.add), [t0, t1, neglam], [t0])
                            V(lambda e: e.tensor_tensor(out=t1[:], in0=t0[:], in1=t0[:], op=ALU.mult), [t0], [t1])
                            V(lambda e: e.tensor_reduce(out=ssq[:], in_=t1[:], axis=mybir.AxisListType.X, op=ALU.add), [t1], [ssq])
                            V(lambda e: e.tensor_scalar(out=ssq[:], in0=ssq[:], scalar1=1.0 / 128, scalar2=EPS, op0=ALU.mult, op1=ALU.add), [ssq], [ssq])
                            G(lambda e: e.tensor_tensor(out=ssq[:], in0=ssq[:], in1=mhalf[:, 0:4], op=ALU.pow), [ssq, mhalf], [ssq])
                            for qs in range(4):
                                V(lambda e: e.scalar_tensor_tensor(out=aos[:, qs, h * 128:(h + 1) * 128], in0=t0[:, qs, :], scalar=ssq[:, qs:qs + 1],
                                                                   in1=gsub08[:], op0=ALU.mult, op1=ALU.mult), [t0, ssq, gsub08], [aos])
                    dump(kind + "_aos0", aos[:], [128, 4, 512], BF16, [aos])
                    cb = 0 if mla else 512
                    for qs in range(4):
                        r0 = (qc * 4 + qs) * 128
                        DM("sync", lambda e, qs=qs, r0=r0: e.dma_start(out=ao_d[r0:r0 + 128, cb:cb + 512], in_=aos[:, qs, :]), [aos], [R_ao], nowaw=True)
                scope_end([kT, Vaug, qT, junk, tab, tmpA, tmpB, aos, rec] + hTr + xts + xsr + st + PT)

        attention_pass("mla")
        attention_pass("diff")

        x1 = sb(es0, "x1", [128, NQT, D], F32)
        rows = sb(es0, "rows", [128, 5, D], F32)
        R_GT1, R_G2, R_SH2, R_GT2, R_GF = range(5)
        with ExitStack() as es:
            gff = sb(es, "gff", [128, D], F32)
            DM("sync", lambda e: e.dma_start(out=gff[:], in_=g_ffn.rearrange("a b -> (a b)").partition_broadcast(128)), [], [gff])
            DM("sync", lambda e: e.dma_start(out=rows[:, R_GF, :], in_=g_final.rearrange("a b -> (a b)").partition_broadcast(128)), [], [rows])
            for (dst, ch) in [(R_GT1, 2), (R_SH2, 3), (R_G2, 4), (R_GT2, 5)]:
                DM("sync", lambda e, dst=dst, ch=ch: e.dma_start(out=rows[:, dst, :], in_=modrow_d[0, ch * D:(ch + 1) * D].partition_broadcast(128)), [R_modd], [rows])
            V(lambda e: e.scalar_tensor_tensor(out=rows[:, R_G2, :], in0=rows[:, R_G2, :], scalar=1.0, in1=gff[:], op0=ALU.add, op1=ALU.mult), [rows, gff], [rows])
            wo = sb(es, "wo", [128, 8, D], BF16)
            DM("gpsimd", lambda e: e.dma_start(out=wo[:], in_=w_out.rearrange("(k p) n -> p k n", p=128)), [], [wo])
            aot = ring(es, "aot", 2, [128, D], BF16)
            aoT = ring(es, "aoT", 2, [128, 8, 128], BF16)
            xq = ring(es, "xq", 2, [128, D], F32)
            ty = ring(es, "ty", 2, [128, D], F32)
            for i in range(NQT):
                a_, aT_, x_, ty_ = aot[i % 2], aoT[i % 2], xq[i % 2], ty[i % 2]
                DM("sync", lambda e, a_=a_, i=i: e.dma_start(out=a_[:], in_=ao_d[i * 128:(i + 1) * 128, :]), [R_ao], [a_])
                DM("sync", lambda e, x_=x_, i=i: e.dma_start(out=x_[:], in_=xk[(QT0 + i) * 128:(QT0 + i + 1) * 128, :]), [], [x_])
                for k in range(8):
                    T(lambda e, k=k, a_=a_: e.transpose(out=pbf(2)[:, k * 128:(k + 1) * 128], in_=a_[:, k * 128:(k + 1) * 128], identity=ident_b[:]), [a_, ident_b], [PB[2]])
                A(lambda e, aT_=aT_: e.activation(out=aT_[:].rearrange("p k t -> p (k t)"), in_=pbf(2)[:, :], func=AF.Copy), [PB[2]], [aT_])
                for hf in range(2):
                    pb = PB[hf]
                    for k in range(8):
                        T(lambda e, k=k, pb=pb, aT_=aT_, hf=hf: e.matmul(pb[:, :], lhsT=aT_[:, k, :], rhs=wo[:, k, hf * 512:(hf + 1) * 512], start=(k == 0), stop=(k == 7)),
                          [aT_, wo], [pb])
                    V(lambda e, pb=pb, hf=hf, ty_=ty_: e.tensor_tensor(out=ty_[:, hf * 512:(hf + 1) * 512], in0=pb[:, :], in1=rows[:, R_GT1, hf * 512:(hf + 1) * 512], op=ALU.mult),
                      [pb, rows], [ty_])
                G(lambda e, i=i, ty_=ty_, x_=x_: e.tensor_tensor(out=x1[:, i, :], in0=ty_[:], in1=x_[:], op=ALU.add), [ty_, x_], [x1])
            scope_end([gff, wo] + aot + aoT + xq + ty)

        if stage == "dbg":
            dump("x1a", x1[:], [128, NQT, D], F32, [x1])
            for i in range(NQT):
                DM("sync", lambda e, i=i: e.dma_start(out=x1[:, i, :], in_=xk[(QT0 + i) * 128:(QT0 + i + 1) * 128, :]), [], [x1])
        if stage == "attn":
            R_out = Res("out")
            for i in range(NQT):
                DM("sync", lambda e, i=i: e.dma_start(out=out[i * 128:(i + 1) * 128, :], in_=x1[:, i, :]), [x1], [R_out], nowaw=True)
            scope_end([R_out, R_ao])
            fw.finish()
            return nc
        NBLK = 384
        idx_all = sb(es0, "idx_all", [128, NQT, 8], I32)
        wk_all = sb(es0, "wk_all", [128, NQT, 8], F32)
        idxw = sb(es0, "idxw", [128, NBLK], I32)
        R_xg = Res("xg")
        R_yg = Res("yg")
        R_h2d = Res("h2d")
        pk = lambda ap: ap.rearrange("(p k) n -> p k n", k=8)
        with ExitStack() as es:
            wr = sb(es, "wr", [128, 8, 256], F32)
            wsg = sb(es, "wsg", [128, 8, 512], F32)
            wsd = sb(es, "wsd", [128, 2, D], F32)
            rbias = sb(es, "rbias", [128, 256], F32)
            base = sb(es, "base", [128, 256], F32)
            cnt = sb(es, "cnt", [128, 256], F32)
            selb_all = sb(es, "selb_all", [128, NQT, 256], BF16)
            wn_all = sb(es, "wn_all", [128, NQT, 256], F32)
            DM("sync", lambda e: e.dma_start(out=wr[:], in_=pk(w_router)), [], [wr])
            DM("sync", lambda e: e.dma_start(out=wsg[:, :, 0:256], in_=pk(ws1)), [], [wsg])
            DM("sync", lambda e: e.dma_start(out=wsg[:, :, 256:512], in_=pk(ws3)), [], [wsg])
            DM("sync", lambda e: e.dma_start(out=wsd[:], in_=ws2.rearrange("(j p) n -> p j n", p=128)), [], [wsd])
            DM("sync", lambda e: e.dma_start(out=rbias[:], in_=r_bias.rearrange("a b -> (a b)").partition_broadcast(128)), [], [rbias])
            G(lambda e: e.memset(base[:], 0.0), [], [base])
            G(lambda e: e.memset(cnt[:], 0.0), [], [cnt])
            h2 = ring(es, "h2", 2, [128, D], F32)
            h2T = sb(es, "h2T", [128, 8, 128], F32)
            junk = sb(es, "junkf", [128, D], F32)
            st = ring(es, "st2", 2, [128, 4], F32)
            s_ = sb(es, "s_", [128, 256], F32)
            ssel = sb(es, "ssel", [128, 256], F32)
            m8 = sb(es, "m8", [128, 8, 8], F32)
            gs = sb(es, "gs", [128, 8], F32)
            gm = sb(es, "gm", [128, 8], F32)
            msk = sb(es, "msk", [128, 256], F32)
            self_ = sb(es, "self", [128, 256], F32)
            Dm = sb(es, "Dm", [128, 256], F32)
            d8 = sb(es, "d8", [128, 8], F32)
            wsum = sb(es, "wsum", [128, 2], F32)
            sg = sb(es, "sg", [128, 256], F32)
            hs = sb(es, "hs", [128, 256], F32)
            hsT = sb(es, "hsT", [128, 2, 128], F32)
            ysh = sb(es, "ysh", [128, D], F32)
            for i in range(NQT):
                h2_ = h2[i % 2]
                s2 = st[i % 2]
                A(lambda e: e.activation(out=junk[:], in_=x1[:, i, :], func=AF.Square, accum_out=s2[:, 0:1]), [x1], [junk, s2])
                V(lambda e: e.tensor_scalar(out=s2[:, 1:2], in0=s2[:, 0:1], scalar1=1.0 / D, scalar2=EPS, op0=ALU.mult, op1=ALU.add), [s2], [s2])
                G(lambda e: e.tensor_tensor(out=s2[:, 2:3], in0=s2[:, 1:2], in1=mhalf[:, 0:1], op=ALU.pow), [s2, mhalf], [s2])
                V(lambda e: e.scalar_tensor_tensor(out=junk[:], in0=x1[:, i, :], scalar=s2[:, 2:3], in1=rows[:, R_G2, :], op0=ALU.mult, op1=ALU.mult),
                  [x1, s2, rows], [junk])
                V(lambda e: e.tensor_tensor(out=h2_[:].rearrange("t (k p) -> t p k", k=8), in0=junk[:].rearrange("t (p k) -> t p k", k=8),
                                            in1=rows[:, R_SH2, :].rearrange("t (p k) -> t p k", k=8), op=ALU.add), [junk, rows], [h2_])
                DM("sync", lambda e: e.dma_start(out=h2_d[i * 128:(i + 1) * 128, :], in_=h2_[:]), [h2_], [R_h2d], nowaw=True)
                for hf in range(2):
                    for k in range(4):
                        kk = hf * 4 + k
                        T(lambda e: e.transpose(out=PB[hf][:, k * 128:(k + 1) * 128], in_=h2_[:, kk * 128:(kk + 1) * 128], identity=ident_f[:]),
                          [h2_, ident_f], [PB[hf]])
                    A(lambda e: e.activation(out=h2T[:, hf * 4:(hf + 1) * 4, :].rearrange("p k t -> p (k t)"), in_=PB[hf][:, :], func=AF.Copy), [PB[hf]], [h2T])
                for k in range(8):
                    T(lambda e: e.matmul(PB[2][:, 0:256], lhsT=h2T[:, k, :], rhs=wr[:, k, :], start=(k == 0), stop=(k == 7)), [h2T, wr], [PB[2]])
                A(lambda e: e.activation(out=s_[:], in_=PB[2][:, 0:256], func=AF.Sigmoid), [PB[2]], [s_])
                V(lambda e: e.tensor_tensor(out=ssel[:], in0=s_[:], in1=rbias[:], op=ALU.add), [s_, rbias], [ssel])
                for g in range(8):
                    V(lambda e: e.max(out=m8[:, g, :], in_=ssel[:, g * 32:(g + 1) * 32]), [ssel], [m8])
                V(lambda e: e.tensor_tensor(out=gs[:], in0=m8[:, :, 0], in1=m8[:, :, 1], op=ALU.add), [m8], [gs])
                V(lambda e: e.max(out=d8[:], in_=gs[:]), [gs], [d8])
                V(lambda e: e.tensor_scalar(out=gm[:], in0=gs[:], scalar1=d8[:, 3:4], scalar2=None, op0=ALU.is_ge), [gs, d8], [gm])
                for g in range(8):
                    V(lambda e: e.tensor_scalar(out=msk[:, g * 32:(g + 1) * 32], in0=ssel[:, g * 32:(g + 1) * 32], scalar1=4.0, scalar2=gm[:, g:g + 1],
                                                op0=ALU.add, op1=ALU.mult), [ssel, gm], [msk])
                V(lambda e: e.max(out=d8[:], in_=msk[:]), [msk], [d8])
                V(lambda e: e.tensor_scalar(out=self_[:], in0=msk[:], scalar1=d8[:, 7:8], scalar2=None, op0=ALU.is_ge), [msk, d8], [self_])
                V(lambda e: e.tensor_copy(out=selb_all[:, i, :], in_=self_[:]), [self_], [selb_all])
                V(lambda e: e.tensor_tensor(out=wn_all[:, i, :], in0=s_[:], in1=self_[:], op=ALU.mult), [s_, self_], [wn_all])
                V(lambda e: e.tensor_reduce(out=wsum[:, 0:1], in_=wn_all[:, i, :], axis=mybir.AxisListType.X, op=ALU.add), [wn_all], [wsum])
                V(lambda e: e.reciprocal(out=wsum[:, 1:2], in_=wsum[:, 0:1]), [wsum], [wsum])
                V(lambda e: e.tensor_scalar(out=wn_all[:, i, :], in0=wn_all[:, i, :], scalar1=wsum[:, 1:2], scalar2=2.5, op0=ALU.mult, op1=ALU.mult), [wn_all, wsum], [wn_all])
                T(lambda e: e.matmul(PB[3][:, 0:256], lhsT=ones_b[:], rhs=selb_all[:, i, :], start=True, stop=True), [ones_b, selb_all], [PB[3]])
                V(lambda e: e.tensor_tensor(out=cnt[:], in0=PB[3][:, 0:256], in1=cnt[:], op=ALU.add), [PB[3], cnt], [cnt])
                dump("h2_0", h2_[:], [128, D], F32, [h2_])
                dump("s_0", s_[:], [128, 256], F32, [s_])
                dump("self0", self_[:], [128, 256], F32, [self_])
                dump("gs0", gs[:], [128, 8], F32, [gs])
                for k in range(8):
                    T(lambda e: e.matmul(PB[4][:, :], lhsT=h2T[:, k, :], rhs=wsg[:, k, :], start=(k == 0), stop=(k == 7)), [h2T, wsg], [PB[4]])
                A(lambda e: e.activation(out=sg[:], in_=PB[4][:, 0:256], func=AF.Silu), [PB[4]], [sg])
                V(lambda e: e.tensor_tensor(out=hs[:], in0=PB[4][:, 256:512], in1=sg[:], op=ALU.mult), [PB[4], sg], [hs])
                for j in range(2):
                    T(lambda e: e.transpose(out=PB[5][:, j * 128:(j + 1) * 128], in_=hs[:, j * 128:(j + 1) * 128], identity=ident_f[:]), [hs, ident_f], [PB[5]])
                A(lambda e: e.activation(out=hsT[:].rearrange("p j t -> p (j t)"), in_=PB[5][:, 0:256], func=AF.Copy), [PB[5]], [hsT])
                for hf in range(2):
                    pb = PB[6 + hf]
                    for j in range(2):
                        T(lambda e: e.matmul(pb[:, :], lhsT=hsT[:, j, :], rhs=wsd[:, j, hf * 512:(hf + 1) * 512], start=(j == 0), stop=(j == 1)),
                          [hsT, wsd], [pb])
                    V(lambda e: e.tensor_tensor(out=ysh[:, hf * 512:(hf + 1) * 512], in0=pb[:, :], in1=rows[:, R_GT2, hf * 512:(hf + 1) * 512], op=ALU.mult),
                      [pb, rows], [ysh])
                G(lambda e: e.tensor_tensor(out=x1[:, i, :], in0=x1[:, i, :], in1=ysh[:], op=ALU.add), [x1, ysh], [x1])
            dump("x1s", x1[:], [128, NQT, D], F32, [x1])
            dump("cnt", cnt[:], [128, 256], F32, [cnt])
            ci = sb(es, "ci", [128, 256], I32)
            padf = sb(es, "padf", [128, 256], F32)
            pend = sb(es, "pend", [128, 256], F32)
            pst1 = sb(es, "pst1", [128, 256], F32)
            thr = sb(es, "thr", [128, 3], F32)
            thr_i = sb(es, "thr_i", [128, 3], I32)
            blk = sb(es, "blk", [128, 3], F32)
            dg = sb(es, "dg", [128, 128], F32)
            iop = sb(es, "iop", [128, 1], F32)
            iop_i = sb(es, "iop_i", [128, 1], I32)
            V(lambda e: e.memset(padf[:], 0.0), [], [padf])
            for m in range(16):
                V(lambda e: e.scalar_tensor_tensor(out=padf[:], in0=cnt[:], scalar=128.0 * m, in1=padf[:], op0=ALU.is_gt, op1=ALU.add), [cnt, padf], [padf])
            V(lambda e: e.tensor_scalar(out=padf[:], in0=padf[:], scalar1=128.0, scalar2=None, op0=ALU.mult), [padf], [padf])
            V(lambda e: e.memset(msk[:], 1.0), [], [msk])
            V(lambda e: e.tensor_tensor_scan(out=pend[:], data0=msk[:], data1=padf[:], initial=0.0, op0=ALU.mult, op1=ALU.add), [padf, msk], [pend])
            V(lambda e: e.tensor_tensor(out=pst1[:], in0=pend[:], in1=padf[:], op=ALU.subtract), [pend, padf], [pst1])
            V(lambda e: e.tensor_scalar(out=pst1[:], in0=pst1[:], scalar1=1.0, scalar2=None, op0=ALU.add), [pst1], [pst1])
            G(lambda e: e.iota(thr_i[:], pattern=[[128 * 128, 3]], base=0, channel_multiplier=128), [], [thr_i])
            V(lambda e: e.tensor_copy(out=thr[:], in_=thr_i[:]), [thr_i], [thr])
            G(lambda e: e.iota(iop_i[:], pattern=[[0, 1]], base=0, channel_multiplier=1), [], [iop_i])
            V(lambda e: e.tensor_copy(out=iop[:], in_=iop_i[:]), [iop_i], [iop])
            for j in range(3):
                V(lambda e: e.tensor_scalar(out=Dm[:], in0=pend[:], scalar1=thr[:, j:j + 1], scalar2=None, op0=ALU.is_le), [pend, thr], [Dm])
                V(lambda e: e.tensor_reduce(out=blk[:, j:j + 1], in_=Dm[:], axis=mybir.AxisListType.X, op=ALU.add), [Dm], [blk])
            for j in range(3):
                V(lambda e: e.tensor_scalar(out=dg[:], in0=ident_f[:], scalar1=blk[:, j:j + 1], scalar2=None, op0=ALU.mult), [ident_f, blk], [dg])
                T(lambda e: e.matmul(PB[2][:, 0:128], lhsT=ones_f[:], rhs=dg[:], start=True, stop=True), [ones_f, dg], [PB[2]])
                V(lambda e: e.tensor_scalar(out=idxw[:, j * 128:(j + 1) * 128], in0=PB[2][:, 0:128], scalar1=128.0, scalar2=iop[:, 0:1], op0=ALU.mult, op1=ALU.add),
                  [PB[2], iop], [idxw])
            dump("pend", pend[:], [128, 256], F32, [pend])
            dump("blk", blk[:], [128, 3], F32, [blk])
            dump("idxw", idxw[:], [128, NBLK], I32, [idxw])
            h2b = ring(es, "h2b", 2, [128, D], BF16)
            for i in range(NQT):
                h2f = h2[i % 2]
                h2_ = h2b[i % 2]
                DM("sync", lambda e: e.dma_start(out=h2f[:], in_=h2_d[i * 128:(i + 1) * 128, :]), [R_h2d], [h2f])
                A(lambda e: e.activation(out=h2_[:], in_=h2f[:], func=AF.Copy), [h2f], [h2_])
                T(lambda e: e.matmul(PB[3][:, 0:256], lhsT=tri_b[:], rhs=selb_all[:, i, :], start=True, stop=True), [tri_b, selb_all], [PB[3]])
                T(lambda e: e.matmul(PB[3][:, 256:512], lhsT=ones_b[:], rhs=selb_all[:, i, :], start=False, stop=True, skip_group_check=True), [ones_b, selb_all], [PB[3]])
                V(lambda e: e.tensor_tensor(out=Dm[:], in0=PB[3][:, 0:256], in1=base[:], op=ALU.add), [PB[3], base], [Dm])
                V(lambda e: e.tensor_tensor(out=Dm[:], in0=Dm[:], in1=pst1[:], op=ALU.add), [Dm, pst1], [Dm])
                V(lambda e: e.tensor_tensor(out=Dm[:], in0=Dm[:], in1=selb_all[:, i, :], op=ALU.mult), [Dm, selb_all], [Dm])
                V(lambda e: e.tensor_tensor(out=base[:], in0=PB[3][:, 256:512], in1=base[:], op=ALU.add), [PB[3], base], [base])
                V(lambda e: e.max(out=d8[:], in_=Dm[:]), [Dm], [d8])
                V(lambda e: e.tensor_scalar(out=idx_all[:, i, :], in0=d8[:], scalar1=-1.0, scalar2=None, op0=ALU.add), [d8], [idx_all])
                for k in range(8):
                    V(lambda e: e.tensor_scalar(out=msk[:], in0=Dm[:], scalar1=d8[:, k:k + 1], scalar2=None, op0=ALU.is_equal), [Dm, d8], [msk])
                    V(lambda e: e.tensor_tensor(out=msk[:], in0=msk[:], in1=wn_all[:, i, :], op=ALU.mult), [msk, wn_all], [msk])
                    V(lambda e: e.tensor_reduce(out=wk_all[:, i, k:k + 1], in_=msk[:], axis=mybir.AxisListType.X, op=ALU.add), [msk], [wk_all])
                dump("Dm0", Dm[:], [128, 256], F32, [Dm])
                for k in range(8):
                    DM("gpsimd", lambda e: e.indirect_dma_start(out=xg, out_offset=bass.IndirectOffsetOnAxis(ap=idx_all[:, i, k:k + 1], axis=0),
                                                                in_=h2_[:], in_offset=None), [h2_, idx_all], [R_xg], nowaw=True)
            dump("idx", idx_all[:], [128, NQT, 8], I32, [idx_all])
            dump("wk", wk_all[:], [128, NQT, 8], F32, [wk_all])
            dump("base", base[:], [128, 256], F32, [base])
            allb = [wr, wsg, wsd, rbias, base, cnt, selb_all, wn_all, h2T, junk, s_, ssel, m8, gs, gm, msk, self_, Dm, d8, wsum, sg, hs, hsT, ysh,
                    ci, padf, pend, pst1, thr, thr_i, blk, dg, iop, iop_i] + h2 + st + h2b
            scope_end(allb)

        w1v = w1.rearrange("e (p k) n -> (e p) (k n)", k=8)
        w3v = w3.rearrange("e (p k) n -> (e p) (k n)", k=8)
        w2v = w2.rearrange("e (p j) n -> (e p) (j n)", j=2)
        with ExitStack() as es:
            NS = 3
            wg1f = ring(es, "wg1f", NS, [128, 2048], F32)
            wg3f = ring(es, "wg3f", NS, [128, 2048], F32)
            wdf = ring(es, "wdf", NS, [128, 2048], F32)
            wg1 = ring(es, "wg1", 2, [128, 8, 256], BF16)
            wg3 = ring(es, "wg3", 2, [128, 8, 256], BF16)
            wd = ring(es, "wd", 2, [128, 2, D], BF16)
            Xe = ring(es, "Xe", NS + 1, [128, D], BF16)
            XeT = ring(es, "XeT", 2, [128, 8, 128], BF16)
            sgr = ring(es, "sgr", 2, [128, 256], F32)
            he = ring(es, "he", 2, [128, 256], BF16)
            heT = ring(es, "heT", 2, [128, 2, 128], BF16)
            Ye = ring(es, "Ye", 2, [128, D], BF16)

            breg = {}

            def gat(e, dst, src, off):
                if "r" not in breg:
                    breg["r"] = e.to_reg(NEXP * 128 - 1)
                return e.indirect_dma_start(out=dst, out_offset=None, in_=src, in_offset=off, bounds_check=breg["r"], oob_is_err=False)

            def gathers(bi):
                b = bi % NS
                off = bass.IndirectOffsetOnAxis(ap=idxw[:, bi:bi + 1], axis=0)
                DM("gpsimd", lambda e, d=wg1f[b][:], o=off: gat(e, d, w1v, o), [idxw], [wg1f[b]], eager=False)
                DM("gpsimd", lambda e, d=wg3f[b][:], o=off: gat(e, d, w3v, o), [idxw], [wg3f[b]], eager=False)
                DM("gpsimd", lambda e, d=wdf[b][:], o=off: gat(e, d, w2v, o), [idxw], [wdf[b]], eager=False)
                xe = Xe[bi % (NS + 1)]
                DM("sync", lambda e: e.dma_start(out=xe[:], in_=xg[bi * 128:(bi + 1) * 128, :]), [R_xg], [xe])

            def casts(bi):
                b = bi % 2
                f = bi % NS
                A(lambda e: e.activation(out=wg1[b][:].rearrange("p k n -> p (k n)"), in_=wg1f[f][:], func=AF.Copy), [wg1f[f]], [wg1[b]])
                V(lambda e: e.tensor_copy(out=wg3[b][:].rearrange("p k n -> p (k n)"), in_=wg3f[f][:]), [wg3f[f]], [wg3[b]])
                A(lambda e: e.activation(out=wd[b][:].rearrange("p j n -> p (j n)"), in_=wdf[f][:], func=AF.Copy), [wdf[f]], [wd[b]])

            def stage1(bi):
                b = bi % 2
                xe = Xe[bi % (NS + 1)]
                for k in range(8):
                    T(lambda e: e.transpose(out=pbf(0)[:, k * 128:(k + 1) * 128], in_=xe[:, k * 128:(k + 1) * 128], identity=ident_b[:]), [xe, ident_b], [PB[0]])
                A(lambda e: e.activation(out=XeT[b][:].rearrange("p k t -> p (k t)"), in_=pbf(0)[:, :], func=AF.Copy), [PB[0]], [XeT[b]])
                pg = PB[2 + b]
                for k in range(8):
                    T(lambda e: e.matmul(pg[:, 0:256], lhsT=XeT[b][:, k, :], rhs=wg1[b][:, k, :], start=(k == 0), stop=(k == 7)), [XeT[b], wg1[b]], [pg])
                for k in range(8):
                    T(lambda e: e.matmul(pg[:, 256:512], lhsT=XeT[b][:, k, :], rhs=wg3[b][:, k, :], start=(k == 0), stop=(k == 7), skip_group_check=True), [XeT[b], wg3[b]], [pg])

            def stage2(bi):
                b = bi % 2
                pg = PB[2 + b]
                A(lambda e: e.activation(out=sgr[b][:], in_=pg[:, 0:256], func=AF.Silu), [pg], [sgr[b]])
                V(lambda e: e.tensor_tensor(out=he[b][:].rearrange("t (j p) -> t p j", j=2), in0=pg.t[:, 256:512].rearrange("t (p j) -> t p j", j=2),
                                            in1=sgr[b][:].rearrange("t (p j) -> t p j", j=2), op=ALU.mult), [pg, sgr[b]], [he[b]])
                for j in range(2):
                    T(lambda e: e.transpose(out=pbf(1)[:, j * 128:(j + 1) * 128], in_=he[b][:, j * 128:(j + 1) * 128], identity=ident_b[:]), [he[b], ident_b], [PB[1]])
                V(lambda e: e.tensor_copy(out=heT[b][:].rearrange("p j t -> p (j t)"), in_=pbf(1)[:, 0:256]), [PB[1]], [heT[b]])
                for hf in range(2):
                    pb = PB[4 + 2 * b + hf]
                    for j in range(2):
                        T(lambda e: e.matmul(pb[:, :], lhsT=heT[b][:, j, :], rhs=wd[b][:, j, hf * 512:(hf + 1) * 512], start=(j == 0), stop=(j == 1)),
                          [heT[b], wd[b]], [pb])
                V(lambda e: e.tensor_copy(out=Ye[b][:, 0:512], in_=PB[4 + 2 * b][:, :]), [PB[4 + 2 * b]], [Ye[b]])
                V(lambda e: e.tensor_copy(out=Ye[b][:, 512:1024], in_=PB[5 + 2 * b][:, :]), [PB[5 + 2 * b]], [Ye[b]])
                dump("he0", he[b][:], [128, 256], BF16, [he[b]])
                dump("Ye0", Ye[b][:], [128, D], BF16, [Ye[b]])
                DM("sync", lambda e: e.dma_start(out=yg[bi * 128:(bi + 1) * 128, :], in_=Ye[b][:]), [Ye[b]], [R_yg], nowaw=True)

            for g0 in range(NS):
                gathers(g0)
            casts(0)
            stage1(0)
            for bi in range(NBLK):
                if bi + NS < NBLK:
                    gathers(bi + NS)
                if bi + 1 < NBLK:
                    casts(bi + 1)
                    stage1(bi + 1)
                stage2(bi)
            allb = wg1f + wg3f + wdf + wg1 + wg3 + wd + Xe + XeT + sgr + he + heT + Ye
            scope_end(allb)

        R_out = Res("out")
        with ExitStack() as es:
            Yg = ring(es, "Yg", 4, [128, D], BF16)
            acc = ring(es, "acc", 2, [128, D], F32)
            st = ring(es, "st3", 2, [128, 4], F32)
            junk = sb(es, "junk3", [128, D], F32)
            gi = 0
            for i in range(NQT):
                ac = acc[i % 2]
                s2 = st[i % 2]
                for k in range(8):
                    y_ = Yg[gi % 4]
                    gi += 1
                    DM("gpsimd", lambda e, y_=y_, i=i, k=k: e.indirect_dma_start(out=y_[:], out_offset=None, in_=yg,
                                                                                 in_offset=bass.IndirectOffsetOnAxis(ap=idx_all[:, i, k:k + 1], axis=0)),
                       [R_yg, idx_all], [y_])
                    if k == 0:
                        V(lambda e, y_=y_, ac=ac, i=i: e.tensor_scalar(out=ac[:], in0=y_[:], scalar1=wk_all[:, i, 0:1], scalar2=None, op0=ALU.mult), [y_, wk_all], [ac])
                    else:
                        V(lambda e, y_=y_, ac=ac, i=i, k=k: e.scalar_tensor_tensor(out=ac[:], in0=y_[:], scalar=wk_all[:, i, k:k + 1], in1=ac[:], op0=ALU.mult, op1=ALU.add),
                          [y_, wk_all, ac], [ac])
                V(lambda e, ac=ac: e.tensor_tensor(out=ac[:], in0=ac[:], in1=rows[:, R_GT2, :], op=ALU.mult), [ac, rows], [ac])
                V(lambda e, ac=ac, i=i: e.tensor_tensor(out=ac[:], in0=ac[:], in1=x1[:, i, :], op=ALU.add), [ac, x1], [ac])
                A(lambda e, ac=ac, s2=s2: e.activation(out=junk[:], in_=ac[:], func=AF.Square, accum_out=s2[:, 0:1]), [ac], [junk, s2])
                V(lambda e, s2=s2: e.tensor_scalar(out=s2[:, 1:2], in0=s2[:, 0:1], scalar1=1.0 / D, scalar2=EPS, op0=ALU.mult, op1=ALU.add), [s2], [s2])
                G(lambda e, s2=s2: e.tensor_tensor(out=s2[:, 2:3], in0=s2[:, 1:2], in1=mhalf[:, 0:1], op=ALU.pow), [s2, mhalf], [s2])
                V(lambda e, ac=ac, s2=s2: e.scalar_tensor_tensor(out=ac[:], in0=ac[:], scalar=s2[:, 2:3], in1=rows[:, R_GF, :], op0=ALU.mult, op1=ALU.mult),
                  [ac, s2, rows], [ac])
                DM("sync", lambda e, ac=ac, i=i: e.dma_start(out=out[i * 128:(i + 1) * 128, :], in_=ac[:]), [ac], [R_out], nowaw=True)
            allb = Yg + acc + st + [junk]
            scope_end(allb + [R_out, R_dump])
        fw.finish()
    return nc


def _rope_tables(order_pos):
    n = order_pos.shape[0]
    pos = np.maximum(order_pos, 0)
    row = (pos // 64).astype(np.float32)
    col = (pos % 64).astype(np.float32)
    isctx = (order_pos < 0)

    def theta(rot_dim):
        nf = rot_dim // 4
        inv = (np.float32(10000.0) ** (-(np.arange(nf, dtype=np.float32) / np.float32(nf)))).astype(np.float32)
        th = np.concatenate([row[:, None] * inv, col[:, None] * inv], axis=-1).astype(np.float32)
        th[isctx] = 0.0
        return th

    thm = theta(32)
    thd = theta(64)
    tabm = np.zeros((2, 128, n), np.float32)
    tabm[0, 0:64, :] = 1.0
    cm, sm = np.cos(thm).T, np.sin(thm).T
    tabm[0, 64:80], tabm[0, 80:96] = cm, cm
    tabm[1, 64:80], tabm[1, 80:96] = -sm, sm
    tabd = np.zeros((2, 128, n), np.float32)
    cd, sd = np.cos(thd).T, np.sin(thd).T
    for g in range(2):
        tabd[0, g * 64:g * 64 + 32], tabd[0, g * 64 + 32:g * 64 + 64] = cd, cd
        tabd[1, g * 64:g * 64 + 32], tabd[1, g * 64 + 32:g * 64 + 64] = -sd, sd
    return tabm, tabd


def _swap_halves(w, group):
    shp = w.shape
    v = w.reshape(shp[0], -1, 2, group // 2)
    return np.ascontiguousarray(v[:, :, ::-1, :]).reshape(shp)


_PROGRAM = None


def kernel(x, c, ctx, c_ctx, w_mod, b_mod, g_attn, g_ffn, w_in, g_q_lat, w_uq, g_kv_lat, w_ukv,
           lam_q1, lam_k1, lam_q2, lam_k2, g_subln, w_out, w_router, router_bias, w1, w3, w2,
           ws1, ws3, ws2, g_final):
    global _PROGRAM
    f = lambda a: np.ascontiguousarray(np.asarray(a, dtype=np.float32))
    x, c, ctx, c_ctx = f(x), f(c), f(ctx), f(c_ctx)
    w_in0 = f(w_in)[0]
    cq, ckv, kr = w_in0[:, 0:256], w_in0[:, 256:384], w_in0[:, 384:416]
    dq, dk, dv = w_in0[:, 416:928], w_in0[:, 928:1440], w_in0[:, 1440:1952]
    krp = np.zeros((D, 2, 96), np.float32)
    krp[:, 0, 64:96] = kr
    krp[:, 1, 64:96] = _swap_halves(kr, 32)
    dqw = np.stack([dq, _swap_halves(dq, 64)], axis=1)
    dkw = np.stack([dk, _swap_halves(dk, 64)], axis=1)
    uq = f(w_uq)[0].reshape(256, 8, 96)
    uqp = np.zeros((256, 8, 96), np.float32)
    uqp[:, :, 64:96] = _swap_halves(np.ascontiguousarray(uq[:, :, 64:96]).reshape(256, 256), 32).reshape(256, 8, 32)
    uqw = np.ascontiguousarray(np.stack([uq, uqp], axis=1))
    ukv = f(w_ukv)[0].reshape(128, 8, 128)
    wkn = np.ascontiguousarray(ukv[:, :, 0:64]).reshape(128, 512)
    wv = np.ascontiguousarray(ukv[:, :, 64:128]).reshape(128, 512)
    shared = {
        "w_mod": f(w_mod)[0], "b_mod": f(b_mod), "g_attn": f(g_attn).reshape(8, 128), "g_ffn": f(g_ffn), "g_final": f(g_final).reshape(1, D),
        "w_cq": f(cq), "w_ckv": f(ckv), "w_krp": krp, "w_dq": f(dqw), "w_dk": f(dkw), "w_dv": f(dv),
        "g_q": f(g_q_lat).reshape(2, 128), "w_uq": uqw, "g_kv": f(g_kv_lat).reshape(128, 1), "w_kn": wkn, "w_v": wv,
        "lam4": np.concatenate([f(lam_q1), f(lam_k1), f(lam_q2), f(lam_k2)], axis=0), "g_sub": f(g_subln),
        "w_out": f(w_out)[0], "w_router": f(w_router)[0], "r_bias": f(router_bias),
        "w1": f(w1)[0], "w3": f(w3)[0], "w2": f(w2)[0], "ws1": f(ws1)[0], "ws3": f(ws3)[0], "ws2": f(ws2)[0],
    }
    in_maps = []
    for core in range(NCORES):
        b, qh = core // 2, core % 2
        own = slice(qh * 2048, (qh + 1) * 2048)
        oth = slice((1 - qh) * 2048, (2 - qh) * 2048)
        xkc = np.concatenate([ctx[b], x[b, oth], x[b, own]], axis=0)
        pos = np.concatenate([-np.ones(256, np.int64), np.arange(oth.start, oth.stop), np.arange(own.start, own.stop)])
        tabm, tabd = _rope_tables(pos)
        m = dict(shared)
        m.update({"xk": np.ascontiguousarray(xkc), "cv": np.ascontiguousarray(np.stack([c[b], c_ctx], axis=0)), "tabm": tabm, "tabd": tabd})
        in_maps.append(m)
    if _PROGRAM is None:
        _PROGRAM = build_program()
    res = run_bass_kernel_spmd(_PROGRAM, in_maps, core_ids=list(range(NCORES)))
    outp = np.zeros((4, 4096, D), np.float32)
    for core in range(NCORES):
        b, qh = core // 2, core % 2
        outp[b, qh * 2048:(qh + 1) * 2048] = np.asarray(res.results[core]["out"], dtype=np.float32)
    return outp
```
